# Optimizing a Trainium2 kernel written in Bass

```python
import jax, jax.numpy as jnp
from jax import lax
import numpy as np

D_MODEL = 1024
BATCH = 8
SEQ = 2048
DEPTH = 2

HEAD_DIM = 64
RET_HEADS = 4
RET_W = RET_HEADS * HEAD_DIM
RET_CHUNK = 128
ROPE_BASE = 10000.0
MOBA_HEADS = 6
MOBA_W = MOBA_HEADS * HEAD_DIM
MOBA_BLOCK = 256
MOBA_TOPK = 3
MOBA_QCHUNK = 32
LRU_BLOCKS = 6
LRU_BLOCK_W = 64
LRU_W = LRU_BLOCKS * LRU_BLOCK_W
CONV_WIDTH = 4
LRU_C = 8.0
MIX_W = RET_W + MOBA_W + LRU_W
IN_SIZES = [RET_W] * 4 + [MOBA_W] * 3 + [LRU_W] * 2
IN_W = sum(IN_SIZES)
IN_OFFSETS = np.cumsum(IN_SIZES)[:-1].tolist()
N_EXPERTS = 32
TOPK_EXPERTS = 4
D_FF = D_MODEL
SWIGLU_LIMIT = 7.0
SWIGLU_ALPHA = 1.702
EXPERT_BLOCK = 256
NORM_EPS = 1e-6

kernel_name = "hybrid_retention_moba_rglru_moe_adaln"


def rms_norm(x, w):
    xf = x.astype(jnp.float32)
    y = xf * lax.rsqrt(jnp.mean(xf * xf, axis=-1, keepdims=True) + NORM_EPS)
    return (y * w.astype(jnp.float32)).astype(x.dtype)


def rotary(x, pos):
    half = x.shape[-1] // 2
    inv = 1.0 / (ROPE_BASE ** (jnp.arange(half, dtype=jnp.float32) / half))
    ang = pos.astype(jnp.float32)[:, None] * inv[None, :]
    cos = jnp.cos(ang)[None, :, None, :]
    sin = jnp.sin(ang)[None, :, None, :]
    xf = x.astype(jnp.float32)
    x1, x2 = xf[..., :half], xf[..., half:]
    return jnp.concatenate([x1 * cos - x2 * sin, x2 * cos + x1 * sin], axis=-1)


def retention(q, k, v, g, norm_w):
    B, S, _ = q.shape
    H, dh, C = RET_HEADS, HEAD_DIM, RET_CHUNK
    N = S // C
    pos = jnp.arange(S)
    qh = rotary(q.reshape(B, S, H, dh), pos)
    kh = rotary(k.reshape(B, S, H, dh), pos) * (dh ** -0.5)
    vh = v.reshape(B, S, H, dh).astype(jnp.float32)
    to_chunks = lambda t: t.reshape(B, N, C, H, dh).transpose(0, 3, 1, 2, 4)
    qc, kc, vc = to_chunks(qh), to_chunks(kh), to_chunks(vh)
    log_g = jnp.log1p(-jnp.exp2(-5.0 - jnp.arange(H, dtype=jnp.float32)))
    idx = jnp.arange(C, dtype=jnp.float32)
    diff = idx[:, None] - idx[None, :]
    dmat = jnp.where(diff >= 0, jnp.exp(log_g[:, None, None] * jnp.maximum(diff, 0.0)), 0.0)
    scores = jnp.einsum('bhnid,bhnjd->bhnij', qc, kc) * dmat[None, :, None]
    intra = jnp.einsum('bhnij,bhnje->bhnie', scores, vc)
    zeta = jnp.exp(log_g[:, None] * (C - 1 - idx)[None, :])
    xi = jnp.exp(log_g[:, None] * (idx + 1)[None, :])
    kv = jnp.einsum('bhnjd,bhnje->nbhde', kc * zeta[None, :, None, :, None], vc)
    chunk_decay = jnp.exp(log_g * C)[None, :, None, None]

    def step(state, kv_n):
        return state * chunk_decay + kv_n, state

    _, prev = lax.scan(step, jnp.zeros((B, H, dh, dh), jnp.float32), kv)
    cross = jnp.einsum('bhnid,nbhde->bhnie', qc, prev) * xi[None, :, None, :, None]
    y = (intra + cross).transpose(0, 2, 3, 1, 4).reshape(B, S, H, dh)
    mu = jnp.mean(y, axis=-1, keepdims=True)
    var = jnp.mean(jnp.square(y - mu), axis=-1, keepdims=True)
    y = ((y - mu) * lax.rsqrt(var + NORM_EPS)).reshape(B, S, RET_W) * norm_w.astype(jnp.float32)
    return (jax.nn.silu(g.astype(jnp.float32)) * y).astype(q.dtype)


def moba_attention(q, k, v):
    B, S, _ = q.shape
    H, dh, Bk, Qc = MOBA_HEADS, HEAD_DIM, MOBA_BLOCK, MOBA_QCHUNK
    NBk = -(-S // Bk)
    Sp = NBk * Bk
    topk = min(MOBA_TOPK, NBk)
    to_heads = lambda t: t.reshape(B, S, H, dh).transpose(0, 2, 1, 3).astype(jnp.float32)
    qh, kh, vh = to_heads(q), to_heads(k), to_heads(v)
    pad = ((0, 0), (0, 0), (0, Sp - S), (0, 0))
    kb = jnp.pad(kh, pad).reshape(B, H, NBk, Bk, dh)
    vb = jnp.pad(vh, pad).reshape(B, H, NBk, Bk, dh)
    kmean = jnp.mean(kb, axis=3)
    NQ = S // Qc
    qch = qh.reshape(B, H, NQ, Qc, dh).transpose(2, 0, 1, 3, 4)
    bi = jnp.arange(B)[:, None, None, None]
    hi = jnp.arange(H)[None, :, None, None]
    scale = dh ** -0.5
    blk_ids = jnp.arange(NBk)

    def chunk(args):
        qq, ci = args
        qpos = ci * Qc + jnp.arange(Qc)
        own = (ci * Qc) // Bk
        gs = jnp.einsum('bhqd,bhnd->bhqn', qq, kmean)
        gs = jnp.where(blk_ids < own, gs, -jnp.inf)
        _, sel = lax.top_k(gs, topk)
        valid = sel < own
        kg = kb[bi, hi, sel]
        vg = vb[bi, hi, sel]
        s_sel = jnp.einsum('bhqd,bhqnkd->bhqnk', qq, kg) * scale
        s_sel = jnp.where(valid[..., None], s_sel, -jnp.inf).reshape(B, H, Qc, topk * Bk)
        ko = lax.dynamic_index_in_dim(kb, own, axis=2, keepdims=False)
        vo = lax.dynamic_index_in_dim(vb, own, axis=2, keepdims=False)
        kpos = own * Bk + jnp.arange(Bk)
        s_own = jnp.einsum('bhqd,bhkd->bhqk', qq, ko) * scale
        s_own = jnp.where(kpos[None, :] <= qpos[:, None], s_own, -jnp.inf)
        p = jax.nn.softmax(jnp.concatenate([s_sel, s_own], axis=-1), axis=-1)
        p_sel = p[..., :topk * Bk].reshape(B, H, Qc, topk, Bk)
        p_own = p[..., topk * Bk:]
        return (jnp.einsum('bhqnk,bhqnkd->bhqd', p_sel, vg)
                + jnp.einsum('bhqk,bhkd->bhqd', p_own, vo))

    out = lax.map(chunk, (qch, jnp.arange(NQ)))
    return out.transpose(1, 0, 3, 2, 4).reshape(B, S, MOBA_W).astype(q.dtype)


def rg_lru_branch(xb, gb, conv_w, conv_b, wa, ba, wx, bx, lam):
    B, S, _ = xb.shape
    u = lax.conv_general_dilated(xb, conv_w[:, None, :], window_strides=(1,),
                                 padding=[(CONV_WIDTH - 1, 0)],
                                 dimension_numbers=('NWC', 'WIO', 'NWC'),
                                 feature_group_count=LRU_W) + conv_b
    uf = u.astype(jnp.float32)
    ub = uf.reshape(B, S, LRU_BLOCKS, LRU_BLOCK_W)
    r = jax.nn.sigmoid(jnp.einsum('bsgi,gij->bsgj', ub, wa.astype(jnp.float32)).reshape(B, S, LRU_W) + ba)
    i = jax.nn.sigmoid(jnp.einsum('bsgi,gij->bsgj', ub, wx.astype(jnp.float32)).reshape(B, S, LRU_W) + bx)
    log_a = -LRU_C * r * jax.nn.softplus(-lam.astype(jnp.float32))
    a = jnp.exp(log_a)
    b = jnp.sqrt(-jnp.expm1(2.0 * log_a)) * (i * uf)

    def combine(left, right):
        a1, b1 = left
        a2, b2 = right
        return a1 * a2, a2 * b1 + b2

    _, h = lax.associative_scan(combine, (a, b), axis=1)
    return (jax.nn.gelu(gb.astype(jnp.float32)) * h).astype(xb.dtype)


def moe_ffn(h, router_w, router_b, w_gu, b_gu, w_dn, b_dn):
    B, S, D = h.shape
    T = B * S
    ht = h.reshape(T, D)
    logits = (ht @ router_w + router_b).astype(jnp.float32)
    top_v, top_i = lax.top_k(logits, TOPK_EXPERTS)
    gates = jax.nn.softmax(top_v, axis=-1)
    A = T * TOPK_EXPERTS
    e_flat = top_i.reshape(A)
    tok_flat = jnp.repeat(jnp.arange(T), TOPK_EXPERTS)
    g_flat = gates.reshape(A)
    order = jnp.argsort(e_flat, stable=True)
    e_sorted, tok_sorted, g_sorted = e_flat[order], tok_flat[order], g_flat[order]
    counts = jnp.bincount(e_flat, length=N_EXPERTS)
    padded = ((counts + EXPERT_BLOCK - 1) // EXPERT_BLOCK) * EXPERT_BLOCK
    pend = jnp.cumsum(padded)
    pstart = pend - padded
    ustart = jnp.cumsum(counts) - counts
    ppos = pstart[e_sorted] + (jnp.arange(A) - ustart[e_sorted])
    NB = -(-A // EXPERT_BLOCK) + N_EXPERTS
    P = NB * EXPERT_BLOCK
    row_tok = jnp.zeros((P,), jnp.int32).at[ppos].set(tok_sorted.astype(jnp.int32))
    row_g = jnp.zeros((P,), jnp.float32).at[ppos].set(g_sorted)
    blk_e = jnp.minimum(jnp.searchsorted(pend, jnp.arange(NB) * EXPERT_BLOCK, side='right'), N_EXPERTS - 1)
    xr = ht[row_tok].reshape(NB, EXPERT_BLOCK, D)

    def expert_block(args):
        xb, e = args
        gu = (xb @ w_gu[e] + b_gu[e]).astype(jnp.float32)
        gate, up = gu[:, :D_FF], gu[:, D_FF:]
        gate = jnp.minimum(gate, SWIGLU_LIMIT)
        up = jnp.clip(up, -SWIGLU_LIMIT, SWIGLU_LIMIT)
        act = ((up + 1.0) * gate * jax.nn.sigmoid(SWIGLU_ALPHA * gate)).astype(xb.dtype)
        return act @ w_dn[e] + b_dn[e]

    yr = lax.map(expert_block, (xr, blk_e)).reshape(P, D)
    y = jnp.zeros((T, D), jnp.float32).at[row_tok].add(yr.astype(jnp.float32) * row_g[:, None])
    return y.reshape(B, S, D).astype(h.dtype)


def setup_inputs(seed: int = 0) -> dict:
    key = jax.random.key(seed)
    ks = jax.random.split(key, 24)
    f32 = jnp.float32
    nrm = lambda k, shape, s: jax.random.normal(k, shape, f32) * s
    u = jax.random.uniform(ks[12], (DEPTH, LRU_W), f32, 0.9, 0.999)
    a_base = u ** (1.0 / LRU_C)
    return {
        "x": nrm(ks[0], (BATCH, SEQ, D_MODEL), 1.0),
        "c": nrm(ks[1], (BATCH, D_MODEL), 1.0),
        "ada_w": nrm(ks[2], (DEPTH, D_MODEL, 6 * D_MODEL), 0.5 * D_MODEL ** -0.5),
        "ada_b": nrm(ks[3], (DEPTH, 6 * D_MODEL), 0.02),
        "norm_mix_w": 1.0 + nrm(ks[4], (DEPTH, D_MODEL), 0.02),
        "w_in": nrm(ks[5], (DEPTH, D_MODEL, IN_W), D_MODEL ** -0.5),
        "ret_norm_w": 1.0 + nrm(ks[6], (DEPTH, RET_W), 0.02),
        "lru_conv_w": nrm(ks[7], (DEPTH, CONV_WIDTH, LRU_W), CONV_WIDTH ** -0.5),
        "lru_conv_b": nrm(ks[8], (DEPTH, LRU_W), 0.02),
        "lru_gate_a_w": nrm(ks[9], (DEPTH, LRU_BLOCKS, LRU_BLOCK_W, LRU_BLOCK_W), LRU_BLOCK_W ** -0.5),
        "lru_gate_a_b": nrm(ks[10], (DEPTH, LRU_W), 0.02),
        "lru_gate_x_w": nrm(ks[11], (DEPTH, LRU_BLOCKS, LRU_BLOCK_W, LRU_BLOCK_W), LRU_BLOCK_W ** -0.5),
        "lru_gate_x_b": nrm(ks[13], (DEPTH, LRU_W), 0.02),
        "lru_lambda": jnp.log(a_base) - jnp.log1p(-a_base),
        "w_out": nrm(ks[14], (DEPTH, MIX_W, D_MODEL), MIX_W ** -0.5),
        "norm_ffn_w": 1.0 + nrm(ks[15], (DEPTH, D_MODEL), 0.02),
        "router_w": nrm(ks[16], (DEPTH, D_MODEL, N_EXPERTS), D_MODEL ** -0.5),
        "router_b": nrm(ks[17], (DEPTH, N_EXPERTS), 0.01),
        "moe_w_gu": nrm(ks[18], (DEPTH, N_EXPERTS, D_MODEL, 2 * D_FF), D_MODEL ** -0.5),
        "moe_b_gu": nrm(ks[19], (DEPTH, N_EXPERTS, 2 * D_FF), 0.02),
        "moe_w_down": nrm(ks[20], (DEPTH, N_EXPERTS, D_FF, D_MODEL), D_FF ** -0.5),
        "moe_b_down": nrm(ks[21], (DEPTH, N_EXPERTS, D_MODEL), 0.02),
        "final_norm_w": 1.0 + nrm(ks[22], (D_MODEL,), 0.02),
    }


def reference(x, c, ada_w, ada_b, norm_mix_w, w_in, ret_norm_w, lru_conv_w, lru_conv_b,
              lru_gate_a_w, lru_gate_a_b, lru_gate_x_w, lru_gate_x_b, lru_lambda, w_out,
              norm_ffn_w, router_w, router_b, moe_w_gu, moe_b_gu, moe_w_down, moe_b_down,
              final_norm_w):
    c_act = jax.nn.silu(c)
    for l in range(DEPTH):
        mod = c_act @ ada_w[l] + ada_b[l]
        shift1, scale1, gate1, shift2, scale2, gate2 = [m[:, None, :] for m in jnp.split(mod, 6, axis=-1)]
        h = rms_norm(x, norm_mix_w[l]) * (1.0 + scale1) + shift1
        proj = h @ w_in[l]
        rq, rk, rv, rg, mq, mk, mv, lx, lg = jnp.split(proj, IN_OFFSETS, axis=-1)
        y_ret = retention(rq, rk, rv, rg, ret_norm_w[l])
        y_moba = moba_attention(mq, mk, mv)
        y_lru = rg_lru_branch(lx, lg, lru_conv_w[l], lru_conv_b[l], lru_gate_a_w[l], lru_gate_a_b[l],
                              lru_gate_x_w[l], lru_gate_x_b[l], lru_lambda[l])
        mixed = jnp.concatenate([y_ret, y_moba, y_lru], axis=-1) @ w_out[l]
        x = x + gate1 * mixed
        h2 = rms_norm(x, norm_ffn_w[l]) * (1.0 + scale2) + shift2
        x = x + gate2 * moe_ffn(h2, router_w[l], router_b[l], moe_w_gu[l], moe_b_gu[l],
                                moe_w_down[l], moe_b_down[l])
    return rms_norm(x, final_norm_w)
```

```python
import numpy as np
from contextlib import ExitStack
import concourse.bass as bass
import concourse.mybir as mybir
from concourse.bass_utils import run_bass_kernel_spmd

F32 = mybir.dt.float32
F32R = mybir.dt.float32r
BF16 = mybir.dt.bfloat16
AF = mybir.ActivationFunctionType
ALU = mybir.AluOpType
AX = mybir.AxisListType

D = 1024
S_LEN = 2048
NL = 2
NE = 32
IN_W = 2944
EPS = 1e-6
NEG = -32768.0
NDMA = 24
SAME_ENGINE_SYNC = True
ENGS = ["sync", "scalar", "vector", "gpsimd", "tensor"]

SP_ADAB = 0
SP_NW1 = 48
SP_NW2 = 56
SP_RNW = 64
SP_CW = 68
SP_CB = 80
SP_BA = 83
SP_BX = 86
SP_LAM = 89
SP_BGU = 92
SP_N = 92 + 512


class Sched:
    def __init__(self):
        self.streams = {e: [] for e in ENGS}
        self.cnt = {e: 0 for e in ENGS}
        self.known = {e: {} for e in ENGS}
        self.lastw = {}
        self.rd = {}
        self.ndma = 0
        self.dma_val = [0] * NDMA

    def _deps(self, reads, writes):
        toks = []
        for k in reads:
            t = self.lastw.get(k)
            if t is not None:
                toks.append(t)
        for k in writes:
            t = self.lastw.get(k)
            if t is not None:
                toks.append(t)
            for s, v in self.rd.get(k, {}).items():
                toks.append((s, v))
        return toks

    def _commit(self, tok, reads, writes):
        s, v = tok
        for k in reads:
            d = self.rd.setdefault(k, {})
            if d.get(s, 0) < v:
                d[s] = v
        for k in writes:
            self.lastw[k] = tok
            self.rd[k] = {}

    def _filter(self, eng, toks):
        best = {}
        for s, v in toks:
            if s == eng and (eng == "tensor" or not SAME_ENGINE_SYNC):
                continue
            if v > best.get(s, 0):
                best[s] = v
        out = []
        kn = self.known[eng]
        for s, v in best.items():
            if kn.get(s, 0) >= v:
                continue
            kn[s] = v
            out.append((s, v))
        return out

    def op(self, eng, fn, reads=(), writes=()):
        toks = self._deps(reads, writes)
        waits = self._filter(eng, toks)
        self.cnt[eng] += 1
        tok = (eng, self.cnt[eng])
        self.streams[eng].append((waits, fn, tok))
        self._commit(tok, reads, writes)

    def dma(self, eng, fn, reads=(), writes=()):
        toks = self._deps(reads, writes)
        i = self.ndma % NDMA
        self.ndma += 1
        if self.dma_val[i] > 0:
            toks.append((("dma", i), self.dma_val[i]))
        self.dma_val[i] += 16
        tok = (("dma", i), self.dma_val[i])
        waits = self._filter(eng, toks)
        self.streams[eng].append((waits, fn, tok))
        self._commit(tok, reads, writes)

    def wait_keys(self, eng, keys):
        toks = self._deps(keys, keys)
        waits = self._filter(eng, toks)
        self.streams[eng].append((waits, None, None))


def tl(name, cs, t0, t1):
    return [(name, c, t) for c in cs for t in range(t0 // 128, (t1 + 127) // 128)]


def bk(name, t0=0, t1=S_LEN):
    return [(name, b) for b in range(t0 // 512, (t1 + 511) // 512)]


def build_program(L=NL, dbg=(), stop=None):
    nc = bass.Bass("TRN2", target_bir_lowering=False)
    S = Sched()
    dt = lambda name, shape, kind="ExternalInput": nc.dram_tensor(name, shape, F32, kind=kind).ap()
    x_d = dt("x", [S_LEN, D])
    sp_d = dt("sp", [NL, 128, SP_N])
    gsp_d = dt("gsp", [128, 16])
    rbt_d = dt("rbt", [NL, 128, NE])
    bdn_d = dt("bdn", [NL, NE, D])
    adaw_d = dt("ada_w", [NL, D, 6 * D])
    win_d = dt("w_in", [NL, D, IN_W])
    wout_d = dt("w_out", [NL, D, D])
    rw_d = dt("router_w", [NL, D, NE])
    if stop is None:
        wgu_d = dt("moe_w_gu", [NL, NE, D, 2 * D])
        wdn_d = dt("moe_w_down", [NL, NE, D, D])
    wabd_d = dt("wabd", [NL, 3, 128, 128])
    wxbd_d = dt("wxbd", [NL, 3, 128, 128])
    cst_d = dt("cst", [128, 6 * 128])
    kind_d = dt("kind", [8, S_LEN])
    rot_d = dt("rot", [4, 4, 64, S_LEN])
    out_d = dt("out", [S_LEN, D], kind="ExternalOutput")
    xsp_d = dt("xsp", [128, 8, S_LEN], kind="Internal")
    dbg_d = {}
    for name, shape in dbg:
        dbg_d[name] = dt("dbg_" + name, list(shape), kind="ExternalOutput")

    es = ExitStack()
    sb = lambda name, shape: es.enter_context(nc.sbuf_tensor("sb_" + name, shape, F32))
    bufA = sb("bufA", [128, 8, S_LEN])
    bufB = sb("bufB", [128, 8, S_LEN])
    cst = sb("cst", [128, 6 * 128])
    sp = sb("sp", [128, SP_N])
    gsp = sb("gsp", [128, 16])
    rbt = sb("rbt", [128, NE])
    P = [sb(f"P{i}", [128, 2052]) for i in range(5)]
    St = [sb(f"S{i}", [128, 512]) for i in range(8)]
    wbuf = [sb(f"wb{i}", [128, 8, 128]) for i in range(2)]
    wo = sb("wo", [128, D])
    Gt = sb("G", [128, 16, NE])
    mv = sb("mv", [128, 64])
    fz = sb("fz", [128, 8])
    ps = [es.enter_context(nc.psum_tensor(f"ps{i}", [128, 512], F32)) for i in range(8)]
    sems = {}
    for e in ENGS[1:]:
        sems[e] = es.enter_context(nc.semaphore("sem_" + e))
    for i in range(NDMA):
        sems[("dma", i)] = es.enter_context(nc.semaphore(f"sem_dma{i}"))

    ident = cst[:, 0:128]
    ones = cst[:, 128:256]
    avg64 = cst[:, 256:384]
    tri = cst[:, 640:768]

    rot_i = [0]
    acc_i = [0]

    def ps_rot():
        i = rot_i[0] % 4
        rot_i[0] += 1
        return i

    def ps_acc():
        i = 4 + acc_i[0] % 4
        acc_i[0] += 1
        return i

    def dma(out, in_, R, W, eng="sync"):
        S.dma(eng, lambda e: e.dma_start(out=out, in_=in_), R, W)

    def mm(out, lhsT, rhs, start, stop, R, W, r32=False):
        if r32:
            lhsT, rhs = lhsT.bitcast(F32R), rhs.bitcast(F32R)
        S.op("tensor", lambda e: e.matmul(out, lhsT, rhs, start=start, stop=stop), R, W)

    def tp(out, in_, idn, R, W):
        S.op("tensor", lambda e: e.transpose(out, in_, idn), R, W)

    def act(out, in_, func, R, W, bias=None, scale=None):
        kw = {}
        if bias is not None:
            kw["bias"] = bias
        if scale is not None:
            kw["scale"] = scale
        S.op("scalar", lambda e: e.activation(out, in_, func, **kw), R, W)

    def tt(eng, out, in0, in1, op, R, W):
        S.op(eng, lambda e: e.tensor_tensor(out, in0, in1, op), R, W)

    def ts(eng, out, in0, s1, s2, op0, op1, R, W):
        if op1 is None:
            S.op(eng, lambda e: e.tensor_scalar(out, in0, s1, None, op0), R, W)
        else:
            S.op(eng, lambda e: e.tensor_scalar(out, in0, s1, s2, op0, op1), R, W)

    def stt(out, in0, sc, in1, op0, op1, R, W):
        S.op("vector", lambda e: e.scalar_tensor_tensor(out, in0, sc, in1, op0, op1), R, W)

    def cp(eng, out, in_, R, W):
        if eng == "scalar":
            S.op(eng, lambda e: e.activation(out, in_, AF.Identity), R, W)
        else:
            S.op(eng, lambda e: e.tensor_copy(out, in_), R, W)

    def recip(out, in_, R, W):
        S.op("vector", lambda e: e.reciprocal(out, in_), R, W)

    def memset(eng, ap, val, W):
        S.op(eng, lambda e: e.memset(ap, val), (), W)

    def dump(name, src, R):
        if name in dbg_d:
            dma(dbg_d[name], src, R, [("dbg", name)])

    dma(cst[:], cst_d, [], ["cst"])
    dma(gsp[:], gsp_d, [], ["gsp"])

    bufs = {"A": bufA, "B": bufB}

    def bfl(name):
        return bufs[name][:, :, :].rearrange("p c t -> p (c t)")

    def hbv(name, c):
        return bfl(name)[:, 0:8192].bitcast(BF16)[:, c * 2048:(c + 1) * 2048]

    def wbb(name, i):
        return bfl(name)[:, 8192 + i * 512:8192 + (i + 1) * 512].bitcast(BF16).rearrange("p (k n) -> p k n", k=8)

    def wob(name):
        return bfl(name)[:, 9216:9728].bitcast(BF16)

    def mix_fence(name):
        S.op("gpsimd", lambda e: e.memset(fz[:, 1:2], 0.0), (),
             tl(name, range(8), 0, S_LEN) + [("wbb", 0), ("wbb", 1), "wob", "fz"])

    def load_x(dst):
        X = bufs[dst]
        for tt_ in range(16):
            stg = P[tt_ % 2]
            dma(stg[:, 0:D], x_d[tt_ * 128:(tt_ + 1) * 128, :], [], [("P", tt_ % 2)])
            for half in range(2):
                pi = ps_rot()
                for q in range(4):
                    c = half * 4 + q
                    tp(ps[pi][:, q * 128:(q + 1) * 128], stg[:, c * 128:(c + 1) * 128], ident,
                       [("P", tt_ % 2), "cst"], [("ps", pi)])
                cp("vector" if half == 0 else "scalar",
                   X[:, half * 4:half * 4 + 4, tt_ * 128:(tt_ + 1) * 128],
                   ps[pi][:, :].rearrange("p (q t) -> p q t", q=4),
                   [("ps", pi)], tl(dst, range(half * 4, half * 4 + 4), tt_ * 128, tt_ * 128 + 128))

    def rstd_compute(src, rbuf_key, rbuf):
        X = bufs[src]
        for b in range(4):
            pi = ps_acc()
            for c in range(8):
                sq = St[c % 4]
                act(sq[:, :], X[:, c, b * 512:(b + 1) * 512], AF.Square,
                    tl(src, [c], b * 512, b * 512 + 512), [("S", c % 4)])
                mm(ps[pi][:, :], ones, sq[:, :], c == 0, c == 7, [("S", c % 4), "cst"], [("ps", pi)])
            act(rbuf[:, b * 512:(b + 1) * 512], ps[pi][:, :], AF.Sqrt, [("ps", pi), "mv"], [(rbuf_key, b)],
                bias=mv[:, 63:64], scale=1.0 / D)
            recip(rbuf[:, b * 512:(b + 1) * 512], rbuf[:, b * 512:(b + 1) * 512], [(rbuf_key, b)], [(rbuf_key, b)])

    def norm_mod(src, dst, rbuf_key, rbuf, acol, bcol, bf=False):
        X, H = bufs[src], bufs[dst]
        for c in range(8):
            tmp_i = 1 + c % 2
            tmp = P[tmp_i]
            tt("vector", tmp[:, 0:S_LEN], X[:, c, :], rbuf[:, 0:S_LEN], ALU.mult,
               tl(src, [c], 0, S_LEN) + bk(rbuf_key), bk(("P", tmp_i)))
            act(hbv(dst, c) if bf else H[:, c, :], tmp[:, 0:S_LEN], AF.Identity, bk(("P", tmp_i)) + ["mv"], tl(dst, [c], 0, S_LEN),
                bias=mv[:, bcol + c:bcol + c + 1], scale=mv[:, acol + c:acol + c + 1])

    memset("vector", mv[:, :], 0.0, ["mv"])
    memset("vector", mv[:, 63:64], EPS, ["mv"])
    for i_ in range(5):
        memset("gpsimd" if i_ % 2 else "vector", P[i_][:, :], 0.0, bk(("P", i_)))
    for i_ in range(8):
        memset("gpsimd" if i_ % 2 else "vector", St[i_][:, :], 0.0, [("S", i_)])

    def layer_prologue(l):
        dma(sp[:], sp_d[l], [], ["sp"])
        dma(rbt[:], rbt_d[l], [], ["rbt"])
        cact = St[7]
        act(cact[:, 0:8], gsp[:, 0:8], AF.Silu, ["gsp"], [("S", 7)])
        pm = ps_acc()
        aw = adaw_d[l].rearrange("(kc p) n -> p kc n", p=128)
        for blk in range(24):
            wtile = P[3 + blk % 2]
            wv = wtile[:, 0:2048].rearrange("p (k n) -> p k n", k=8)
            dma(wv, aw[:, :, blk * 256:(blk + 1) * 256], [], bk(("P", 3 + blk % 2)))
            for jj in range(2):
                j = blk * 2 + jj
                for kc in range(8):
                    mm(ps[pm][:, j:j + 1], wv[:, kc, jj * 128:(jj + 1) * 128], cact[:, kc:kc + 1], kc == 0, kc == 7,
                       bk(("P", 3 + blk % 2)) + [("S", 7)], [("ps", pm)], r32=False)
        modt = St[6]
        tt("vector", modt[:, 0:48], ps[pm][:, 0:48], sp[:, SP_ADAB:SP_ADAB + 48], ALU.add, [("ps", pm), "sp"], [("S", 6)])
        ts("vector", mv[:, 0:8], modt[:, 8:16], 1.0, None, ALU.add, None, [("S", 6)], ["mv"])
        tt("vector", mv[:, 0:8], mv[:, 0:8], sp[:, SP_NW1:SP_NW1 + 8], ALU.mult, ["mv", "sp"], ["mv"])
        cp("vector", mv[:, 8:16], modt[:, 0:8], [("S", 6)], ["mv"])
        cp("vector", mv[:, 16:24], modt[:, 16:24], [("S", 6)], ["mv"])
        ts("vector", mv[:, 24:32], modt[:, 32:40], 1.0, None, ALU.add, None, [("S", 6)], ["mv"])
        tt("vector", mv[:, 24:32], mv[:, 24:32], sp[:, SP_NW2:SP_NW2 + 8], ALU.mult, ["mv", "sp"], ["mv"])
        cp("vector", mv[:, 32:40], modt[:, 24:32], [("S", 6)], ["mv"])
        cp("vector", mv[:, 40:48], modt[:, 40:48], [("S", 6)], ["mv"])
        act(mv[:, 54:57], sp[:, SP_LAM:SP_LAM + 3], AF.Exp, ["sp"], ["mv"], scale=-1.0)
        act(mv[:, 54:57], mv[:, 54:57], AF.Ln, ["mv"], ["mv"], bias=1.0)
        ts("vector", mv[:, 48:51], mv[:, 54:57], -8.0, None, ALU.mult, None, ["mv"], ["mv"])
        ts("vector", mv[:, 51:54], mv[:, 54:57], -16.0, None, ALU.mult, None, ["mv"], ["mv"])
        if l == 0:
            dump("mod0", modt[:, 0:48], [("S", 6)])

    wb_i = [0]

    def load_wcols(l, col_pieces, hsrc):
        i = wb_i[0] % 2
        wb_i[0] += 1
        wv = win_d[l].rearrange("(kc p) n -> p kc n", p=128)
        o = 0
        for c0, n in col_pieces:
            dma(wbuf[i][:, :, o:o + n], wv[:, :, c0:c0 + n], [], [("wb", i)])
            o += n
        cp("gpsimd", wbb(hsrc, i)[:, :, 0:o], wbuf[i][:, :, 0:o], [("wb", i)], [("wbb", i)])
        return i, o

    def proj_fm(l, hsrc, col_pieces, evac):
        i, M = load_wcols(l, col_pieces, hsrc)
        for b in range(4):
            pi = ps_rot()
            for kc in range(8):
                mm(ps[pi][0:M, :], wbb(hsrc, i)[:, kc, 0:M], hbv(hsrc, kc)[:, b * 512:(b + 1) * 512], kc == 0, kc == 7,
                   [("wbb", i)] + tl(hsrc, [kc], b * 512, b * 512 + 512), [("ps", pi)])
            evac(b, pi, M)

    def proj_tm(l, hsrc, col0, n, evac):
        i, M = load_wcols(l, [(col0, n)], hsrc)
        for t in range(16):
            pi = ps_rot()
            for kc in range(8):
                mm(ps[pi][:, 0:n], hbv(hsrc, kc)[:, t * 128:(t + 1) * 128], wbb(hsrc, i)[:, kc, 0:n], kc == 0, kc == 7,
                   [("wbb", i)] + tl(hsrc, [kc], t * 128, t * 128 + 128), [("ps", pi)])
            evac(t, pi)

    def wout_partial(l, xdst, row0, nrows, ysrc_ap, ykeys_fn, hsrc):
        X = bufs[xdst]
        dma(wo[0:nrows, :], wout_d[l, row0:row0 + nrows, :], [], ["wo"])
        cp("gpsimd", wob(hsrc)[0:nrows, :], wo[0:nrows, :], ["wo"], ["wob"])
        for b in range(4):
            for dc in range(8):
                pi = ps_rot()
                mm(ps[pi][:, :], wob(hsrc)[0:nrows, dc * 128:(dc + 1) * 128], ysrc_ap(b), True, True,
                   ["wob"] + ykeys_fn(b), [("ps", pi)])
                k = tl(xdst, [dc], b * 512, b * 512 + 512)
                stt(X[:, dc, b * 512:(b + 1) * 512], ps[pi][:, :], mv[:, 16 + dc:17 + dc], X[:, dc, b * 512:(b + 1) * 512],
                    ALU.mult, ALU.add, [("ps", pi), "mv"] + k, k)

    def retention_head(l, h, xsrc, hsrc):
        qb_, kb_, vb_, gb_ = P[0], P[1], P[2], P[3]
        c0 = h * 64

        def rot_evac(dst, dkey, tab):
            store = {}

            def ev_a(b, pi, M):
                store[b] = pi
            return store, ev_a

        for (dst, dkey, base, tab) in ((qb_, ("P", 0), 0, 0), (kb_, ("P", 1), 256, 2)):
            ia, _ = load_wcols(l, [(base + c0, 64)], hsrc)
            ib_, _ = load_wcols(l, [(base + c0 + 32, 32), (base + c0, 32)], hsrc)
            for b in range(4):
                dma(St[0][0:64, :], rot_d[tab, h, :, b * 512:(b + 1) * 512], [], [("S", 0)])
                dma(St[1][0:64, :], rot_d[tab + 1, h, :, b * 512:(b + 1) * 512], [], [("S", 1)])
                pa, pb = ps_rot(), ps_rot()
                for (pi, wi) in ((pa, ia), (pb, ib_)):
                    for kc in range(8):
                        mm(ps[pi][0:64, :], wbb(hsrc, wi)[:, kc, 0:64], hbv(hsrc, kc)[:, b * 512:(b + 1) * 512], kc == 0, kc == 7,
                           [("wbb", wi)] + tl(hsrc, [kc], b * 512, b * 512 + 512), [("ps", pi)])
                tt("vector", St[2][0:64, :], ps[pa][0:64, :], St[0][0:64, :], ALU.mult, [("ps", pa), ("S", 0)], [("S", 2)])
                tt("vector", St[3][0:64, :], ps[pb][0:64, :], St[1][0:64, :], ALU.mult, [("ps", pb), ("S", 1)], [("S", 3)])
                tt("gpsimd", dst[0:64, b * 512:(b + 1) * 512], St[2][0:64, :], St[3][0:64, :], ALU.add,
                   [("S", 2), ("S", 3)], [(dkey, b)])
        vv = vb_[:, 0:1024].rearrange("p (t e) -> p t e", t=16)

        def ev_v(t, pi):
            cp("scalar", vv[:, t, :], ps[pi][:, 0:64], [("ps", pi)], [(("P", 2), t // 4)])
        proj_tm(l, hsrc, 512 + c0, 64, ev_v)

        def ev_g(b, pi, M):
            act(gb_[0:64, b * 512:(b + 1) * 512], ps[pi][0:64, :], AF.Silu, [("ps", pi)], [(("P", 3), b)])
        proj_fm(l, hsrc, [(768 + c0, 64)], ev_g)
        if l == 0 and h == 0:
            dump("qrot0", qb_[0:64, 0:S_LEN], bk(("P", 0)))
            dump("krot0", kb_[0:64, 0:S_LEN], bk(("P", 1)))
        for ib in range(4):
            po = ps_acc()
            njt = ib * 4 + 4
            for jt in range(njt):
                d = jt - 4 * ib
                c_lo = max(d, 0) * 128
                pi = ps_rot()
                mm(ps[pi][:, c_lo:512], kb_[0:64, jt * 128:(jt + 1) * 128], qb_[0:64, ib * 512 + c_lo:(ib + 1) * 512], True, True,
                   [(("P", 1), jt // 4), (("P", 0), ib)], [("ps", pi)])
                si = 4 + jt % 2
                sT = St[si]
                if d >= 0:
                    tt("vector", sT[:, c_lo:c_lo + 128], ps[pi][:, c_lo:c_lo + 128], tri, ALU.mult, [("ps", pi), "cst"], [("S", si)])
                    if c_lo + 128 < 512:
                        cp("scalar", sT[:, c_lo + 128:512], ps[pi][:, c_lo + 128:512], [("ps", pi)], [("S", si)])
                else:
                    cp("scalar" if jt % 2 else "vector", sT[:, :], ps[pi][:, :], [("ps", pi)], [("S", si)])
                mm(ps[po][0:64, c_lo:512], vv[:, jt, :], sT[:, c_lo:512], jt == 0, jt == njt - 1,
                   [(("P", 2), jt // 4), ("S", si)], [("ps", po)])
            y = St[6]
            cp("vector", y[0:64, :], ps[po][0:64, :], [("ps", po)], [("S", 6)])
            pm = ps_rot()
            mm(ps[pm][0:64, :], avg64[0:64, 0:64], y[0:64, :], True, True, [("S", 6), "cst"], [("ps", pm)])
            yc = St[7]
            tt("vector", yc[0:64, :], y[0:64, :], ps[pm][0:64, :], ALU.subtract, [("S", 6), ("ps", pm)], [("S", 7)])
            act(y[0:64, :], yc[0:64, :], AF.Square, [("S", 7)], [("S", 6)])
            pv = ps_rot()
            mm(ps[pv][0:64, :], avg64[0:64, 0:64], y[0:64, :], True, True, [("S", 6), "cst"], [("ps", pv)])
            act(y[0:64, :], ps[pv][0:64, :], AF.Sqrt, [("ps", pv), "mv"], [("S", 6)], bias=mv[0:64, 63:64], scale=1.0)
            recip(y[0:64, :], y[0:64, :], [("S", 6)], [("S", 6)])
            tt("vector", yc[0:64, :], yc[0:64, :], y[0:64, :], ALU.mult, [("S", 6), ("S", 7)], [("S", 7)])
            stt(qb_[0:64, ib * 512:ib * 512 + 256].bitcast(BF16), yc[0:64, :], sp[0:64, SP_RNW + h:SP_RNW + h + 1],
                gb_[0:64, ib * 512:(ib + 1) * 512], ALU.mult, ALU.mult,
                [("S", 7), "sp", (("P", 3), ib)], [(("P", 0), ib)])
        if l == 0 and h == 0:
            dump("yret0", qb_[0:64, 0:S_LEN], bk(("P", 0)))
        wout_partial(l, xsrc, c0, 64, lambda b: qb_[0:64, b * 512:b * 512 + 256].bitcast(BF16), lambda b: [(("P", 0), b)], hsrc)

    def moba_head(l, h, xsrc, hsrc):
        qa_, ka_, vb_, bp_ = P[0], P[1], P[2], P[3]
        c0 = h * 64
        dma(ka_[64:72, 0:S_LEN], kind_d, [], bk(("P", 1)))

        def ev_q(b, pi, M):
            act(qa_[0:64, b * 512:(b + 1) * 512], ps[pi][0:64, :], AF.Identity, [("ps", pi)], [(("P", 0), b)], scale=0.125)
        proj_fm(l, hsrc, [(1024 + c0, 64)], ev_q)

        def ev_k(b, pi, M):
            cp("vector", ka_[0:64, b * 512:(b + 1) * 512], ps[pi][0:64, :], [("ps", pi)], [(("P", 1), b)])
        proj_fm(l, hsrc, [(1408 + c0, 64)], ev_k)
        vv = vb_[:, 0:2048].rearrange("p (t e) -> p t e", t=16)
        memset("gpsimd", vv[:, :, 64:128], 1.0, bk(("P", 2)))

        def ev_v(t, pi):
            cp("scalar", vv[:, t, 0:64], ps[pi][:, 0:64], [("ps", pi)], [(("P", 2), t // 4)])
        proj_tm(l, hsrc, 1792 + c0, 64, ev_v)
        km = St[6]
        S.op("vector", lambda e: e.tensor_reduce(km[0:64, 0:8], ka_[0:64, 0:S_LEN].rearrange("p (n k) -> p n k", n=8), AX.X, ALU.add),
             bk(("P", 1)), [("S", 6)])
        bpv = bp_[:, 0:16 * 72].rearrange("p (t c) -> p t c", t=16)
        memset("gpsimd", bpv[:, :, 0:64], 0.0, bk(("P", 3)))
        memset("gpsimd", bpv[:, :, 64:72], NEG, bk(("P", 3)))
        gsb = St[7]
        for t in range(16):
            own = t // 2
            if own <= 3:
                if own > 0:
                    memset("gpsimd", bpv[:, t, 64:64 + own], 0.0, bk(("P", 3)))
            else:
                pi = ps_rot()
                mm(ps[pi][:, 0:8], qa_[0:64, t * 128:(t + 1) * 128], km[0:64, 0:8], True, True,
                   [(("P", 0), t // 4), ("S", 6)], [("ps", pi)], r32=False)
                g8 = gsb[:, t * 16:t * 16 + 8]
                m8 = gsb[:, t * 16 + 8:t * 16 + 16]
                memset("vector", g8, -1e30, [("S", 7)])
                cp("vector", gsb[:, t * 16:t * 16 + own], ps[pi][:, 0:own], [("ps", pi)], [("S", 7)])
                S.op("vector", lambda e, m8=m8, g8=g8: e.max(m8, g8), [("S", 7)], [("S", 7)])
                ts("vector", g8[:, 0:own], g8[:, 0:own], m8[:, 2:3], None, ALU.is_ge, None, [("S", 7)], [("S", 7)])
                ts("vector", bpv[:, t, 64:64 + own], g8[:, 0:own], -NEG, NEG, ALU.mult, ALU.add, [("S", 7)], bk(("P", 3)))
            memset("gpsimd", bpv[:, t, 64 + own:65 + own], 0.0, bk(("P", 3)))
            pi = ps_rot()
            mm(ps[pi][0:72, 0:128], bpv[:, t, :], ident, True, True, bk(("P", 3)) + ["cst"], [("ps", pi)])
            cp("vector", qa_[64:72, t * 128:(t + 1) * 128], ps[pi][64:72, 0:128], [("ps", pi)], [(("P", 0), t // 4)])
        if l == 0 and h == 0:
            dump("mqaug0", qa_[0:72, 0:S_LEN], bk(("P", 0)))
        for ib in range(4):
            po = ps_acc()
            njt = ib * 4 + 4
            for jt in range(njt):
                d = jt - 4 * ib
                c_lo = max(d, 0) * 128
                pi = ps_rot()
                mm(ps[pi][:, c_lo:512], ka_[0:72, jt * 128:(jt + 1) * 128], qa_[0:72, ib * 512 + c_lo:(ib + 1) * 512], True, True,
                   [(("P", 1), jt // 4), (("P", 0), ib)], [("ps", pi)])
                si = 4 + jt % 2
                eT = St[si]
                act(eT[:, c_lo:512], ps[pi][:, c_lo:512], AF.Exp, [("ps", pi)], [("S", si)])
                if d >= 0:
                    tt("gpsimd", eT[:, c_lo:c_lo + 128], eT[:, c_lo:c_lo + 128], tri, ALU.mult, [("S", si), "cst"], [("S", si)])
                mm(ps[po][:, c_lo:512], vv[:, jt, :], eT[:, c_lo:512], jt == 0, jt == njt - 1,
                   [(("P", 2), jt // 4), ("S", si)], [("ps", po)])
            rd = St[6]
            memset("gpsimd", rd[0:64, :], 0.0, [("S", 6)])
            recip(rd[64:128, :], ps[po][64:128, :], [("ps", po)], [("S", 6)])
            pm = ps_rot()
            mm(ps[pm][0:64, :], ident[:, 64:128], rd[:, :], True, True, [("S", 6), "cst"], [("ps", pm)], r32=False)
            yn = St[7]
            cp("scalar", yn[0:64, :], ps[pm][0:64, :], [("ps", pm)], [("S", 7)])
            tt("vector", qa_[0:64, ib * 512:ib * 512 + 256].bitcast(BF16), ps[po][0:64, :], yn[0:64, :], ALU.mult,
               [("ps", po), ("S", 7)], [(("P", 0), ib)])
        if l == 0 and h == 0:
            dump("ymoba0", qa_[0:64, 0:S_LEN], bk(("P", 0)))
        wout_partial(l, xsrc, 256 + c0, 64, lambda b: qa_[0:64, b * 512:b * 512 + 256].bitcast(BF16), lambda b: [(("P", 0), b)], hsrc)

    def lru_chunk(l, i, xsrc, hsrc):
        lx, ub, rb_, ib_, gb_ = P[0], P[1], P[2], P[3], P[4]
        c0 = i * 128
        memset("gpsimd", lx[:, 0:3], 0.0, [(("P", 0), 0)])

        def ev_x(b, pi, M):
            cp("vector", lx[:, 3 + b * 512:3 + (b + 1) * 512], ps[pi][:, :], [("ps", pi)], [(("P", 0), b), (("P", 0), min(b + 1, 3))])
        proj_fm(l, hsrc, [(2176 + c0, 128)], ev_x)
        allx = bk(("P", 0))
        cw = lambda j: sp[:, SP_CW + j * 3 + i:SP_CW + j * 3 + i + 1]
        ts("vector", ub[:, 0:S_LEN], lx[:, 0:S_LEN], cw(0), sp[:, SP_CB + i:SP_CB + i + 1], ALU.mult, ALU.add,
           allx + ["sp"], bk(("P", 1)))
        for j in range(1, 4):
            stt(ub[:, 0:S_LEN], lx[:, j:j + S_LEN], cw(j), ub[:, 0:S_LEN], ALU.mult, ALU.add, allx + ["sp"] + bk(("P", 1)), bk(("P", 1)))
        if stop == "lruA":
            dump("ylru0", ub[:, 0:S_LEN], bk(("P", 1)))
            return
        dma(wo[:, 0:128], wabd_d[l, i], [], ["wo"])
        dma(wo[:, 128:256], wxbd_d[l, i], [], ["wo"])
        for b in range(4):
            pa, px = ps_rot(), ps_rot()
            mm(ps[pa][:, :], wo[:, 0:128], ub[:, b * 512:(b + 1) * 512], True, True, ["wo", (("P", 1), b)], [("ps", pa)])
            mm(ps[px][:, :], wo[:, 128:256], ub[:, b * 512:(b + 1) * 512], True, True, ["wo", (("P", 1), b)], [("ps", px)])
            act(rb_[:, b * 512:(b + 1) * 512], ps[pa][:, :], AF.Sigmoid, [("ps", pa), "sp"], [(("P", 2), b)],
                bias=sp[:, SP_BA + i:SP_BA + i + 1], scale=1.0)
            act(ib_[:, b * 512:(b + 1) * 512], ps[px][:, :], AF.Sigmoid, [("ps", px), "sp"], [(("P", 3), b)],
                bias=sp[:, SP_BX + i:SP_BX + i + 1], scale=1.0)
        if stop == "lruB":
            dump("ylru0", rb_[:, 0:S_LEN], bk(("P", 2)))
            return
        tt("gpsimd", ib_[:, 0:S_LEN], ib_[:, 0:S_LEN], ub[:, 0:S_LEN], ALU.mult, bk(("P", 3)) + bk(("P", 1)), bk(("P", 3)))
        act(ub[:, 0:S_LEN], rb_[:, 0:S_LEN], AF.Exp, bk(("P", 2)) + ["mv"], bk(("P", 1)), scale=mv[:, 51 + i:52 + i])
        act(ub[:, 0:S_LEN], ub[:, 0:S_LEN], AF.Sqrt, bk(("P", 1)), bk(("P", 1)), bias=1.0, scale=-1.0)
        tt("gpsimd", ib_[:, 0:S_LEN], ib_[:, 0:S_LEN], ub[:, 0:S_LEN], ALU.mult, bk(("P", 3)) + bk(("P", 1)), bk(("P", 3)))
        act(rb_[:, 0:S_LEN], rb_[:, 0:S_LEN], AF.Exp, bk(("P", 2)) + ["mv"], bk(("P", 2)), scale=mv[:, 48 + i:49 + i])
        S.op("vector", lambda e: e.tensor_tensor_scan(ub[:, 0:S_LEN], rb_[:, 0:S_LEN], ib_[:, 0:S_LEN], 0.0, ALU.mult, ALU.add),
             bk(("P", 2)) + bk(("P", 3)), bk(("P", 1)))
        if stop == "lruC":
            dump("ylru0", ub[:, 0:S_LEN], bk(("P", 1)))
            return

        def ev_g(b, pi, M):
            sl = slice(b * 512, (b + 1) * 512)
            cp("vector", gb_[:, sl], ps[pi][:, :], [("ps", pi)], [(("P", 4), b)])
            act(rb_[:, sl], gb_[:, sl], AF.Square, [(("P", 4), b)], [(("P", 2), b)])
        proj_fm(l, hsrc, [(2560 + c0, 128)], ev_g)
        if stop == "lruD":
            dump("ylru0", gb_[:, 0:S_LEN], bk(("P", 4)))
            return
        ts("vector", rb_[:, 0:S_LEN], rb_[:, 0:S_LEN], 0.044715, 1.0, ALU.mult, ALU.add, bk(("P", 2)), bk(("P", 2)))
        tt("gpsimd", rb_[:, 0:S_LEN], rb_[:, 0:S_LEN], gb_[:, 0:S_LEN], ALU.mult, bk(("P", 2)) + bk(("P", 4)), bk(("P", 2)))
        act(rb_[:, 0:S_LEN], rb_[:, 0:S_LEN], AF.Sigmoid, bk(("P", 2)), bk(("P", 2)), scale=1.5957691216057308)
        tt("vector", gb_[:, 0:S_LEN], gb_[:, 0:S_LEN], rb_[:, 0:S_LEN], ALU.mult, bk(("P", 2)) + bk(("P", 4)), bk(("P", 4)))
        ylb = rb_[:, 0:1024].bitcast(BF16)
        tt("vector", ylb, gb_[:, 0:S_LEN], ub[:, 0:S_LEN], ALU.mult, bk(("P", 1)) + bk(("P", 4)) + bk(("P", 2)), bk(("P", 2)))
        if l == 0 and i == 0:
            dump("ylru0", gb_[:, 0:S_LEN], bk(("P", 4)))
        if stop == "lruE":
            return
        wout_partial(l, xsrc, 640 + c0, 128, lambda b: ylb[:, b * 512:(b + 1) * 512], lambda b: bk(("P", 2)), hsrc)

    def fence(keys_wait, keys_new):
        S.op("gpsimd", lambda e: e.memset(fz[:, 0:1], 0.0), (), list(keys_wait) + list(keys_new) + ["fz"])

    def moe(l, xsrc, hsrc):
        H = bufs[hsrc]
        acc = bufs[xsrc][:, :, :].rearrange("p c t -> p (c t)").rearrange("p (t d) -> p t d", t=16)
        ak = lambda t: [(xsrc, t // 2, 8 * (t % 2) + k_) for k_ in range(8)]
        hb = lambda c: P[c // 2][:, 0:2048].bitcast(BF16)[:, (c % 2) * 2048:(c % 2 + 1) * 2048]
        hbk = lambda c: bk(("P", c // 2))
        for c in range(8):
            cp("vector" if c % 2 == 0 else "gpsimd", hb(c), H[:, c, :], tl(hsrc, [c], 0, S_LEN), hbk(c))
        rwv = wo[:, 0:256].rearrange("p (k n) -> p k n", k=8)
        dma(rwv, rw_d[l].rearrange("(kc p) n -> p kc n", p=128), [], ["wo"])
        gtT = P[4]
        for t in range(16):
            pi = ps_rot()
            for kc in range(8):
                mm(ps[pi][:, 0:NE], H[:, kc, t * 128:(t + 1) * 128], rwv[:, kc, :], kc == 0, kc == 7,
                   ["wo"] + tl(hsrc, [kc], t * 128, t * 128 + 128), [("ps", pi)])
            lg = St[0]
            tt("vector", lg[:, 0:NE], ps[pi][:, 0:NE], rbt[:, :], ALU.add, [("ps", pi), "rbt"], [("S", 0)])
            S.op("vector", lambda e, lg=lg: e.max(lg[:, 32:40], lg[:, 0:NE]), [("S", 0)], [("S", 0)])
            ts("vector", lg[:, 40:41], lg[:, 32:33], -1.0, None, ALU.mult, None, [("S", 0)], [("S", 0)])
            ts("vector", lg[:, 64:96], lg[:, 0:NE], lg[:, 35:36], None, ALU.is_ge, None, [("S", 0)], [("S", 0)])
            act(lg[:, 96:128], lg[:, 0:NE], AF.Exp, [("S", 0)], [("S", 0)], bias=lg[:, 40:41], scale=1.0)
            tt("vector", lg[:, 96:128], lg[:, 96:128], lg[:, 64:96], ALU.mult, [("S", 0)], [("S", 0)])
            S.op("vector", lambda e, lg=lg: e.tensor_reduce(lg[:, 41:42], lg[:, 96:128], AX.X, ALU.add), [("S", 0)], [("S", 0)])
            recip(lg[:, 41:42], lg[:, 41:42], [("S", 0)], [("S", 0)])
            ts("vector", Gt[:, t, :], lg[:, 96:128], lg[:, 41:42], None, ALU.mult, None, [("S", 0)], ["G"])
            pt = ps_rot()
            mm(ps[pt][0:NE, 0:128], Gt[:, t, :], ident, True, True, ["G", "cst"], [("ps", pt)])
            cp("scalar", gtT[0:NE, t * 128:(t + 1) * 128], ps[pt][0:NE, 0:128], [("ps", pt)], [(("P", 4), t // 4)])
        if l == 0:
            dump("gates0", Gt[:, :, :], ["G"])
        dma(wo[0:NE, 0:D], bdn_d[l], [], ["wo"])
        for t in range(16):
            for hf in range(2):
                pi = ps_rot()
                mm(ps[pi][:, :], gtT[0:NE, t * 128:(t + 1) * 128], wo[0:NE, hf * 512:(hf + 1) * 512], True, True,
                   [(("P", 4), t // 4), "wo"], [("ps", pi)])
                cp("vector" if hf else "scalar", acc[:, t, hf * 512:(hf + 1) * 512], ps[pi][:, :], [("ps", pi)], ak(t))
        Bf = H[:, :, :].rearrange("p c t -> p (c t)")
        actall = Bf[:, 0:8192].bitcast(BF16)
        actv = lambda tb, j: actall[:, (tb * 8 + j) * 512:(tb * 8 + j + 1) * 512]
        wst = lambda s_: Bf[:, 8192 + s_ * 2048:8192 + (s_ + 1) * 2048].rearrange("p (k n) -> p k n", k=8)
        wgb = lambda s_: Bf[:, 12288 + s_ * 1024:12288 + (s_ + 1) * 1024].bitcast(BF16).rearrange("p (k n) -> p k n", k=8)
        dstg = lambda s_: Bf[:, 14336 + s_ * 1024:14336 + (s_ + 1) * 1024]
        wdb = lambda j: St[j][:, :].bitcast(BF16)
        wbf = [wbuf[i][:, :, :].rearrange("p k n -> p (k n)") for i in range(2)]
        T = [P[4][:, k * 512:(k + 1) * 512] for k in range(4)] + [wbf[0][:, 0:512], wbf[0][:, 512:1024], wbf[1][:, 0:512], wbf[1][:, 512:1024]] + \
            [wo[:, 0:512], wo[:, 512:1024]]
        Tk = [(("P", 4), k) for k in range(4)] + [("wbh", 0, 0), ("wbh", 0, 1), ("wbh", 1, 0), ("wbh", 1, 1), ("woh", 0), ("woh", 1)]
        ovk = [("ov", "act", tb, j) for tb in range(4) for j in range(8)] + \
              [("ov", n_, s_) for n_ in ("wst", "wgb", "dst") for s_ in range(2)] + Tk[4:]
        oldk = tl(hsrc, range(8), 0, S_LEN) + [("wb", 0), ("wb", 1), "wo"]
        PGU = [(4, 5), (6, 7), (2, 3)]
        it_ = [0]
        fence(oldk, ovk)

        pieces = [(e_, j) for e_ in range(NE) for j in range(8)]

        def load_piece(p):
            e_, j = pieces[p]
            s_ = p % 2
            wg = wgu_d[l, e_].rearrange("(kc p) n -> p kc n", p=128)
            dma(wst(s_)[:, :, 0:128], wg[:, :, j * 128:(j + 1) * 128], [], [("ov", "wst", s_)])
            dma(wst(s_)[:, :, 128:256], wg[:, :, D + j * 128:D + (j + 1) * 128], [], [("ov", "wst", s_)])
            dma(dstg(s_), wdn_d[l, e_, j * 128:(j + 1) * 128, :], [], [("ov", "dst", s_)])
            act(wgb(s_), wst(s_), AF.Identity, [("ov", "wst", s_)], [("ov", "wgb", s_)])

        load_piece(0)
        for p, (e_, j) in enumerate(pieces):
            s_ = p % 2
            if p + 1 < len(pieces):
                load_piece(p + 1)
            cp("gpsimd", wdb(j), dstg(s_), [("ov", "dst", s_)], [("S", j)])
            bg = sp[:, SP_BGU + e_ * 16 + j:SP_BGU + e_ * 16 + j + 1]
            bu = sp[:, SP_BGU + e_ * 16 + 8 + j:SP_BGU + e_ * 16 + 8 + j + 1]
            for tb in range(4):
                pg, pu = PGU[it_[0] % 3]
                k3 = (it_[0] % 3) * 3
                it_[0] += 1
                for (pi, o) in ((pg, 0), (pu, 128)):
                    for kc in range(8):
                        mm(ps[pi][:, :], wgb(s_)[:, kc, o:o + 128], hb(kc)[:, tb * 512:(tb + 1) * 512], kc == 0, kc == 7,
                           [("ov", "wgb", s_)] + hbk(kc), [("ps", pi)])
                g1, sg, u2 = T[k3], T[k3 + 1], T[k3 + 2]
                k0, k1, k2 = Tk[k3], Tk[k3 + 1], Tk[k3 + 2]
                ts("vector", g1, ps[pg][:, :], bg, 7.0, ALU.add, ALU.min, [("ps", pg), "sp"], [k0])
                act(sg, g1, AF.Sigmoid, [k0], [k1], scale=1.702)
                ts("vector", u2, ps[pu][:, :], bu, 7.0, ALU.add, ALU.min, [("ps", pu), "sp"], [k2])
                ts("vector", u2, u2, -7.0, 1.0, ALU.max, ALU.add, [k2], [k2])
                tt("gpsimd", g1, g1, sg, ALU.mult, [k0, k1], [k0])
                tt("vector", actv(tb, j), g1, u2, ALU.mult, [k0, k2], [("ov", "act", tb, j)])
            if j == 7:
                for t in range(16):
                    tb, tq = t // 4, t % 4
                    for hf in range(2):
                        pi = hf
                        for jj in range(8):
                            mm(ps[pi][:, :], actv(tb, jj)[:, tq * 128:(tq + 1) * 128], wdb(jj)[:, hf * 512:(hf + 1) * 512],
                               jj == 0, jj == 7, [("ov", "act", tb, jj), ("S", jj)], [("ps", pi)])
                        act(T[9], ps[pi][:, :], AF.Identity, [("ps", pi), "G"], [Tk[9]], scale=Gt[:, t, e_:e_ + 1])
                        tt("gpsimd", acc[:, t, hf * 512:(hf + 1) * 512], acc[:, t, hf * 512:(hf + 1) * 512], T[9], ALU.add,
                           [Tk[9]] + ak(t), ak(t))
        fence(ovk, oldk)

    def moe_finish(l, accsrc, dst):
        acc = bufs[accsrc][:, :, :].rearrange("p c t -> p (c t)").rearrange("p (t d) -> p t d", t=16)
        ak = lambda t: [(accsrc, t // 2, 8 * (t % 2) + k_) for k_ in range(8)]
        Xn = bufs[dst]
        for t in range(16):
            xo_i = t % 2
            xo = P[xo_i][:, 0:1024].rearrange("p (c q) -> p c q", c=8)
            dma(xo, xsp_d[:, :, t * 128:(t + 1) * 128], ["xsp"], bk(("P", xo_i)))
            for half in range(2):
                pi = ps_rot()
                for q in range(4):
                    c = half * 4 + q
                    tp(ps[pi][:, q * 128:(q + 1) * 128], acc[:, t, c * 128:(c + 1) * 128], ident, ak(t) + ["cst"], [("ps", pi)])
                for q in range(4):
                    c = half * 4 + q
                    stt(Xn[:, c, t * 128:(t + 1) * 128], ps[pi][:, q * 128:(q + 1) * 128], mv[:, 40 + c:41 + c], xo[:, c, :],
                        ALU.mult, ALU.add, [("ps", pi), "mv"] + bk(("P", xo_i)), tl(dst, [c], t * 128, t * 128 + 128))

    def final_out(src, other):
        X, Y = bufs[src], bufs[other]
        rstd_compute(src, ("P", 0), P[0])
        for c in range(8):
            tmp_i = 1 + c % 2
            tt("vector", P[tmp_i][:, 0:S_LEN], X[:, c, :], P[0][:, 0:S_LEN], ALU.mult, tl(src, [c], 0, S_LEN) + bk(("P", 0)), bk(("P", tmp_i)))
            act(Y[:, c, :], P[tmp_i][:, 0:S_LEN], AF.Identity, bk(("P", tmp_i)) + ["gsp"], tl(other, [c], 0, S_LEN), scale=gsp[:, 8 + c:9 + c])
        for t in range(16):
            stg_i = 3 + t % 2
            stg = P[stg_i]
            for half in range(2):
                pi = ps_rot()
                for q in range(4):
                    c = half * 4 + q
                    tp(ps[pi][:, q * 128:(q + 1) * 128], Y[:, c, t * 128:(t + 1) * 128], ident,
                       tl(other, [c], t * 128, t * 128 + 128) + ["cst"], [("ps", pi)])
                cp("vector" if half == 0 else "scalar", stg[:, half * 512:(half + 1) * 512], ps[pi][:, :], [("ps", pi)], bk(("P", stg_i)))
            dma(out_d[t * 128:(t + 1) * 128, :], stg[:, 0:D], bk(("P", stg_i)), [("out", t)])

    cur, oth = "A", "B"
    load_x(cur)
    for l in range(L):
        layer_prologue(l)
        if stop == "mod":
            break
        rstd_compute(cur, ("P", 0), P[0])
        mix_fence(oth)
        norm_mod(cur, oth, ("P", 0), P[0], 0, 8, bf=True)
        if l == 0:
            dump("h0", bufs[oth][:, :, :], tl(oth, range(8), 0, S_LEN))
        if stop == "h":
            break
        for h in range(4):
            retention_head(l, h, cur, oth)
            if stop == "ret0":
                break
        if stop in ("ret0", "ret"):
            break
        for h in range(6):
            moba_head(l, h, cur, oth)
            if stop == "moba0":
                break
        if stop in ("moba0", "moba"):
            break
        for i in range(3):
            lru_chunk(l, i, cur, oth)
            if stop in ("lru0", "lruA", "lruB", "lruC", "lruD", "lruE"):
                break
        if l == 0:
            dump("xmix0", bufs[cur][:, :, :], tl(cur, range(8), 0, S_LEN))
        if stop in ("lru0", "mix", "lruA", "lruB", "lruC", "lruD", "lruE"):
            break
        rstd_compute(cur, ("P", 0), P[0])
        mix_fence(oth)
        norm_mod(cur, oth, ("P", 0), P[0], 24, 32)
        for c_ in range(8):
            dma(xsp_d[:, c_, :], bufs[cur][:, c_, :], tl(cur, [c_], 0, S_LEN), ["xsp"])
        moe(l, cur, oth)
        moe_finish(l, cur, oth)
        cur, oth = oth, cur
    final_out(cur, oth)
    S.wait_keys("sync", [("out", t) for t in range(16)] + [("dbg", n) for n in dbg_d])

    def replay(name, e):
        for waits, fn, tok in S.streams[name]:
            for s_, v in waits:
                e.wait_ge(sems[s_], v)
            if fn is None:
                continue
            inst = fn(e)
            if tok[0] == name:
                inst.then_inc(sems[name], 1)
            else:
                inst.then_inc(sems[tok[0]], 16)

    with nc.Block() as block:
        @block.sync
        def _(e):
            replay("sync", e)

        @block.scalar
        def _(e):
            replay("scalar", e)

        @block.vector
        def _(e):
            replay("vector", e)

        @block.gpsimd
        def _(e):
            replay("gpsimd", e)

        @block.tensor
        def _(e):
            replay("tensor", e)
    es.close()
    return nc


def _consts():
    cst = np.zeros((128, 6 * 128), np.float32)
    cst[:, 0:128] = np.eye(128, dtype=np.float32)
    cst[:, 128:256] = 1.0
    a = np.zeros((128, 128), np.float32)
    a[0:64, 0:64] = 1.0 / 64
    a[64:128, 64:128] = 1.0 / 64
    cst[:, 256:384] = a
    cst[:, 384:448] = 1.0
    cst[:, 576:640] = 1.0
    p = np.arange(128)[:, None]
    c = np.arange(128)[None, :]
    cst[:, 640:768] = (c >= p).astype(np.float32)
    kind = np.zeros((8, S_LEN), np.float32)
    for n in range(8):
        kind[n, n * 256:(n + 1) * 256] = 1.0
    pos = np.arange(S_LEN, dtype=np.float64)
    half = 32
    inv = 1.0 / (10000.0 ** (np.arange(half, dtype=np.float64) / half))
    ang = pos[None, :] * inv[:, None]
    cos = np.concatenate([np.cos(ang), np.cos(ang)], 0)
    sin = np.concatenate([-np.sin(ang), np.sin(ang)], 0)
    rot = np.zeros((4, 4, 64, S_LEN), np.float64)
    for h in range(4):
        lg = np.log1p(-2.0 ** (-5.0 - h))
        dq = np.exp(lg * pos)[None, :]
        dk = np.exp(-lg * pos)[None, :] * (64 ** -0.5)
        rot[0, h] = cos * dq
        rot[1, h] = sin * dq
        rot[2, h] = cos * dk
        rot[3, h] = sin * dk
    return cst, kind, rot.astype(np.float32)


def _fm(v):
    v = np.asarray(v, np.float32)
    return np.ascontiguousarray(v.reshape(-1, 128).T)


def prepare_inputs(inp):
    cst, kind, rot = _consts()
    sp = np.zeros((NL, 128, SP_N), np.float32)
    wabd = np.zeros((NL, 3, 128, 128), np.float32)
    wxbd = np.zeros((NL, 3, 128, 128), np.float32)
    rbt = np.zeros((NL, 128, NE), np.float32)
    for l in range(NL):
        sp[l, :, SP_ADAB:SP_ADAB + 48] = _fm(inp["ada_b"][l])
        sp[l, :, SP_NW1:SP_NW1 + 8] = _fm(inp["norm_mix_w"][l])
        sp[l, :, SP_NW2:SP_NW2 + 8] = _fm(inp["norm_ffn_w"][l])
        sp[l, 0:64, SP_RNW:SP_RNW + 4] = np.asarray(inp["ret_norm_w"][l], np.float32).reshape(4, 64).T
        for j in range(4):
            sp[l, :, SP_CW + j * 3:SP_CW + j * 3 + 3] = _fm(inp["lru_conv_w"][l, j])
        sp[l, :, SP_CB:SP_CB + 3] = _fm(inp["lru_conv_b"][l])
        sp[l, :, SP_BA:SP_BA + 3] = _fm(inp["lru_gate_a_b"][l])
        sp[l, :, SP_BX:SP_BX + 3] = _fm(inp["lru_gate_x_b"][l])
        sp[l, :, SP_LAM:SP_LAM + 3] = _fm(inp["lru_lambda"][l])
        for e in range(NE):
            sp[l, :, SP_BGU + e * 16:SP_BGU + e * 16 + 16] = _fm(inp["moe_b_gu"][l, e])
        for i in range(3):
            for g in range(2):
                wabd[l, i, g * 64:(g + 1) * 64, g * 64:(g + 1) * 64] = inp["lru_gate_a_w"][l, 2 * i + g]
                wxbd[l, i, g * 64:(g + 1) * 64, g * 64:(g + 1) * 64] = inp["lru_gate_x_w"][l, 2 * i + g]
        rbt[l] = np.broadcast_to(np.asarray(inp["router_b"][l], np.float32)[None, :], (128, NE))
    shared = {
        "sp": sp, "rbt": rbt, "bdn": np.ascontiguousarray(inp["moe_b_down"], dtype=np.float32),
        "ada_w": np.ascontiguousarray(inp["ada_w"], dtype=np.float32),
        "w_in": np.ascontiguousarray(inp["w_in"], dtype=np.float32),
        "w_out": np.ascontiguousarray(inp["w_out"], dtype=np.float32),
        "router_w": np.ascontiguousarray(inp["router_w"], dtype=np.float32),
        "moe_w_gu": np.ascontiguousarray(inp["moe_w_gu"], dtype=np.float32),
        "moe_w_down": np.ascontiguousarray(inp["moe_w_down"], dtype=np.float32),
        "wabd": wabd, "wxbd": wxbd, "cst": cst, "kind": kind, "rot": rot,
    }
    maps = []
    for b in range(inp["x"].shape[0]):
        gsp = np.zeros((128, 16), np.float32)
        gsp[:, 0:8] = _fm(inp["c"][b])
        gsp[:, 8:16] = _fm(inp["final_norm_w"])
        m = dict(shared)
        m["x"] = np.ascontiguousarray(inp["x"][b], dtype=np.float32)
        m["gsp"] = gsp
        maps.append(m)
    return maps


def kernel(**inputs):
    inp = {k: np.asarray(v) for k, v in inputs.items()}
    maps = prepare_inputs(inp)
    nc = build_program()
    res = run_bass_kernel_spmd(nc, maps, core_ids=list(range(len(maps))))
    out = np.stack([np.asarray(r["out"], dtype=np.float32) for r in res.results], axis=0)
    return out
```

```python
import numpy as np
from contextlib import ExitStack
import concourse.bass as bass
import concourse.mybir as mybir
from concourse.bass_utils import run_bass_kernel_spmd

F32 = mybir.dt.float32
F32R = mybir.dt.float32r
BF16 = mybir.dt.bfloat16
AF = mybir.ActivationFunctionType
ALU = mybir.AluOpType
AX = mybir.AxisListType

D = 1024
S_LEN = 2048
NL = 2
NE = 32
IN_W = 2944
EPS = 1e-6
NEG = -32768.0
NDMA = 24
SAME_ENGINE_SYNC = True
ENGS = ["sync", "scalar", "vector", "gpsimd", "tensor"]

SP_ADAB = 0
SP_NW1 = 48
SP_NW2 = 56
SP_RNW = 64
SP_CW = 68
SP_CB = 80
SP_BA = 83
SP_BX = 86
SP_LAM = 89
SP_BGU = 92
SP_N = 92 + 512


class Sched:
    def __init__(self):
        self.streams = {e: [] for e in ENGS}
        self.cnt = {e: 0 for e in ENGS}
        self.known = {e: {} for e in ENGS}
        self.lastw = {}
        self.rd = {}
        self.ndma = 0
        self.dma_val = [0] * NDMA

    def _deps(self, reads, writes):
        toks = []
        for k in reads:
            t = self.lastw.get(k)
            if t is not None:
                toks.append(t)
        for k in writes:
            t = self.lastw.get(k)
            if t is not None:
                toks.append(t)
            for s, v in self.rd.get(k, {}).items():
                toks.append((s, v))
        return toks

    def _commit(self, tok, reads, writes):
        s, v = tok
        for k in reads:
            d = self.rd.setdefault(k, {})
            if d.get(s, 0) < v:
                d[s] = v
        for k in writes:
            self.lastw[k] = tok
            self.rd[k] = {}

    def _filter(self, eng, toks):
        best = {}
        for s, v in toks:
            if s == eng and (eng == "tensor" or not SAME_ENGINE_SYNC):
                continue
            if v > best.get(s, 0):
                best[s] = v
        out = []
        kn = self.known[eng]
        for s, v in best.items():
            if kn.get(s, 0) >= v:
                continue
            kn[s] = v
            out.append((s, v))
        return out

    def op(self, eng, fn, reads=(), writes=()):
        toks = self._deps(reads, writes)
        waits = self._filter(eng, toks)
        self.cnt[eng] += 1
        tok = (eng, self.cnt[eng])
        self.streams[eng].append((waits, fn, tok))
        self._commit(tok, reads, writes)

    def dma(self, eng, fn, reads=(), writes=()):
        toks = self._deps(reads, writes)
        i = self.ndma % NDMA
        self.ndma += 1
        if self.dma_val[i] > 0:
            toks.append((("dma", i), self.dma_val[i]))
        self.dma_val[i] += 16
        tok = (("dma", i), self.dma_val[i])
        waits = self._filter(eng, toks)
        self.streams[eng].append((waits, fn, tok))
        self._commit(tok, reads, writes)

    def wait_keys(self, eng, keys):
        toks = self._deps(keys, keys)
        waits = self._filter(eng, toks)
        self.streams[eng].append((waits, None, None))


def tl(name, cs, t0, t1):
    return [(name, c, t) for c in cs for t in range(t0 // 128, (t1 + 127) // 128)]


def bk(name, t0=0, t1=S_LEN):
    return [(name, b) for b in range(t0 // 512, (t1 + 511) // 512)]


def build_program(L=NL, dbg=(), stop=None):
    nc = bass.Bass("TRN2", target_bir_lowering=False)
    S = Sched()
    dt = lambda name, shape, kind="ExternalInput": nc.dram_tensor(name, shape, F32, kind=kind).ap()
    x_d = dt("x", [S_LEN, D])
    sp_d = dt("sp", [NL, 128, SP_N])
    gsp_d = dt("gsp", [128, 16])
    rbt_d = dt("rbt", [NL, 128, NE])
    bdn_d = dt("bdn", [NL, NE, D])
    adaw_d = dt("ada_w", [NL, D, 6 * D])
    win_d = dt("w_in", [NL, D, IN_W])
    wout_d = dt("w_out", [NL, D, D])
    rw_d = dt("router_w", [NL, D, NE])
    if stop is None:
        wgu_d = dt("moe_w_gu", [NL, NE, D, 2 * D])
        wdn_d = dt("moe_w_down", [NL, NE, D, D])
    wabd_d = dt("wabd", [NL, 3, 128, 128])
    wxbd_d = dt("wxbd", [NL, 3, 128, 128])
    cst_d = dt("cst", [128, 6 * 128])
    kind_d = dt("kind", [8, S_LEN])
    rot_d = dt("rot", [4, 4, 64, S_LEN])
    out_d = dt("out", [S_LEN, D], kind="ExternalOutput")
    xsp_d = dt("xsp", [128, 8, S_LEN], kind="Internal")
    dbg_d = {}
    for name, shape in dbg:
        dbg_d[name] = dt("dbg_" + name, list(shape), kind="ExternalOutput")

    es = ExitStack()
    sb = lambda name, shape: es.enter_context(nc.sbuf_tensor("sb_" + name, shape, F32))
    bufA = sb("bufA", [128, 8, S_LEN])
    bufB = sb("bufB", [128, 8, S_LEN])
    cst = sb("cst", [128, 6 * 128])
    sp = sb("sp", [128, SP_N])
    gsp = sb("gsp", [128, 16])
    rbt = sb("rbt", [128, NE])
    P = [sb(f"P{i}", [128, 2052]) for i in range(5)]
    St = [sb(f"S{i}", [128, 512]) for i in range(8)]
    wbuf = [sb(f"wb{i}", [128, 8, 128]) for i in range(2)]
    wo = sb("wo", [128, D])
    Gt = sb("G", [128, 16, NE])
    mv = sb("mv", [128, 64])
    fz = sb("fz", [128, 8])
    ps = [es.enter_context(nc.psum_tensor(f"ps{i}", [128, 512], F32)) for i in range(8)]
    sems = {}
    for e in ENGS[1:]:
        sems[e] = es.enter_context(nc.semaphore("sem_" + e))
    for i in range(NDMA):
        sems[("dma", i)] = es.enter_context(nc.semaphore(f"sem_dma{i}"))

    ident = cst[:, 0:128]
    ones = cst[:, 128:256]
    avg64 = cst[:, 256:384]
    tri = cst[:, 640:768]

    rot_i = [0]
    acc_i = [0]

    def ps_rot():
        i = rot_i[0] % 4
        rot_i[0] += 1
        return i

    def ps_acc():
        i = 4 + acc_i[0] % 4
        acc_i[0] += 1
        return i

    def dma(out, in_, R, W, eng="sync"):
        S.dma(eng, lambda e: e.dma_start(out=out, in_=in_), R, W)

    def mm(out, lhsT, rhs, start, stop, R, W, r32=False):
        if r32:
            lhsT, rhs = lhsT.bitcast(F32R), rhs.bitcast(F32R)
        S.op("tensor", lambda e: e.matmul(out, lhsT, rhs, start=start, stop=stop), R, W)

    def tp(out, in_, idn, R, W):
        S.op("tensor", lambda e: e.transpose(out, in_, idn), R, W)

    def act(out, in_, func, R, W, bias=None, scale=None):
        kw = {}
        if bias is not None:
            kw["bias"] = bias
        if scale is not None:
            kw["scale"] = scale
        S.op("scalar", lambda e: e.activation(out, in_, func, **kw), R, W)

    def tt(eng, out, in0, in1, op, R, W):
        S.op(eng, lambda e: e.tensor_tensor(out, in0, in1, op), R, W)

    def ts(eng, out, in0, s1, s2, op0, op1, R, W):
        if op1 is None:
            S.op(eng, lambda e: e.tensor_scalar(out, in0, s1, None, op0), R, W)
        else:
            S.op(eng, lambda e: e.tensor_scalar(out, in0, s1, s2, op0, op1), R, W)

    def stt(out, in0, sc, in1, op0, op1, R, W):
        S.op("vector", lambda e: e.scalar_tensor_tensor(out, in0, sc, in1, op0, op1), R, W)

    def cp(eng, out, in_, R, W):
        if eng == "scalar":
            S.op(eng, lambda e: e.activation(out, in_, AF.Identity), R, W)
        else:
            S.op(eng, lambda e: e.tensor_copy(out, in_), R, W)

    def recip(out, in_, R, W):
        S.op("vector", lambda e: e.reciprocal(out, in_), R, W)

    def memset(eng, ap, val, W):
        S.op(eng, lambda e: e.memset(ap, val), (), W)

    def dump(name, src, R):
        if name in dbg_d:
            dma(dbg_d[name], src, R, [("dbg", name)])

    dma(cst[:], cst_d, [], ["cst"])
    dma(gsp[:], gsp_d, [], ["gsp"])

    bufs = {"A": bufA, "B": bufB}

    def bfl(name):
        return bufs[name][:, :, :].rearrange("p c t -> p (c t)")

    def hbv(name, c):
        return bfl(name)[:, 0:8192].bitcast(BF16)[:, c * 2048:(c + 1) * 2048]

    def wbb(name, i):
        return bfl(name)[:, 8192 + i * 512:8192 + (i + 1) * 512].bitcast(BF16).rearrange("p (k n) -> p k n", k=8)

    def wob(name):
        return bfl(name)[:, 9216:9728].bitcast(BF16)

    def mix_fence(name):
        S.op("gpsimd", lambda e: e.memset(fz[:, 1:2], 0.0), (),
             tl(name, range(8), 0, S_LEN) + [("wbb", 0), ("wbb", 1), "wob", "fz"])

    def load_x(dst):
        X = bufs[dst]
        for tt_ in range(16):
            stg = P[tt_ % 2]
            dma(stg[:, 0:D], x_d[tt_ * 128:(tt_ + 1) * 128, :], [], [("P", tt_ % 2)])
            for half in range(2):
                pi = ps_rot()
                for q in range(4):
                    c = half * 4 + q
                    tp(ps[pi][:, q * 128:(q + 1) * 128], stg[:, c * 128:(c + 1) * 128], ident,
                       [("P", tt_ % 2), "cst"], [("ps", pi)])
                cp("vector" if half == 0 else "scalar",
                   X[:, half * 4:half * 4 + 4, tt_ * 128:(tt_ + 1) * 128],
                   ps[pi][:, :].rearrange("p (q t) -> p q t", q=4),
                   [("ps", pi)], tl(dst, range(half * 4, half * 4 + 4), tt_ * 128, tt_ * 128 + 128))

    def rstd_compute(src, rbuf_key, rbuf):
        X = bufs[src]
        for b in range(4):
            pi = ps_acc()
            for c in range(8):
                sq = St[c % 4]
                act(sq[:, :], X[:, c, b * 512:(b + 1) * 512], AF.Square,
                    tl(src, [c], b * 512, b * 512 + 512), [("S", c % 4)])
                mm(ps[pi][:, :], ones, sq[:, :], c == 0, c == 7, [("S", c % 4), "cst"], [("ps", pi)])
            act(rbuf[:, b * 512:(b + 1) * 512], ps[pi][:, :], AF.Sqrt, [("ps", pi), "mv"], [(rbuf_key, b)],
                bias=mv[:, 63:64], scale=1.0 / D)
            recip(rbuf[:, b * 512:(b + 1) * 512], rbuf[:, b * 512:(b + 1) * 512], [(rbuf_key, b)], [(rbuf_key, b)])

    def norm_mod(src, dst, rbuf_key, rbuf, acol, bcol, bf=False):
        X, H = bufs[src], bufs[dst]
        for c in range(8):
            tmp_i = 1 + c % 2
            tmp = P[tmp_i]
            tt("vector", tmp[:, 0:S_LEN], X[:, c, :], rbuf[:, 0:S_LEN], ALU.mult,
               tl(src, [c], 0, S_LEN) + bk(rbuf_key), bk(("P", tmp_i)))
            act(hbv(dst, c) if bf else H[:, c, :], tmp[:, 0:S_LEN], AF.Identity, bk(("P", tmp_i)) + ["mv"], tl(dst, [c], 0, S_LEN),
                bias=mv[:, bcol + c:bcol + c + 1], scale=mv[:, acol + c:acol + c + 1])

    memset("vector", mv[:, :], 0.0, ["mv"])
    memset("vector", mv[:, 63:64], EPS, ["mv"])
    for i_ in range(5):
        memset("gpsimd" if i_ % 2 else "vector", P[i_][:, :], 0.0, bk(("P", i_)))
    for i_ in range(8):
        memset("gpsimd" if i_ % 2 else "vector", St[i_][:, :], 0.0, [("S", i_)])

    def layer_prologue(l):
        dma(sp[:], sp_d[l], [], ["sp"])
        dma(rbt[:], rbt_d[l], [], ["rbt"])
        cact = St[7]
        act(cact[:, 0:8], gsp[:, 0:8], AF.Silu, ["gsp"], [("S", 7)])
        pm = ps_acc()
        aw = adaw_d[l].rearrange("(kc p) n -> p kc n", p=128)
        for blk in range(24):
            wtile = P[3 + blk % 2]
            wv = wtile[:, 0:2048].rearrange("p (k n) -> p k n", k=8)
            dma(wv, aw[:, :, blk * 256:(blk + 1) * 256], [], bk(("P", 3 + blk % 2)))
            for jj in range(2):
                j = blk * 2 + jj
                for kc in range(8):
                    mm(ps[pm][:, j:j + 1], wv[:, kc, jj * 128:(jj + 1) * 128], cact[:, kc:kc + 1], kc == 0, kc == 7,
                       bk(("P", 3 + blk % 2)) + [("S", 7)], [("ps", pm)], r32=False)
        modt = St[6]
        tt("vector", modt[:, 0:48], ps[pm][:, 0:48], sp[:, SP_ADAB:SP_ADAB + 48], ALU.add, [("ps", pm), "sp"], [("S", 6)])
        ts("vector", mv[:, 0:8], modt[:, 8:16], 1.0, None, ALU.add, None, [("S", 6)], ["mv"])
        tt("vector", mv[:, 0:8], mv[:, 0:8], sp[:, SP_NW1:SP_NW1 + 8], ALU.mult, ["mv", "sp"], ["mv"])
        cp("vector", mv[:, 8:16], modt[:, 0:8], [("S", 6)], ["mv"])
        cp("vector", mv[:, 16:24], modt[:, 16:24], [("S", 6)], ["mv"])
        ts("vector", mv[:, 24:32], modt[:, 32:40], 1.0, None, ALU.add, None, [("S", 6)], ["mv"])
        tt("vector", mv[:, 24:32], mv[:, 24:32], sp[:, SP_NW2:SP_NW2 + 8], ALU.mult, ["mv", "sp"], ["mv"])
        cp("vector", mv[:, 32:40], modt[:, 24:32], [("S", 6)], ["mv"])
        cp("vector", mv[:, 40:48], modt[:, 40:48], [("S", 6)], ["mv"])
        act(mv[:, 54:57], sp[:, SP_LAM:SP_LAM + 3], AF.Exp, ["sp"], ["mv"], scale=-1.0)
        act(mv[:, 54:57], mv[:, 54:57], AF.Ln, ["mv"], ["mv"], bias=1.0)
        ts("vector", mv[:, 48:51], mv[:, 54:57], -8.0, None, ALU.mult, None, ["mv"], ["mv"])
        ts("vector", mv[:, 51:54], mv[:, 54:57], -16.0, None, ALU.mult, None, ["mv"], ["mv"])
        if l == 0:
            dump("mod0", modt[:, 0:48], [("S", 6)])

    wb_i = [0]

    def load_wcols(l, col_pieces, hsrc):
        i = wb_i[0] % 2
        wb_i[0] += 1
        wv = win_d[l].rearrange("(kc p) n -> p kc n", p=128)
        o = 0
        for c0, n in col_pieces:
            dma(wbuf[i][:, :, o:o + n], wv[:, :, c0:c0 + n], [], [("wb", i)])
            o += n
        cp("gpsimd", wbb(hsrc, i)[:, :, 0:o], wbuf[i][:, :, 0:o], [("wb", i)], [("wbb", i)])
        return i, o

    def proj_fm(l, hsrc, col_pieces, evac):
        i, M = load_wcols(l, col_pieces, hsrc)
        for b in range(4):
            pi = ps_rot()
            for kc in range(8):
                mm(ps[pi][0:M, :], wbb(hsrc, i)[:, kc, 0:M], hbv(hsrc, kc)[:, b * 512:(b + 1) * 512], kc == 0, kc == 7,
                   [("wbb", i)] + tl(hsrc, [kc], b * 512, b * 512 + 512), [("ps", pi)])
            evac(b, pi, M)

    def proj_tm(l, hsrc, col0, n, evac):
        i, M = load_wcols(l, [(col0, n)], hsrc)
        for t in range(16):
            pi = ps_rot()
            for kc in range(8):
                mm(ps[pi][:, 0:n], hbv(hsrc, kc)[:, t * 128:(t + 1) * 128], wbb(hsrc, i)[:, kc, 0:n], kc == 0, kc == 7,
                   [("wbb", i)] + tl(hsrc, [kc], t * 128, t * 128 + 128), [("ps", pi)])
            evac(t, pi)

    def wout_partial(l, xdst, row0, nrows, ysrc_ap, ykeys_fn, hsrc):
        X = bufs[xdst]
        dma(wo[0:nrows, :], wout_d[l, row0:row0 + nrows, :], [], ["wo"])
        cp("gpsimd", wob(hsrc)[0:nrows, :], wo[0:nrows, :], ["wo"], ["wob"])
        for b in range(4):
            for dc in range(8):
                pi = ps_rot()
                mm(ps[pi][:, :], wob(hsrc)[0:nrows, dc * 128:(dc + 1) * 128], ysrc_ap(b), True, True,
                   ["wob"] + ykeys_fn(b), [("ps", pi)])
                k = tl(xdst, [dc], b * 512, b * 512 + 512)
                stt(X[:, dc, b * 512:(b + 1) * 512], ps[pi][:, :], mv[:, 16 + dc:17 + dc], X[:, dc, b * 512:(b + 1) * 512],
                    ALU.mult, ALU.add, [("ps", pi), "mv"] + k, k)

    def retention_head(l, h, xsrc, hsrc):
        qb_, kb_, vb_, gb_ = P[0], P[1], P[2], P[3]
        c0 = h * 64

        def rot_evac(dst, dkey, tab):
            store = {}

            def ev_a(b, pi, M):
                store[b] = pi
            return store, ev_a

        for (dst, dkey, base, tab) in ((qb_, ("P", 0), 0, 0), (kb_, ("P", 1), 256, 2)):
            ia, _ = load_wcols(l, [(base + c0, 64)], hsrc)
            ib_, _ = load_wcols(l, [(base + c0 + 32, 32), (base + c0, 32)], hsrc)
            for b in range(4):
                dma(St[0][0:64, :], rot_d[tab, h, :, b * 512:(b + 1) * 512], [], [("S", 0)])
                dma(St[1][0:64, :], rot_d[tab + 1, h, :, b * 512:(b + 1) * 512], [], [("S", 1)])
                pa, pb = ps_rot(), ps_rot()
                for (pi, wi) in ((pa, ia), (pb, ib_)):
                    for kc in range(8):
                        mm(ps[pi][0:64, :], wbb(hsrc, wi)[:, kc, 0:64], hbv(hsrc, kc)[:, b * 512:(b + 1) * 512], kc == 0, kc == 7,
                           [("wbb", wi)] + tl(hsrc, [kc], b * 512, b * 512 + 512), [("ps", pi)])
                tt("vector", St[2][0:64, :], ps[pa][0:64, :], St[0][0:64, :], ALU.mult, [("ps", pa), ("S", 0)], [("S", 2)])
                tt("vector", St[3][0:64, :], ps[pb][0:64, :], St[1][0:64, :], ALU.mult, [("ps", pb), ("S", 1)], [("S", 3)])
                tt("gpsimd", dst[0:64, b * 512:(b + 1) * 512], St[2][0:64, :], St[3][0:64, :], ALU.add,
                   [("S", 2), ("S", 3)], [(dkey, b)])
        vv = vb_[:, 0:1024].rearrange("p (t e) -> p t e", t=16)

        def ev_v(t, pi):
            cp("scalar", vv[:, t, :], ps[pi][:, 0:64], [("ps", pi)], [(("P", 2), t // 4)])
        proj_tm(l, hsrc, 512 + c0, 64, ev_v)

        def ev_g(b, pi, M):
            act(gb_[0:64, b * 512:(b + 1) * 512], ps[pi][0:64, :], AF.Silu, [("ps", pi)], [(("P", 3), b)])
        proj_fm(l, hsrc, [(768 + c0, 64)], ev_g)
        if l == 0 and h == 0:
            dump("qrot0", qb_[0:64, 0:S_LEN], bk(("P", 0)))
            dump("krot0", kb_[0:64, 0:S_LEN], bk(("P", 1)))
        for ib in range(4):
            po = ps_acc()
            njt = ib * 4 + 4
            for jt in range(njt):
                d = jt - 4 * ib
                c_lo = max(d, 0) * 128
                pi = ps_rot()
                mm(ps[pi][:, c_lo:512], kb_[0:64, jt * 128:(jt + 1) * 128], qb_[0:64, ib * 512 + c_lo:(ib + 1) * 512], True, True,
                   [(("P", 1), jt // 4), (("P", 0), ib)], [("ps", pi)])
                si = 4 + jt % 2
                sT = St[si]
                if d >= 0:
                    tt("vector", sT[:, c_lo:c_lo + 128], ps[pi][:, c_lo:c_lo + 128], tri, ALU.mult, [("ps", pi), "cst"], [("S", si)])
                    if c_lo + 128 < 512:
                        cp("scalar", sT[:, c_lo + 128:512], ps[pi][:, c_lo + 128:512], [("ps", pi)], [("S", si)])
                else:
                    cp("scalar" if jt % 2 else "vector", sT[:, :], ps[pi][:, :], [("ps", pi)], [("S", si)])
                mm(ps[po][0:64, c_lo:512], vv[:, jt, :], sT[:, c_lo:512], jt == 0, jt == njt - 1,
                   [(("P", 2), jt // 4), ("S", si)], [("ps", po)])
            y = St[6]
            cp("vector", y[0:64, :], ps[po][0:64, :], [("ps", po)], [("S", 6)])
            pm = ps_rot()
            mm(ps[pm][0:64, :], avg64[0:64, 0:64], y[0:64, :], True, True, [("S", 6), "cst"], [("ps", pm)])
            yc = St[7]
            tt("vector", yc[0:64, :], y[0:64, :], ps[pm][0:64, :], ALU.subtract, [("S", 6), ("ps", pm)], [("S", 7)])
            act(y[0:64, :], yc[0:64, :], AF.Square, [("S", 7)], [("S", 6)])
            pv = ps_rot()
            mm(ps[pv][0:64, :], avg64[0:64, 0:64], y[0:64, :], True, True, [("S", 6), "cst"], [("ps", pv)])
            act(y[0:64, :], ps[pv][0:64, :], AF.Sqrt, [("ps", pv), "mv"], [("S", 6)], bias=mv[0:64, 63:64], scale=1.0)
            recip(y[0:64, :], y[0:64, :], [("S", 6)], [("S", 6)])
            tt("vector", yc[0:64, :], yc[0:64, :], y[0:64, :], ALU.mult, [("S", 6), ("S", 7)], [("S", 7)])
            stt(qb_[0:64, ib * 512:ib * 512 + 256].bitcast(BF16), yc[0:64, :], sp[0:64, SP_RNW + h:SP_RNW + h + 1],
                gb_[0:64, ib * 512:(ib + 1) * 512], ALU.mult, ALU.mult,
                [("S", 7), "sp", (("P", 3), ib)], [(("P", 0), ib)])
        if l == 0 and h == 0:
            dump("yret0", qb_[0:64, 0:S_LEN], bk(("P", 0)))
        wout_partial(l, xsrc, c0, 64, lambda b: qb_[0:64, b * 512:b * 512 + 256].bitcast(BF16), lambda b: [(("P", 0), b)], hsrc)

    def moba_head(l, h, xsrc, hsrc):
        qa_, ka_, vb_, bp_ = P[0], P[1], P[2], P[3]
        c0 = h * 64
        dma(ka_[64:72, 0:S_LEN], kind_d, [], bk(("P", 1)))

        def ev_q(b, pi, M):
            act(qa_[0:64, b * 512:(b + 1) * 512], ps[pi][0:64, :], AF.Identity, [("ps", pi)], [(("P", 0), b)], scale=0.125)
        proj_fm(l, hsrc, [(1024 + c0, 64)], ev_q)

        def ev_k(b, pi, M):
            cp("vector", ka_[0:64, b * 512:(b + 1) * 512], ps[pi][0:64, :], [("ps", pi)], [(("P", 1), b)])
        proj_fm(l, hsrc, [(1408 + c0, 64)], ev_k)
        vv = vb_[:, 0:2048].rearrange("p (t e) -> p t e", t=16)
        memset("gpsimd", vv[:, :, 64:128], 1.0, bk(("P", 2)))

        def ev_v(t, pi):
            cp("scalar", vv[:, t, 0:64], ps[pi][:, 0:64], [("ps", pi)], [(("P", 2), t // 4)])
        proj_tm(l, hsrc, 1792 + c0, 64, ev_v)
        km = St[6]
        S.op("vector", lambda e: e.tensor_reduce(km[0:64, 0:8], ka_[0:64, 0:S_LEN].rearrange("p (n k) -> p n k", n=8), AX.X, ALU.add),
             bk(("P", 1)), [("S", 6)])
        bpv = bp_[:, 0:16 * 72].rearrange("p (t c) -> p t c", t=16)
        memset("gpsimd", bpv[:, :, 0:64], 0.0, bk(("P", 3)))
        memset("gpsimd", bpv[:, :, 64:72], NEG, bk(("P", 3)))
        gsb = St[7]
        for t in range(16):
            own = t // 2
            if own <= 3:
                if own > 0:
                    memset("gpsimd", bpv[:, t, 64:64 + own], 0.0, bk(("P", 3)))
            else:
                pi = ps_rot()
                mm(ps[pi][:, 0:8], qa_[0:64, t * 128:(t + 1) * 128], km[0:64, 0:8], True, True,
                   [(("P", 0), t // 4), ("S", 6)], [("ps", pi)], r32=False)
                g8 = gsb[:, t * 16:t * 16 + 8]
                m8 = gsb[:, t * 16 + 8:t * 16 + 16]
                memset("vector", g8, -1e30, [("S", 7)])
                cp("vector", gsb[:, t * 16:t * 16 + own], ps[pi][:, 0:own], [("ps", pi)], [("S", 7)])
                S.op("vector", lambda e, m8=m8, g8=g8: e.max(m8, g8), [("S", 7)], [("S", 7)])
                ts("vector", g8[:, 0:own], g8[:, 0:own], m8[:, 2:3], None, ALU.is_ge, None, [("S", 7)], [("S", 7)])
                ts("vector", bpv[:, t, 64:64 + own], g8[:, 0:own], -NEG, NEG, ALU.mult, ALU.add, [("S", 7)], bk(("P", 3)))
            memset("gpsimd", bpv[:, t, 64 + own:65 + own], 0.0, bk(("P", 3)))
            pi = ps_rot()
            mm(ps[pi][0:72, 0:128], bpv[:, t, :], ident, True, True, bk(("P", 3)) + ["cst"], [("ps", pi)])
            cp("vector", qa_[64:72, t * 128:(t + 1) * 128], ps[pi][64:72, 0:128], [("ps", pi)], [(("P", 0), t // 4)])
        if l == 0 and h == 0:
            dump("mqaug0", qa_[0:72, 0:S_LEN], bk(("P", 0)))
        for ib in range(4):
            po = ps_acc()
            njt = ib * 4 + 4
            for jt in range(njt):
                d = jt - 4 * ib
                c_lo = max(d, 0) * 128
                pi = ps_rot()
                mm(ps[pi][:, c_lo:512], ka_[0:72, jt * 128:(jt + 1) * 128], qa_[0:72, ib * 512 + c_lo:(ib + 1) * 512], True, True,
                   [(("P", 1), jt // 4), (("P", 0), ib)], [("ps", pi)])
                si = 4 + jt % 2
                eT = St[si]
                act(eT[:, c_lo:512], ps[pi][:, c_lo:512], AF.Exp, [("ps", pi)], [("S", si)])
                if d >= 0:
                    tt("gpsimd", eT[:, c_lo:c_lo + 128], eT[:, c_lo:c_lo + 128], tri, ALU.mult, [("S", si), "cst"], [("S", si)])
                mm(ps[po][:, c_lo:512], vv[:, jt, :], eT[:, c_lo:512], jt == 0, jt == njt - 1,
                   [(("P", 2), jt // 4), ("S", si)], [("ps", po)])
            rd = St[6]
            memset("gpsimd", rd[0:64, :], 0.0, [("S", 6)])
            recip(rd[64:128, :], ps[po][64:128, :], [("ps", po)], [("S", 6)])
            pm = ps_rot()
            mm(ps[pm][0:64, :], ident[:, 64:128], rd[:, :], True, True, [("S", 6), "cst"], [("ps", pm)], r32=False)
            yn = St[7]
            cp("scalar", yn[0:64, :], ps[pm][0:64, :], [("ps", pm)], [("S", 7)])
            tt("vector", qa_[0:64, ib * 512:ib * 512 + 256].bitcast(BF16), ps[po][0:64, :], yn[0:64, :], ALU.mult,
               [("ps", po), ("S", 7)], [(("P", 0), ib)])
        if l == 0 and h == 0:
            dump("ymoba0", qa_[0:64, 0:S_LEN], bk(("P", 0)))
        wout_partial(l, xsrc, 256 + c0, 64, lambda b: qa_[0:64, b * 512:b * 512 + 256].bitcast(BF16), lambda b: [(("P", 0), b)], hsrc)

    def lru_chunk(l, i, xsrc, hsrc):
        lx, ub, rb_, ib_, gb_ = P[0], P[1], P[2], P[3], P[4]
        c0 = i * 128
        memset("gpsimd", lx[:, 0:3], 0.0, [(("P", 0), 0)])

        def ev_x(b, pi, M):
            cp("vector", lx[:, 3 + b * 512:3 + (b + 1) * 512], ps[pi][:, :], [("ps", pi)], [(("P", 0), b), (("P", 0), min(b + 1, 3))])
        proj_fm(l, hsrc, [(2176 + c0, 128)], ev_x)
        allx = bk(("P", 0))
        cw = lambda j: sp[:, SP_CW + j * 3 + i:SP_CW + j * 3 + i + 1]
        ts("vector", ub[:, 0:S_LEN], lx[:, 0:S_LEN], cw(0), sp[:, SP_CB + i:SP_CB + i + 1], ALU.mult, ALU.add,
           allx + ["sp"], bk(("P", 1)))
        for j in range(1, 4):
            stt(ub[:, 0:S_LEN], lx[:, j:j + S_LEN], cw(j), ub[:, 0:S_LEN], ALU.mult, ALU.add, allx + ["sp"] + bk(("P", 1)), bk(("P", 1)))
        if stop == "lruA":
            dump("ylru0", ub[:, 0:S_LEN], bk(("P", 1)))
            return
        dma(wo[:, 0:128], wabd_d[l, i], [], ["wo"])
        dma(wo[:, 128:256], wxbd_d[l, i], [], ["wo"])
        for b in range(4):
            pa, px = ps_rot(), ps_rot()
            mm(ps[pa][:, :], wo[:, 0:128], ub[:, b * 512:(b + 1) * 512], True, True, ["wo", (("P", 1), b)], [("ps", pa)])
            mm(ps[px][:, :], wo[:, 128:256], ub[:, b * 512:(b + 1) * 512], True, True, ["wo", (("P", 1), b)], [("ps", px)])
            act(rb_[:, b * 512:(b + 1) * 512], ps[pa][:, :], AF.Sigmoid, [("ps", pa), "sp"], [(("P", 2), b)],
                bias=sp[:, SP_BA + i:SP_BA + i + 1], scale=1.0)
            act(ib_[:, b * 512:(b + 1) * 512], ps[px][:, :], AF.Sigmoid, [("ps", px), "sp"], [(("P", 3), b)],
                bias=sp[:, SP_BX + i:SP_BX + i + 1], scale=1.0)
        if stop == "lruB":
            dump("ylru0", rb_[:, 0:S_LEN], bk(("P", 2)))
            return
        tt("gpsimd", ib_[:, 0:S_LEN], ib_[:, 0:S_LEN], ub[:, 0:S_LEN], ALU.mult, bk(("P", 3)) + bk(("P", 1)), bk(("P", 3)))
        act(ub[:, 0:S_LEN], rb_[:, 0:S_LEN], AF.Exp, bk(("P", 2)) + ["mv"], bk(("P", 1)), scale=mv[:, 51 + i:52 + i])
        act(ub[:, 0:S_LEN], ub[:, 0:S_LEN], AF.Sqrt, bk(("P", 1)), bk(("P", 1)), bias=1.0, scale=-1.0)
        tt("gpsimd", ib_[:, 0:S_LEN], ib_[:, 0:S_LEN], ub[:, 0:S_LEN], ALU.mult, bk(("P", 3)) + bk(("P", 1)), bk(("P", 3)))
        act(rb_[:, 0:S_LEN], rb_[:, 0:S_LEN], AF.Exp, bk(("P", 2)) + ["mv"], bk(("P", 2)), scale=mv[:, 48 + i:49 + i])
        S.op("vector", lambda e: e.tensor_tensor_scan(ub[:, 0:S_LEN], rb_[:, 0:S_LEN], ib_[:, 0:S_LEN], 0.0, ALU.mult, ALU.add),
             bk(("P", 2)) + bk(("P", 3)), bk(("P", 1)))
        if stop == "lruC":
            dump("ylru0", ub[:, 0:S_LEN], bk(("P", 1)))
            return

        def ev_g(b, pi, M):
            sl = slice(b * 512, (b + 1) * 512)
            cp("vector", gb_[:, sl], ps[pi][:, :], [("ps", pi)], [(("P", 4), b)])
            act(rb_[:, sl], gb_[:, sl], AF.Square, [(("P", 4), b)], [(("P", 2), b)])
        proj_fm(l, hsrc, [(2560 + c0, 128)], ev_g)
        if stop == "lruD":
            dump("ylru0", gb_[:, 0:S_LEN], bk(("P", 4)))
            return
        ts("vector", rb_[:, 0:S_LEN], rb_[:, 0:S_LEN], 0.044715, 1.0, ALU.mult, ALU.add, bk(("P", 2)), bk(("P", 2)))
        tt("gpsimd", rb_[:, 0:S_LEN], rb_[:, 0:S_LEN], gb_[:, 0:S_LEN], ALU.mult, bk(("P", 2)) + bk(("P", 4)), bk(("P", 2)))
        act(rb_[:, 0:S_LEN], rb_[:, 0:S_LEN], AF.Sigmoid, bk(("P", 2)), bk(("P", 2)), scale=1.5957691216057308)
        tt("vector", gb_[:, 0:S_LEN], gb_[:, 0:S_LEN], rb_[:, 0:S_LEN], ALU.mult, bk(("P", 2)) + bk(("P", 4)), bk(("P", 4)))
        ylb = rb_[:, 0:1024].bitcast(BF16)
        tt("vector", ylb, gb_[:, 0:S_LEN], ub[:, 0:S_LEN], ALU.mult, bk(("P", 1)) + bk(("P", 4)) + bk(("P", 2)), bk(("P", 2)))
        if l == 0 and i == 0:
            dump("ylru0", gb_[:, 0:S_LEN], bk(("P", 4)))
        if stop == "lruE":
            return
        wout_partial(l, xsrc, 640 + c0, 128, lambda b: ylb[:, b * 512:(b + 1) * 512], lambda b: bk(("P", 2)), hsrc)

    def fence(keys_wait, keys_new):
        S.op("gpsimd", lambda e: e.memset(fz[:, 0:1], 0.0), (), list(keys_wait) + list(keys_new) + ["fz"])

    def moe(l, xsrc, hsrc):
        H = bufs[hsrc]
        acc = bufs[xsrc][:, :, :].rearrange("p c t -> p (c t)").rearrange("p (t d) -> p t d", t=16)
        ak = lambda t: [(xsrc, t // 2, 8 * (t % 2) + k_) for k_ in range(8)]
        hb = lambda c: P[c // 2][:, 0:2048].bitcast(BF16)[:, (c % 2) * 2048:(c % 2 + 1) * 2048]
        hbk = lambda c: bk(("P", c // 2))
        for c in range(8):
            cp("vector" if c % 2 == 0 else "gpsimd", hb(c), H[:, c, :], tl(hsrc, [c], 0, S_LEN), hbk(c))
        rwv = wo[:, 0:256].rearrange("p (k n) -> p k n", k=8)
        dma(rwv, rw_d[l].rearrange("(kc p) n -> p kc n", p=128), [], ["wo"])
        gtT = P[4]
        for t in range(16):
            pi = ps_rot()
            for kc in range(8):
                mm(ps[pi][:, 0:NE], H[:, kc, t * 128:(t + 1) * 128], rwv[:, kc, :], kc == 0, kc == 7,
                   ["wo"] + tl(hsrc, [kc], t * 128, t * 128 + 128), [("ps", pi)])
            lg = St[0]
            tt("vector", lg[:, 0:NE], ps[pi][:, 0:NE], rbt[:, :], ALU.add, [("ps", pi), "rbt"], [("S", 0)])
            S.op("vector", lambda e, lg=lg: e.max(lg[:, 32:40], lg[:, 0:NE]), [("S", 0)], [("S", 0)])
            ts("vector", lg[:, 40:41], lg[:, 32:33], -1.0, None, ALU.mult, None, [("S", 0)], [("S", 0)])
            ts("vector", lg[:, 64:96], lg[:, 0:NE], lg[:, 35:36], None, ALU.is_ge, None, [("S", 0)], [("S", 0)])
            act(lg[:, 96:128], lg[:, 0:NE], AF.Exp, [("S", 0)], [("S", 0)], bias=lg[:, 40:41], scale=1.0)
            tt("vector", lg[:, 96:128], lg[:, 96:128], lg[:, 64:96], ALU.mult, [("S", 0)], [("S", 0)])
            S.op("vector", lambda e, lg=lg: e.tensor_reduce(lg[:, 41:42], lg[:, 96:128], AX.X, ALU.add), [("S", 0)], [("S", 0)])
            recip(lg[:, 41:42], lg[:, 41:42], [("S", 0)], [("S", 0)])
            ts("vector", Gt[:, t, :], lg[:, 96:128], lg[:, 41:42], None, ALU.mult, None, [("S", 0)], ["G"])
            pt = ps_rot()
            mm(ps[pt][0:NE, 0:128], Gt[:, t, :], ident, True, True, ["G", "cst"], [("ps", pt)])
            cp("scalar", gtT[0:NE, t * 128:(t + 1) * 128], ps[pt][0:NE, 0:128], [("ps", pt)], [(("P", 4), t // 4)])
        if l == 0:
            dump("gates0", Gt[:, :, :], ["G"])
        dma(wo[0:NE, 0:D], bdn_d[l], [], ["wo"])
        for t in range(16):
            for hf in range(2):
                pi = ps_rot()
                mm(ps[pi][:, :], gtT[0:NE, t * 128:(t + 1) * 128], wo[0:NE, hf * 512:(hf + 1) * 512], True, True,
                   [(("P", 4), t // 4), "wo"], [("ps", pi)])
                cp("vector" if hf else "scalar", acc[:, t, hf * 512:(hf + 1) * 512], ps[pi][:, :], [("ps", pi)], ak(t))
        Bf = H[:, :, :].rearrange("p c t -> p (c t)")
        actall = Bf[:, 0:8192].bitcast(BF16)
        actv = lambda tb, j: actall[:, (tb * 8 + j) * 512:(tb * 8 + j + 1) * 512]
        wst = lambda s_: Bf[:, 8192 + s_ * 2048:8192 + (s_ + 1) * 2048].rearrange("p (k n) -> p k n", k=8)
        wgb = lambda s_: Bf[:, 12288 + s_ * 1024:12288 + (s_ + 1) * 1024].bitcast(BF16).rearrange("p (k n) -> p k n", k=8)
        dstg = lambda s_: Bf[:, 14336 + s_ * 1024:14336 + (s_ + 1) * 1024]
        wdb = lambda j: St[j][:, :].bitcast(BF16)
        wbf = [wbuf[i][:, :, :].rearrange("p k n -> p (k n)") for i in range(2)]
        T = [P[4][:, k * 512:(k + 1) * 512] for k in range(4)] + [wbf[0][:, 0:512], wbf[0][:, 512:1024], wbf[1][:, 0:512], wbf[1][:, 512:1024]]
        Tk = [(("P", 4), k) for k in range(4)] + [("wbh", 0, 0), ("wbh", 0, 1), ("wbh", 1, 0), ("wbh", 1, 1)]
        ovk = [("ov", "act", tb, j) for tb in range(4) for j in range(8)] + \
              [("ov", n_, s_) for n_ in ("wst", "wgb", "dst") for s_ in range(2)] + Tk[4:]
        oldk = tl(hsrc, range(8), 0, S_LEN) + [("wb", 0), ("wb", 1)]
        fence(oldk, ovk)

        pieces = [(e_, j) for e_ in range(NE) for j in range(8)]
        it_ = [0]

        def load_piece(p):
            e_, j = pieces[p]
            s_ = p % 2
            wg = wgu_d[l, e_].rearrange("(kc p) n -> p kc n", p=128)
            dma(wst(s_)[:, :, 0:128], wg[:, :, j * 128:(j + 1) * 128], [], [("ov", "wst", s_)])
            dma(wst(s_)[:, :, 128:256], wg[:, :, D + j * 128:D + (j + 1) * 128], [], [("ov", "wst", s_)])
            dma(dstg(s_), wdn_d[l, e_, j * 128:(j + 1) * 128, :], [], [("ov", "dst", s_)])
            act(wgb(s_), wst(s_), AF.Identity, [("ov", "wst", s_)], [("ov", "wgb", s_)])

        load_piece(0)
        for p, (e_, j) in enumerate(pieces):
            s_ = p % 2
            if p + 1 < len(pieces):
                load_piece(p + 1)
            cp("gpsimd", wdb(j), dstg(s_), [("ov", "dst", s_)], [("S", j)])
            bg = sp[:, SP_BGU + e_ * 16 + j:SP_BGU + e_ * 16 + j + 1]
            bu = sp[:, SP_BGU + e_ * 16 + 8 + j:SP_BGU + e_ * 16 + 8 + j + 1]
            for tb in range(4):
                pg, pu = ps_acc(), ps_acc()
                for (pi, o) in ((pg, 0), (pu, 128)):
                    for kc in range(8):
                        mm(ps[pi][:, :], wgb(s_)[:, kc, o:o + 128], hb(kc)[:, tb * 512:(tb + 1) * 512], kc == 0, kc == 7,
                           [("ov", "wgb", s_)] + hbk(kc), [("ps", pi)])
                k3 = (it_[0] % 3) * 2
                it_[0] += 1
                g1, u2 = T[k3], T[k3 + 1]
                k0, k2 = Tk[k3], Tk[k3 + 1]
                ts("vector", g1, ps[pg][:, :], bg, 7.0, ALU.add, ALU.min, [("ps", pg), "sp"], [k0])
                act(g1, g1, AF.Silu, [k0], [k0], scale=1.702)
                ts("vector", u2, ps[pu][:, :], bu, 7.0, ALU.add, ALU.min, [("ps", pu), "sp"], [k2])
                ts("vector", u2, u2, -7.0, 1.0, ALU.max, ALU.add, [k2], [k2])
                stt(actv(tb, j), g1, 1.0 / 1.702, u2, ALU.mult, ALU.mult, [k0, k2], [("ov", "act", tb, j)])
            if j == 7:
                for t in range(16):
                    tb, tq = t // 4, t % 4
                    for hf in range(2):
                        pi = ps_rot()
                        for jj in range(8):
                            mm(ps[pi][:, :], actv(tb, jj)[:, tq * 128:(tq + 1) * 128], wdb(jj)[:, hf * 512:(hf + 1) * 512],
                               jj == 0, jj == 7, [("ov", "act", tb, jj), ("S", jj)], [("ps", pi)])
                        act(T[6 + hf], ps[pi][:, :], AF.Identity, [("ps", pi), "G"], [Tk[6 + hf]], scale=Gt[:, t, e_:e_ + 1])
                        tt("gpsimd", acc[:, t, hf * 512:(hf + 1) * 512], acc[:, t, hf * 512:(hf + 1) * 512], T[6 + hf], ALU.add,
                           [Tk[6 + hf]] + ak(t), ak(t))
        fence(ovk, oldk)

    def moe_finish(l, accsrc, dst):
        acc = bufs[accsrc][:, :, :].rearrange("p c t -> p (c t)").rearrange("p (t d) -> p t d", t=16)
        ak = lambda t: [(accsrc, t // 2, 8 * (t % 2) + k_) for k_ in range(8)]
        Xn = bufs[dst]
        for t in range(16):
            xo_i = t % 2
            xo = P[xo_i][:, 0:1024].rearrange("p (c q) -> p c q", c=8)
            dma(xo, xsp_d[:, :, t * 128:(t + 1) * 128], ["xsp"], bk(("P", xo_i)))
            for half in range(2):
                pi = ps_rot()
                for q in range(4):
                    c = half * 4 + q
                    tp(ps[pi][:, q * 128:(q + 1) * 128], acc[:, t, c * 128:(c + 1) * 128], ident, ak(t) + ["cst"], [("ps", pi)])
                for q in range(4):
                    c = half * 4 + q
                    stt(Xn[:, c, t * 128:(t + 1) * 128], ps[pi][:, q * 128:(q + 1) * 128], mv[:, 40 + c:41 + c], xo[:, c, :],
                        ALU.mult, ALU.add, [("ps", pi), "mv"] + bk(("P", xo_i)), tl(dst, [c], t * 128, t * 128 + 128))

    def final_out(src, other):
        X, Y = bufs[src], bufs[other]
        rstd_compute(src, ("P", 0), P[0])
        for c in range(8):
            tmp_i = 1 + c % 2
            tt("vector", P[tmp_i][:, 0:S_LEN], X[:, c, :], P[0][:, 0:S_LEN], ALU.mult, tl(src, [c], 0, S_LEN) + bk(("P", 0)), bk(("P", tmp_i)))
            act(Y[:, c, :], P[tmp_i][:, 0:S_LEN], AF.Identity, bk(("P", tmp_i)) + ["gsp"], tl(other, [c], 0, S_LEN), scale=gsp[:, 8 + c:9 + c])
        for t in range(16):
            stg_i = 3 + t % 2
            stg = P[stg_i]
            for half in range(2):
                pi = ps_rot()
                for q in range(4):
                    c = half * 4 + q
                    tp(ps[pi][:, q * 128:(q + 1) * 128], Y[:, c, t * 128:(t + 1) * 128], ident,
                       tl(other, [c], t * 128, t * 128 + 128) + ["cst"], [("ps", pi)])
                cp("vector" if half == 0 else "scalar", stg[:, half * 512:(half + 1) * 512], ps[pi][:, :], [("ps", pi)], bk(("P", stg_i)))
            dma(out_d[t * 128:(t + 1) * 128, :], stg[:, 0:D], bk(("P", stg_i)), [("out", t)])

    cur, oth = "A", "B"
    load_x(cur)
    for l in range(L):
        layer_prologue(l)
        if stop == "mod":
            break
        rstd_compute(cur, ("P", 0), P[0])
        mix_fence(oth)
        norm_mod(cur, oth, ("P", 0), P[0], 0, 8, bf=True)
        if l == 0:
            dump("h0", bufs[oth][:, :, :], tl(oth, range(8), 0, S_LEN))
        if stop == "h":
            break
        for h in range(4):
            retention_head(l, h, cur, oth)
            if stop == "ret0":
                break
        if stop in ("ret0", "ret"):
            break
        for h in range(6):
            moba_head(l, h, cur, oth)
            if stop == "moba0":
                break
        if stop in ("moba0", "moba"):
            break
        for i in range(3):
            lru_chunk(l, i, cur, oth)
            if stop in ("lru0", "lruA", "lruB", "lruC", "lruD", "lruE"):
                break
        if l == 0:
            dump("xmix0", bufs[cur][:, :, :], tl(cur, range(8), 0, S_LEN))
        if stop in ("lru0", "mix", "lruA", "lruB", "lruC", "lruD", "lruE"):
            break
        rstd_compute(cur, ("P", 0), P[0])
        mix_fence(oth)
        norm_mod(cur, oth, ("P", 0), P[0], 24, 32)
        for c_ in range(8):
            dma(xsp_d[:, c_, :], bufs[cur][:, c_, :], tl(cur, [c_], 0, S_LEN), ["xsp"])
        moe(l, cur, oth)
        moe_finish(l, cur, oth)
        cur, oth = oth, cur
    final_out(cur, oth)
    S.wait_keys("sync", [("out", t) for t in range(16)] + [("dbg", n) for n in dbg_d])

    def replay(name, e):
        for waits, fn, tok in S.streams[name]:
            for s_, v in waits:
                e.wait_ge(sems[s_], v)
            if fn is None:
                continue
            inst = fn(e)
            if tok[0] == name:
                inst.then_inc(sems[name], 1)
            else:
                inst.then_inc(sems[tok[0]], 16)

    with nc.Block() as block:
        @block.sync
        def _(e):
            replay("sync", e)

        @block.scalar
        def _(e):
            replay("scalar", e)

        @block.vector
        def _(e):
            replay("vector", e)

        @block.gpsimd
        def _(e):
            replay("gpsimd", e)

        @block.tensor
        def _(e):
            replay("tensor", e)
    es.close()
    return nc


def _consts():
    cst = np.zeros((128, 6 * 128), np.float32)
    cst[:, 0:128] = np.eye(128, dtype=np.float32)
    cst[:, 128:256] = 1.0
    a = np.zeros((128, 128), np.float32)
    a[0:64, 0:64] = 1.0 / 64
    a[64:128, 64:128] = 1.0 / 64
    cst[:, 256:384] = a
    cst[:, 384:448] = 1.0
    cst[:, 576:640] = 1.0
    p = np.arange(128)[:, None]
    c = np.arange(128)[None, :]
    cst[:, 640:768] = (c >= p).astype(np.float32)
    kind = np.zeros((8, S_LEN), np.float32)
    for n in range(8):
        kind[n, n * 256:(n + 1) * 256] = 1.0
    pos = np.arange(S_LEN, dtype=np.float64)
    half = 32
    inv = 1.0 / (10000.0 ** (np.arange(half, dtype=np.float64) / half))
    ang = pos[None, :] * inv[:, None]
    cos = np.concatenate([np.cos(ang), np.cos(ang)], 0)
    sin = np.concatenate([-np.sin(ang), np.sin(ang)], 0)
    rot = np.zeros((4, 4, 64, S_LEN), np.float64)
    for h in range(4):
        lg = np.log1p(-2.0 ** (-5.0 - h))
        dq = np.exp(lg * pos)[None, :]
        dk = np.exp(-lg * pos)[None, :] * (64 ** -0.5)
        rot[0, h] = cos * dq
        rot[1, h] = sin * dq
        rot[2, h] = cos * dk
        rot[3, h] = sin * dk
    return cst, kind, rot.astype(np.float32)


def _fm(v):
    v = np.asarray(v, np.float32)
    return np.ascontiguousarray(v.reshape(-1, 128).T)


def prepare_inputs(inp):
    cst, kind, rot = _consts()
    sp = np.zeros((NL, 128, SP_N), np.float32)
    wabd = np.zeros((NL, 3, 128, 128), np.float32)
    wxbd = np.zeros((NL, 3, 128, 128), np.float32)
    rbt = np.zeros((NL, 128, NE), np.float32)
    for l in range(NL):
        sp[l, :, SP_ADAB:SP_ADAB + 48] = _fm(inp["ada_b"][l])
        sp[l, :, SP_NW1:SP_NW1 + 8] = _fm(inp["norm_mix_w"][l])
        sp[l, :, SP_NW2:SP_NW2 + 8] = _fm(inp["norm_ffn_w"][l])
        sp[l, 0:64, SP_RNW:SP_RNW + 4] = np.asarray(inp["ret_norm_w"][l], np.float32).reshape(4, 64).T
        for j in range(4):
            sp[l, :, SP_CW + j * 3:SP_CW + j * 3 + 3] = _fm(inp["lru_conv_w"][l, j])
        sp[l, :, SP_CB:SP_CB + 3] = _fm(inp["lru_conv_b"][l])
        sp[l, :, SP_BA:SP_BA + 3] = _fm(inp["lru_gate_a_b"][l])
        sp[l, :, SP_BX:SP_BX + 3] = _fm(inp["lru_gate_x_b"][l])
        sp[l, :, SP_LAM:SP_LAM + 3] = _fm(inp["lru_lambda"][l])
        for e in range(NE):
            sp[l, :, SP_BGU + e * 16:SP_BGU + e * 16 + 16] = _fm(inp["moe_b_gu"][l, e])
        for i in range(3):
            for g in range(2):
                wabd[l, i, g * 64:(g + 1) * 64, g * 64:(g + 1) * 64] = inp["lru_gate_a_w"][l, 2 * i + g]
                wxbd[l, i, g * 64:(g + 1) * 64, g * 64:(g + 1) * 64] = inp["lru_gate_x_w"][l, 2 * i + g]
        rbt[l] = np.broadcast_to(np.asarray(inp["router_b"][l], np.float32)[None, :], (128, NE))
    shared = {
        "sp": sp, "rbt": rbt, "bdn": np.ascontiguousarray(inp["moe_b_down"], dtype=np.float32),
        "ada_w": np.ascontiguousarray(inp["ada_w"], dtype=np.float32),
        "w_in": np.ascontiguousarray(inp["w_in"], dtype=np.float32),
        "w_out": np.ascontiguousarray(inp["w_out"], dtype=np.float32),
        "router_w": np.ascontiguousarray(inp["router_w"], dtype=np.float32),
        "moe_w_gu": np.ascontiguousarray(inp["moe_w_gu"], dtype=np.float32),
        "moe_w_down": np.ascontiguousarray(inp["moe_w_down"], dtype=np.float32),
        "wabd": wabd, "wxbd": wxbd, "cst": cst, "kind": kind, "rot": rot,
    }
    maps = []
    for b in range(inp["x"].shape[0]):
        gsp = np.zeros((128, 16), np.float32)
        gsp[:, 0:8] = _fm(inp["c"][b])
        gsp[:, 8:16] = _fm(inp["final_norm_w"])
        m = dict(shared)
        m["x"] = np.ascontiguousarray(inp["x"][b], dtype=np.float32)
        m["gsp"] = gsp
        maps.append(m)
    return maps


def kernel(**inputs):
    inp = {k: np.asarray(v) for k, v in inputs.items()}
    maps = prepare_inputs(inp)
    nc = build_program()
    res = run_bass_kernel_spmd(nc, maps, core_ids=list(range(len(maps))))
    out = np.stack([np.asarray(r["out"], dtype=np.float32) for r in res.results], axis=0)
    return out
```

```python
import numpy as np
from contextlib import ExitStack
import concourse.bass as bass
import concourse.mybir as mybir
from concourse.bass_utils import run_bass_kernel_spmd

F32 = mybir.dt.float32
F32R = mybir.dt.float32r
BF16 = mybir.dt.bfloat16
AF = mybir.ActivationFunctionType
ALU = mybir.AluOpType
AX = mybir.AxisListType

D = 1024
S_LEN = 2048
NL = 2
NE = 32
IN_W = 2944
EPS = 1e-6
NEG = -32768.0
NDMA = 24
SAME_ENGINE_SYNC = True
ENGS = ["sync", "scalar", "vector", "gpsimd", "tensor"]

SP_ADAB = 0
SP_NW1 = 48
SP_NW2 = 56
SP_RNW = 64
SP_CW = 68
SP_CB = 80
SP_BA = 83
SP_BX = 86
SP_LAM = 89
SP_BGU = 92
SP_N = 92 + 512


class Sched:
    def __init__(self):
        self.streams = {e: [] for e in ENGS}
        self.cnt = {e: 0 for e in ENGS}
        self.known = {e: {} for e in ENGS}
        self.lastw = {}
        self.rd = {}
        self.ndma = 0
        self.dma_val = [0] * NDMA

    def _deps(self, reads, writes):
        toks = []
        for k in reads:
            t = self.lastw.get(k)
            if t is not None:
                toks.append(t)
        for k in writes:
            t = self.lastw.get(k)
            if t is not None:
                toks.append(t)
            for s, v in self.rd.get(k, {}).items():
                toks.append((s, v))
        return toks

    def _commit(self, tok, reads, writes):
        s, v = tok
        for k in reads:
            d = self.rd.setdefault(k, {})
            if d.get(s, 0) < v:
                d[s] = v
        for k in writes:
            self.lastw[k] = tok
            self.rd[k] = {}

    def _filter(self, eng, toks):
        best = {}
        for s, v in toks:
            if s == eng and (eng == "tensor" or not SAME_ENGINE_SYNC):
                continue
            if v > best.get(s, 0):
                best[s] = v
        out = []
        kn = self.known[eng]
        for s, v in best.items():
            if kn.get(s, 0) >= v:
                continue
            kn[s] = v
            out.append((s, v))
        return out

    def op(self, eng, fn, reads=(), writes=()):
        toks = self._deps(reads, writes)
        waits = self._filter(eng, toks)
        self.cnt[eng] += 1
        tok = (eng, self.cnt[eng])
        self.streams[eng].append((waits, fn, tok))
        self._commit(tok, reads, writes)

    def dma(self, eng, fn, reads=(), writes=()):
        toks = self._deps(reads, writes)
        i = self.ndma % NDMA
        self.ndma += 1
        if self.dma_val[i] > 0:
            toks.append((("dma", i), self.dma_val[i]))
        self.dma_val[i] += 16
        tok = (("dma", i), self.dma_val[i])
        waits = self._filter(eng, toks)
        self.streams[eng].append((waits, fn, tok))
        self._commit(tok, reads, writes)

    def wait_keys(self, eng, keys):
        toks = self._deps(keys, keys)
        waits = self._filter(eng, toks)
        self.streams[eng].append((waits, None, None))


def tl(name, cs, t0, t1):
    return [(name, c, t) for c in cs for t in range(t0 // 128, (t1 + 127) // 128)]


def bk(name, t0=0, t1=S_LEN):
    return [(name, b) for b in range(t0 // 512, (t1 + 511) // 512)]


def build_program(L=NL, dbg=(), stop=None):
    nc = bass.Bass("TRN2", target_bir_lowering=False)
    S = Sched()
    dt = lambda name, shape, kind="ExternalInput": nc.dram_tensor(name, shape, F32, kind=kind).ap()
    x_d = dt("x", [S_LEN, D])
    sp_d = dt("sp", [NL, 128, SP_N])
    gsp_d = dt("gsp", [128, 16])
    rbt_d = dt("rbt", [NL, 128, NE])
    bdn_d = dt("bdn", [NL, NE, D])
    adaw_d = dt("ada_w", [NL, D, 6 * D])
    win_d = dt("w_in", [NL, D, IN_W])
    wout_d = dt("w_out", [NL, D, D])
    rw_d = dt("router_w", [NL, D, NE])
    if stop is None:
        wgu_d = dt("moe_w_gu", [NL, NE, D, 2 * D])
        wdn_d = dt("moe_w_down", [NL, NE, D, D])
    wabd_d = dt("wabd", [NL, 3, 128, 128])
    wxbd_d = dt("wxbd", [NL, 3, 128, 128])
    cst_d = dt("cst", [128, 6 * 128])
    kind_d = dt("kind", [8, S_LEN])
    rot_d = dt("rot", [4, 4, 64, S_LEN])
    out_d = dt("out", [S_LEN, D], kind="ExternalOutput")
    xsp_d = dt("xsp", [128, 8, S_LEN], kind="Internal")
    dbg_d = {}
    for name, shape in dbg:
        dbg_d[name] = dt("dbg_" + name, list(shape), kind="ExternalOutput")

    es = ExitStack()
    sb = lambda name, shape: es.enter_context(nc.sbuf_tensor("sb_" + name, shape, F32))
    bufA = sb("bufA", [128, 8, S_LEN])
    bufB = sb("bufB", [128, 8, S_LEN])
    cst = sb("cst", [128, 6 * 128])
    sp = sb("sp", [128, SP_N])
    gsp = sb("gsp", [128, 16])
    rbt = sb("rbt", [128, NE])
    P = [sb(f"P{i}", [128, 2052]) for i in range(5)]
    St = [sb(f"S{i}", [128, 512]) for i in range(8)]
    wbuf = [sb(f"wb{i}", [128, 8, 128]) for i in range(2)]
    wo = sb("wo", [128, D])
    Gt = sb("G", [128, 16, NE])
    mv = sb("mv", [128, 64])
    fz = sb("fz", [128, 8])
    ps = [es.enter_context(nc.psum_tensor(f"ps{i}", [128, 512], F32)) for i in range(8)]
    sems = {}
    for e in ENGS[1:]:
        sems[e] = es.enter_context(nc.semaphore("sem_" + e))
    for i in range(NDMA):
        sems[("dma", i)] = es.enter_context(nc.semaphore(f"sem_dma{i}"))

    ident = cst[:, 0:128]
    ones = cst[:, 128:256]
    avg64 = cst[:, 256:384]
    tri = cst[:, 640:768]

    rot_i = [0]
    acc_i = [0]

    def ps_rot():
        i = rot_i[0] % 4
        rot_i[0] += 1
        return i

    def ps_acc():
        i = 4 + acc_i[0] % 4
        acc_i[0] += 1
        return i

    def dma(out, in_, R, W, eng="sync"):
        S.dma(eng, lambda e: e.dma_start(out=out, in_=in_), R, W)

    def mm(out, lhsT, rhs, start, stop, R, W, r32=False):
        if r32:
            lhsT, rhs = lhsT.bitcast(F32R), rhs.bitcast(F32R)
        S.op("tensor", lambda e: e.matmul(out, lhsT, rhs, start=start, stop=stop), R, W)

    def tp(out, in_, idn, R, W):
        S.op("tensor", lambda e: e.transpose(out, in_, idn), R, W)

    def act(out, in_, func, R, W, bias=None, scale=None):
        kw = {}
        if bias is not None:
            kw["bias"] = bias
        if scale is not None:
            kw["scale"] = scale
        S.op("scalar", lambda e: e.activation(out, in_, func, **kw), R, W)

    def tt(eng, out, in0, in1, op, R, W):
        S.op(eng, lambda e: e.tensor_tensor(out, in0, in1, op), R, W)

    def ts(eng, out, in0, s1, s2, op0, op1, R, W):
        if op1 is None:
            S.op(eng, lambda e: e.tensor_scalar(out, in0, s1, None, op0), R, W)
        else:
            S.op(eng, lambda e: e.tensor_scalar(out, in0, s1, s2, op0, op1), R, W)

    def stt(out, in0, sc, in1, op0, op1, R, W):
        S.op("vector", lambda e: e.scalar_tensor_tensor(out, in0, sc, in1, op0, op1), R, W)

    def cp(eng, out, in_, R, W):
        if eng == "scalar":
            S.op(eng, lambda e: e.activation(out, in_, AF.Identity), R, W)
        else:
            S.op(eng, lambda e: e.tensor_copy(out, in_), R, W)

    def recip(out, in_, R, W):
        S.op("vector", lambda e: e.reciprocal(out, in_), R, W)

    def memset(eng, ap, val, W):
        S.op(eng, lambda e: e.memset(ap, val), (), W)

    def dump(name, src, R):
        if name in dbg_d:
            dma(dbg_d[name], src, R, [("dbg", name)])

    dma(cst[:], cst_d, [], ["cst"])
    dma(gsp[:], gsp_d, [], ["gsp"])

    bufs = {"A": bufA, "B": bufB}

    def bfl(name):
        return bufs[name][:, :, :].rearrange("p c t -> p (c t)")

    def hbv(name, c):
        return bfl(name)[:, 0:8192].bitcast(BF16)[:, c * 2048:(c + 1) * 2048]

    def wbb(name, i):
        return bfl(name)[:, 8192 + i * 512:8192 + (i + 1) * 512].bitcast(BF16).rearrange("p (k n) -> p k n", k=8)

    def wob(name):
        return bfl(name)[:, 9216:9728].bitcast(BF16)

    def mix_fence(name):
        S.op("gpsimd", lambda e: e.memset(fz[:, 1:2], 0.0), (),
             tl(name, range(8), 0, S_LEN) + [("wbb", 0), ("wbb", 1), "wob", "fz"])

    def load_x(dst):
        X = bufs[dst]
        for tt_ in range(16):
            stg = P[tt_ % 2]
            dma(stg[:, 0:D], x_d[tt_ * 128:(tt_ + 1) * 128, :], [], [("P", tt_ % 2)])
            for half in range(2):
                pi = ps_rot()
                for q in range(4):
                    c = half * 4 + q
                    tp(ps[pi][:, q * 128:(q + 1) * 128], stg[:, c * 128:(c + 1) * 128], ident,
                       [("P", tt_ % 2), "cst"], [("ps", pi)])
                cp("vector" if half == 0 else "scalar",
                   X[:, half * 4:half * 4 + 4, tt_ * 128:(tt_ + 1) * 128],
                   ps[pi][:, :].rearrange("p (q t) -> p q t", q=4),
                   [("ps", pi)], tl(dst, range(half * 4, half * 4 + 4), tt_ * 128, tt_ * 128 + 128))

    def rstd_compute(src, rbuf_key, rbuf):
        X = bufs[src]
        for b in range(4):
            pi = ps_acc()
            for c in range(8):
                sq = St[c % 4]
                act(sq[:, :], X[:, c, b * 512:(b + 1) * 512], AF.Square,
                    tl(src, [c], b * 512, b * 512 + 512), [("S", c % 4)])
                mm(ps[pi][:, :], ones, sq[:, :], c == 0, c == 7, [("S", c % 4), "cst"], [("ps", pi)])
            act(rbuf[:, b * 512:(b + 1) * 512], ps[pi][:, :], AF.Sqrt, [("ps", pi), "mv"], [(rbuf_key, b)],
                bias=mv[:, 63:64], scale=1.0 / D)
            recip(rbuf[:, b * 512:(b + 1) * 512], rbuf[:, b * 512:(b + 1) * 512], [(rbuf_key, b)], [(rbuf_key, b)])

    def norm_mod(src, dst, rbuf_key, rbuf, acol, bcol, bf=False):
        X, H = bufs[src], bufs[dst]
        for c in range(8):
            tmp_i = 1 + c % 2
            tmp = P[tmp_i]
            tt("vector", tmp[:, 0:S_LEN], X[:, c, :], rbuf[:, 0:S_LEN], ALU.mult,
               tl(src, [c], 0, S_LEN) + bk(rbuf_key), bk(("P", tmp_i)))
            act(hbv(dst, c) if bf else H[:, c, :], tmp[:, 0:S_LEN], AF.Identity, bk(("P", tmp_i)) + ["mv"], tl(dst, [c], 0, S_LEN),
                bias=mv[:, bcol + c:bcol + c + 1], scale=mv[:, acol + c:acol + c + 1])

    memset("vector", mv[:, :], 0.0, ["mv"])
    memset("vector", mv[:, 63:64], EPS, ["mv"])
    for i_ in range(5):
        memset("gpsimd" if i_ % 2 else "vector", P[i_][:, :], 0.0, bk(("P", i_)))
    for i_ in range(8):
        memset("gpsimd" if i_ % 2 else "vector", St[i_][:, :], 0.0, [("S", i_)])

    def layer_prologue(l):
        dma(sp[:], sp_d[l], [], ["sp"])
        dma(rbt[:], rbt_d[l], [], ["rbt"])
        cact = St[7]
        act(cact[:, 0:8], gsp[:, 0:8], AF.Silu, ["gsp"], [("S", 7)])
        pm = ps_acc()
        aw = adaw_d[l].rearrange("(kc p) n -> p kc n", p=128)
        for blk in range(24):
            wtile = P[3 + blk % 2]
            wv = wtile[:, 0:2048].rearrange("p (k n) -> p k n", k=8)
            dma(wv, aw[:, :, blk * 256:(blk + 1) * 256], [], bk(("P", 3 + blk % 2)))
            for jj in range(2):
                j = blk * 2 + jj
                for kc in range(8):
                    mm(ps[pm][:, j:j + 1], wv[:, kc, jj * 128:(jj + 1) * 128], cact[:, kc:kc + 1], kc == 0, kc == 7,
                       bk(("P", 3 + blk % 2)) + [("S", 7)], [("ps", pm)], r32=False)
        modt = St[6]
        tt("vector", modt[:, 0:48], ps[pm][:, 0:48], sp[:, SP_ADAB:SP_ADAB + 48], ALU.add, [("ps", pm), "sp"], [("S", 6)])
        ts("vector", mv[:, 0:8], modt[:, 8:16], 1.0, None, ALU.add, None, [("S", 6)], ["mv"])
        tt("vector", mv[:, 0:8], mv[:, 0:8], sp[:, SP_NW1:SP_NW1 + 8], ALU.mult, ["mv", "sp"], ["mv"])
        cp("vector", mv[:, 8:16], modt[:, 0:8], [("S", 6)], ["mv"])
        cp("vector", mv[:, 16:24], modt[:, 16:24], [("S", 6)], ["mv"])
        ts("vector", mv[:, 24:32], modt[:, 32:40], 1.0, None, ALU.add, None, [("S", 6)], ["mv"])
        tt("vector", mv[:, 24:32], mv[:, 24:32], sp[:, SP_NW2:SP_NW2 + 8], ALU.mult, ["mv", "sp"], ["mv"])
        cp("vector", mv[:, 32:40], modt[:, 24:32], [("S", 6)], ["mv"])
        cp("vector", mv[:, 40:48], modt[:, 40:48], [("S", 6)], ["mv"])
        act(mv[:, 54:57], sp[:, SP_LAM:SP_LAM + 3], AF.Exp, ["sp"], ["mv"], scale=-1.0)
        act(mv[:, 54:57], mv[:, 54:57], AF.Ln, ["mv"], ["mv"], bias=1.0)
        ts("vector", mv[:, 48:51], mv[:, 54:57], -8.0, None, ALU.mult, None, ["mv"], ["mv"])
        ts("vector", mv[:, 51:54], mv[:, 54:57], -16.0, None, ALU.mult, None, ["mv"], ["mv"])
        if l == 0:
            dump("mod0", modt[:, 0:48], [("S", 6)])

    wb_i = [0]

    def load_wcols(l, col_pieces, hsrc):
        i = wb_i[0] % 2
        wb_i[0] += 1
        wv = win_d[l].rearrange("(kc p) n -> p kc n", p=128)
        o = 0
        for c0, n in col_pieces:
            dma(wbuf[i][:, :, o:o + n], wv[:, :, c0:c0 + n], [], [("wb", i)])
            o += n
        cp("gpsimd", wbb(hsrc, i)[:, :, 0:o], wbuf[i][:, :, 0:o], [("wb", i)], [("wbb", i)])
        return i, o

    def proj_fm(l, hsrc, col_pieces, evac):
        i, M = load_wcols(l, col_pieces, hsrc)
        for b in range(4):
            pi = ps_rot()
            for kc in range(8):
                mm(ps[pi][0:M, :], wbb(hsrc, i)[:, kc, 0:M], hbv(hsrc, kc)[:, b * 512:(b + 1) * 512], kc == 0, kc == 7,
                   [("wbb", i)] + tl(hsrc, [kc], b * 512, b * 512 + 512), [("ps", pi)])
            evac(b, pi, M)

    def proj_tm(l, hsrc, col0, n, evac):
        i, M = load_wcols(l, [(col0, n)], hsrc)
        for t in range(16):
            pi = ps_rot()
            for kc in range(8):
                mm(ps[pi][:, 0:n], hbv(hsrc, kc)[:, t * 128:(t + 1) * 128], wbb(hsrc, i)[:, kc, 0:n], kc == 0, kc == 7,
                   [("wbb", i)] + tl(hsrc, [kc], t * 128, t * 128 + 128), [("ps", pi)])
            evac(t, pi)

    def wout_partial(l, xdst, row0, nrows, ysrc_ap, ykeys_fn, hsrc):
        X = bufs[xdst]
        dma(wo[0:nrows, :], wout_d[l, row0:row0 + nrows, :], [], ["wo"])
        cp("gpsimd", wob(hsrc)[0:nrows, :], wo[0:nrows, :], ["wo"], ["wob"])
        for b in range(4):
            for dc in range(8):
                pi = ps_rot()
                mm(ps[pi][:, :], wob(hsrc)[0:nrows, dc * 128:(dc + 1) * 128], ysrc_ap(b), True, True,
                   ["wob"] + ykeys_fn(b), [("ps", pi)])
                k = tl(xdst, [dc], b * 512, b * 512 + 512)
                if dc % 2 == 0:
                    stt(X[:, dc, b * 512:(b + 1) * 512], ps[pi][:, :], mv[:, 16 + dc:17 + dc], X[:, dc, b * 512:(b + 1) * 512],
                        ALU.mult, ALU.add, [("ps", pi), "mv"] + k, k)
                else:
                    ti = 6 + (dc // 2) % 2
                    act(St[ti][:, :], ps[pi][:, :], AF.Identity, [("ps", pi), "mv"], [("S", ti)], scale=mv[:, 16 + dc:17 + dc])
                    tt("gpsimd", X[:, dc, b * 512:(b + 1) * 512], X[:, dc, b * 512:(b + 1) * 512], St[ti][:, :], ALU.add,
                       [("S", ti)] + k, k)

    def retention_head(l, h, xsrc, hsrc):
        qb_, kb_, vb_, gb_ = P[0], P[1], P[2], P[3]
        c0 = h * 64

        def rot_evac(dst, dkey, tab):
            store = {}

            def ev_a(b, pi, M):
                store[b] = pi
            return store, ev_a

        for (dst, dkey, base, tab) in ((qb_, ("P", 0), 0, 0), (kb_, ("P", 1), 256, 2)):
            ia, _ = load_wcols(l, [(base + c0, 64)], hsrc)
            ib_, _ = load_wcols(l, [(base + c0 + 32, 32), (base + c0, 32)], hsrc)
            for b in range(4):
                dma(St[0][0:64, :], rot_d[tab, h, :, b * 512:(b + 1) * 512], [], [("S", 0)])
                dma(St[1][0:64, :], rot_d[tab + 1, h, :, b * 512:(b + 1) * 512], [], [("S", 1)])
                pa, pb = ps_rot(), ps_rot()
                for (pi, wi) in ((pa, ia), (pb, ib_)):
                    for kc in range(8):
                        mm(ps[pi][0:64, :], wbb(hsrc, wi)[:, kc, 0:64], hbv(hsrc, kc)[:, b * 512:(b + 1) * 512], kc == 0, kc == 7,
                           [("wbb", wi)] + tl(hsrc, [kc], b * 512, b * 512 + 512), [("ps", pi)])
                tt("vector", St[2][0:64, :], ps[pa][0:64, :], St[0][0:64, :], ALU.mult, [("ps", pa), ("S", 0)], [("S", 2)])
                tt("vector", St[3][0:64, :], ps[pb][0:64, :], St[1][0:64, :], ALU.mult, [("ps", pb), ("S", 1)], [("S", 3)])
                tt("gpsimd", dst[0:64, b * 512:(b + 1) * 512], St[2][0:64, :], St[3][0:64, :], ALU.add,
                   [("S", 2), ("S", 3)], [(dkey, b)])
        vv = vb_[:, 0:1024].rearrange("p (t e) -> p t e", t=16)

        def ev_v(t, pi):
            cp("scalar", vv[:, t, :], ps[pi][:, 0:64], [("ps", pi)], [(("P", 2), t // 4)])
        proj_tm(l, hsrc, 512 + c0, 64, ev_v)

        def ev_g(b, pi, M):
            act(gb_[0:64, b * 512:(b + 1) * 512], ps[pi][0:64, :], AF.Silu, [("ps", pi)], [(("P", 3), b)])
        proj_fm(l, hsrc, [(768 + c0, 64)], ev_g)
        if l == 0 and h == 0:
            dump("qrot0", qb_[0:64, 0:S_LEN], bk(("P", 0)))
            dump("krot0", kb_[0:64, 0:S_LEN], bk(("P", 1)))
        for ib in range(4):
            po = ps_acc()
            njt = ib * 4 + 4
            def score(jt, ib=ib):
                d = jt - 4 * ib
                c_lo = max(d, 0) * 128
                pi = ps_rot()
                mm(ps[pi][:, c_lo:512], kb_[0:64, jt * 128:(jt + 1) * 128], qb_[0:64, ib * 512 + c_lo:(ib + 1) * 512], True, True,
                   [(("P", 1), jt // 4), (("P", 0), ib)], [("ps", pi)])
                si = 2 + jt % 4
                sT = St[si]
                if d >= 0:
                    tt("vector", sT[:, c_lo:c_lo + 128], ps[pi][:, c_lo:c_lo + 128], tri, ALU.mult, [("ps", pi), "cst"], [("S", si)])
                    if c_lo + 128 < 512:
                        cp("scalar", sT[:, c_lo + 128:512], ps[pi][:, c_lo + 128:512], [("ps", pi)], [("S", si)])
                else:
                    cp("scalar" if jt % 2 else "vector", sT[:, :], ps[pi][:, :], [("ps", pi)], [("S", si)])
                return si, c_lo
            pend = score(0)
            for jt in range(njt):
                nxt = score(jt + 1) if jt + 1 < njt else None
                si, c_lo = pend
                mm(ps[po][0:64, c_lo:512], vv[:, jt, :], St[si][:, c_lo:512], jt == 0, jt == njt - 1,
                   [(("P", 2), jt // 4), ("S", si)], [("ps", po)])
                pend = nxt
            y = St[6]
            cp("vector", y[0:64, :], ps[po][0:64, :], [("ps", po)], [("S", 6)])
            pm = ps_rot()
            mm(ps[pm][0:64, :], avg64[0:64, 0:64], y[0:64, :], True, True, [("S", 6), "cst"], [("ps", pm)])
            yc = St[7]
            tt("vector", yc[0:64, :], y[0:64, :], ps[pm][0:64, :], ALU.subtract, [("S", 6), ("ps", pm)], [("S", 7)])
            act(y[0:64, :], yc[0:64, :], AF.Square, [("S", 7)], [("S", 6)])
            pv = ps_rot()
            mm(ps[pv][0:64, :], avg64[0:64, 0:64], y[0:64, :], True, True, [("S", 6), "cst"], [("ps", pv)])
            act(y[0:64, :], ps[pv][0:64, :], AF.Sqrt, [("ps", pv), "mv"], [("S", 6)], bias=mv[0:64, 63:64], scale=1.0)
            recip(y[0:64, :], y[0:64, :], [("S", 6)], [("S", 6)])
            tt("vector", yc[0:64, :], yc[0:64, :], y[0:64, :], ALU.mult, [("S", 6), ("S", 7)], [("S", 7)])
            stt(qb_[0:64, ib * 512:ib * 512 + 256].bitcast(BF16), yc[0:64, :], sp[0:64, SP_RNW + h:SP_RNW + h + 1],
                gb_[0:64, ib * 512:(ib + 1) * 512], ALU.mult, ALU.mult,
                [("S", 7), "sp", (("P", 3), ib)], [(("P", 0), ib)])
        if l == 0 and h == 0:
            dump("yret0", qb_[0:64, 0:S_LEN], bk(("P", 0)))
        wout_partial(l, xsrc, c0, 64, lambda b: qb_[0:64, b * 512:b * 512 + 256].bitcast(BF16), lambda b: [(("P", 0), b)], hsrc)

    def moba_head(l, h, xsrc, hsrc):
        qa_, ka_, vb_, bp_ = P[0], P[1], P[2], P[3]
        c0 = h * 64
        dma(ka_[64:72, 0:S_LEN], kind_d, [], bk(("P", 1)))

        def ev_q(b, pi, M):
            act(qa_[0:64, b * 512:(b + 1) * 512], ps[pi][0:64, :], AF.Identity, [("ps", pi)], [(("P", 0), b)], scale=0.125)
        proj_fm(l, hsrc, [(1024 + c0, 64)], ev_q)

        def ev_k(b, pi, M):
            cp("vector", ka_[0:64, b * 512:(b + 1) * 512], ps[pi][0:64, :], [("ps", pi)], [(("P", 1), b)])
        proj_fm(l, hsrc, [(1408 + c0, 64)], ev_k)
        vv = vb_[:, 0:2048].rearrange("p (t e) -> p t e", t=16)
        memset("gpsimd", vv[:, :, 64:128], 1.0, bk(("P", 2)))

        def ev_v(t, pi):
            cp("scalar", vv[:, t, 0:64], ps[pi][:, 0:64], [("ps", pi)], [(("P", 2), t // 4)])
        proj_tm(l, hsrc, 1792 + c0, 64, ev_v)
        km = St[6]
        S.op("vector", lambda e: e.tensor_reduce(km[0:64, 0:8], ka_[0:64, 0:S_LEN].rearrange("p (n k) -> p n k", n=8), AX.X, ALU.add),
             bk(("P", 1)), [("S", 6)])
        bpv = bp_[:, 0:16 * 72].rearrange("p (t c) -> p t c", t=16)
        memset("gpsimd", bpv[:, :, 0:64], 0.0, bk(("P", 3)))
        memset("gpsimd", bpv[:, :, 64:72], NEG, bk(("P", 3)))
        gsb = St[7]
        for t in range(16):
            own = t // 2
            if own <= 3:
                if own > 0:
                    memset("gpsimd", bpv[:, t, 64:64 + own], 0.0, bk(("P", 3)))
            else:
                pi = ps_rot()
                mm(ps[pi][:, 0:8], qa_[0:64, t * 128:(t + 1) * 128], km[0:64, 0:8], True, True,
                   [(("P", 0), t // 4), ("S", 6)], [("ps", pi)], r32=False)
                g8 = gsb[:, t * 16:t * 16 + 8]
                m8 = gsb[:, t * 16 + 8:t * 16 + 16]
                memset("vector", g8, -1e30, [("S", 7)])
                cp("vector", gsb[:, t * 16:t * 16 + own], ps[pi][:, 0:own], [("ps", pi)], [("S", 7)])
                S.op("vector", lambda e, m8=m8, g8=g8: e.max(m8, g8), [("S", 7)], [("S", 7)])
                ts("vector", g8[:, 0:own], g8[:, 0:own], m8[:, 2:3], None, ALU.is_ge, None, [("S", 7)], [("S", 7)])
                ts("vector", bpv[:, t, 64:64 + own], g8[:, 0:own], -NEG, NEG, ALU.mult, ALU.add, [("S", 7)], bk(("P", 3)))
            memset("gpsimd", bpv[:, t, 64 + own:65 + own], 0.0, bk(("P", 3)))
        for t in range(16):
            pi = ps_rot()
            mm(ps[pi][0:72, 0:128], bpv[:, t, :], ident, True, True, bk(("P", 3)) + ["cst"], [("ps", pi)])
            cp("vector", qa_[64:72, t * 128:(t + 1) * 128], ps[pi][64:72, 0:128], [("ps", pi)], [(("P", 0), t // 4)])
        if l == 0 and h == 0:
            dump("mqaug0", qa_[0:72, 0:S_LEN], bk(("P", 0)))
        for ib in range(4):
            po = ps_acc()
            njt = ib * 4 + 4
            def score(jt, ib=ib):
                d = jt - 4 * ib
                c_lo = max(d, 0) * 128
                pi = ps_rot()
                mm(ps[pi][:, c_lo:512], ka_[0:72, jt * 128:(jt + 1) * 128], qa_[0:72, ib * 512 + c_lo:(ib + 1) * 512], True, True,
                   [(("P", 1), jt // 4), (("P", 0), ib)], [("ps", pi)])
                si = 2 + jt % 4
                eT = St[si]
                act(eT[:, c_lo:512], ps[pi][:, c_lo:512], AF.Exp, [("ps", pi)], [("S", si)])
                if d >= 0:
                    tt("gpsimd", eT[:, c_lo:c_lo + 128], eT[:, c_lo:c_lo + 128], tri, ALU.mult, [("S", si), "cst"], [("S", si)])
                return si, c_lo
            pend = score(0)
            for jt in range(njt):
                nxt = score(jt + 1) if jt + 1 < njt else None
                si, c_lo = pend
                mm(ps[po][:, c_lo:512], vv[:, jt, :], St[si][:, c_lo:512], jt == 0, jt == njt - 1,
                   [(("P", 2), jt // 4), ("S", si)], [("ps", po)])
                pend = nxt
            rd = St[6]
            memset("gpsimd", rd[0:64, :], 0.0, [("S", 6)])
            recip(rd[64:128, :], ps[po][64:128, :], [("ps", po)], [("S", 6)])
            pm = ps_rot()
            mm(ps[pm][0:64, :], ident[:, 64:128], rd[:, :], True, True, [("S", 6), "cst"], [("ps", pm)], r32=False)
            yn = St[7]
            cp("scalar", yn[0:64, :], ps[pm][0:64, :], [("ps", pm)], [("S", 7)])
            tt("vector", qa_[0:64, ib * 512:ib * 512 + 256].bitcast(BF16), ps[po][0:64, :], yn[0:64, :], ALU.mult,
               [("ps", po), ("S", 7)], [(("P", 0), ib)])
        if l == 0 and h == 0:
            dump("ymoba0", qa_[0:64, 0:S_LEN], bk(("P", 0)))
        wout_partial(l, xsrc, 256 + c0, 64, lambda b: qa_[0:64, b * 512:b * 512 + 256].bitcast(BF16), lambda b: [(("P", 0), b)], hsrc)

    def lru_chunk(l, i, xsrc, hsrc):
        lx, ub, rb_, ib_, gb_ = P[0], P[1], P[2], P[3], P[4]
        c0 = i * 128
        memset("gpsimd", lx[:, 0:3], 0.0, [(("P", 0), 0)])

        def ev_x(b, pi, M):
            cp("vector", lx[:, 3 + b * 512:3 + (b + 1) * 512], ps[pi][:, :], [("ps", pi)], [(("P", 0), b), (("P", 0), min(b + 1, 3))])
        proj_fm(l, hsrc, [(2176 + c0, 128)], ev_x)
        allx = bk(("P", 0))
        cw = lambda j: sp[:, SP_CW + j * 3 + i:SP_CW + j * 3 + i + 1]
        ts("vector", ub[:, 0:S_LEN], lx[:, 0:S_LEN], cw(0), sp[:, SP_CB + i:SP_CB + i + 1], ALU.mult, ALU.add,
           allx + ["sp"], bk(("P", 1)))
        for j in range(1, 4):
            stt(ub[:, 0:S_LEN], lx[:, j:j + S_LEN], cw(j), ub[:, 0:S_LEN], ALU.mult, ALU.add, allx + ["sp"] + bk(("P", 1)), bk(("P", 1)))
        if stop == "lruA":
            dump("ylru0", ub[:, 0:S_LEN], bk(("P", 1)))
            return
        dma(wo[:, 0:128], wabd_d[l, i], [], ["wo"])
        dma(wo[:, 128:256], wxbd_d[l, i], [], ["wo"])
        for b in range(4):
            pa, px = ps_rot(), ps_rot()
            mm(ps[pa][:, :], wo[:, 0:128], ub[:, b * 512:(b + 1) * 512], True, True, ["wo", (("P", 1), b)], [("ps", pa)])
            mm(ps[px][:, :], wo[:, 128:256], ub[:, b * 512:(b + 1) * 512], True, True, ["wo", (("P", 1), b)], [("ps", px)])
            act(rb_[:, b * 512:(b + 1) * 512], ps[pa][:, :], AF.Sigmoid, [("ps", pa), "sp"], [(("P", 2), b)],
                bias=sp[:, SP_BA + i:SP_BA + i + 1], scale=1.0)
            act(ib_[:, b * 512:(b + 1) * 512], ps[px][:, :], AF.Sigmoid, [("ps", px), "sp"], [(("P", 3), b)],
                bias=sp[:, SP_BX + i:SP_BX + i + 1], scale=1.0)
        if stop == "lruB":
            dump("ylru0", rb_[:, 0:S_LEN], bk(("P", 2)))
            return
        tt("gpsimd", ib_[:, 0:S_LEN], ib_[:, 0:S_LEN], ub[:, 0:S_LEN], ALU.mult, bk(("P", 3)) + bk(("P", 1)), bk(("P", 3)))
        act(ub[:, 0:S_LEN], rb_[:, 0:S_LEN], AF.Exp, bk(("P", 2)) + ["mv"], bk(("P", 1)), scale=mv[:, 51 + i:52 + i])
        act(ub[:, 0:S_LEN], ub[:, 0:S_LEN], AF.Sqrt, bk(("P", 1)), bk(("P", 1)), bias=1.0, scale=-1.0)
        tt("gpsimd", ib_[:, 0:S_LEN], ib_[:, 0:S_LEN], ub[:, 0:S_LEN], ALU.mult, bk(("P", 3)) + bk(("P", 1)), bk(("P", 3)))
        act(rb_[:, 0:S_LEN], rb_[:, 0:S_LEN], AF.Exp, bk(("P", 2)) + ["mv"], bk(("P", 2)), scale=mv[:, 48 + i:49 + i])
        S.op("vector", lambda e: e.tensor_tensor_scan(ub[:, 0:S_LEN], rb_[:, 0:S_LEN], ib_[:, 0:S_LEN], 0.0, ALU.mult, ALU.add),
             bk(("P", 2)) + bk(("P", 3)), bk(("P", 1)))
        if stop == "lruC":
            dump("ylru0", ub[:, 0:S_LEN], bk(("P", 1)))
            return

        def ev_g(b, pi, M):
            sl = slice(b * 512, (b + 1) * 512)
            cp("vector", gb_[:, sl], ps[pi][:, :], [("ps", pi)], [(("P", 4), b)])
            act(rb_[:, sl], gb_[:, sl], AF.Square, [(("P", 4), b)], [(("P", 2), b)])
        proj_fm(l, hsrc, [(2560 + c0, 128)], ev_g)
        if stop == "lruD":
            dump("ylru0", gb_[:, 0:S_LEN], bk(("P", 4)))
            return
        ts("vector", rb_[:, 0:S_LEN], rb_[:, 0:S_LEN], 0.044715, 1.0, ALU.mult, ALU.add, bk(("P", 2)), bk(("P", 2)))
        tt("gpsimd", rb_[:, 0:S_LEN], rb_[:, 0:S_LEN], gb_[:, 0:S_LEN], ALU.mult, bk(("P", 2)) + bk(("P", 4)), bk(("P", 2)))
        act(rb_[:, 0:S_LEN], rb_[:, 0:S_LEN], AF.Sigmoid, bk(("P", 2)), bk(("P", 2)), scale=1.5957691216057308)
        tt("vector", gb_[:, 0:S_LEN], gb_[:, 0:S_LEN], rb_[:, 0:S_LEN], ALU.mult, bk(("P", 2)) + bk(("P", 4)), bk(("P", 4)))
        ylb = rb_[:, 0:1024].bitcast(BF16)
        tt("vector", ylb, gb_[:, 0:S_LEN], ub[:, 0:S_LEN], ALU.mult, bk(("P", 1)) + bk(("P", 4)) + bk(("P", 2)), bk(("P", 2)))
        if l == 0 and i == 0:
            dump("ylru0", gb_[:, 0:S_LEN], bk(("P", 4)))
        if stop == "lruE":
            return
        wout_partial(l, xsrc, 640 + c0, 128, lambda b: ylb[:, b * 512:(b + 1) * 512], lambda b: bk(("P", 2)), hsrc)

    def fence(keys_wait, keys_new):
        S.op("gpsimd", lambda e: e.memset(fz[:, 0:1], 0.0), (), list(keys_wait) + list(keys_new) + ["fz"])

    def moe(l, xsrc, hsrc):
        H = bufs[hsrc]
        acc = bufs[xsrc][:, :, :].rearrange("p c t -> p (c t)").rearrange("p (t d) -> p t d", t=16)
        ak = lambda t: [(xsrc, t // 2, 8 * (t % 2) + k_) for k_ in range(8)]
        hb = lambda c: P[c // 2][:, 0:2048].bitcast(BF16)[:, (c % 2) * 2048:(c % 2 + 1) * 2048]
        hbk = lambda c: bk(("P", c // 2))
        for c in range(8):
            cp("vector" if c % 2 == 0 else "gpsimd", hb(c), H[:, c, :], tl(hsrc, [c], 0, S_LEN), hbk(c))
        rwv = wo[:, 0:256].rearrange("p (k n) -> p k n", k=8)
        dma(rwv, rw_d[l].rearrange("(kc p) n -> p kc n", p=128), [], ["wo"])
        gtT = P[4]
        for t in range(16):
            pi = ps_rot()
            for kc in range(8):
                mm(ps[pi][:, 0:NE], H[:, kc, t * 128:(t + 1) * 128], rwv[:, kc, :], kc == 0, kc == 7,
                   ["wo"] + tl(hsrc, [kc], t * 128, t * 128 + 128), [("ps", pi)])
            lg = St[0]
            tt("vector", lg[:, 0:NE], ps[pi][:, 0:NE], rbt[:, :], ALU.add, [("ps", pi), "rbt"], [("S", 0)])
            S.op("vector", lambda e, lg=lg: e.max(lg[:, 32:40], lg[:, 0:NE]), [("S", 0)], [("S", 0)])
            ts("vector", lg[:, 40:41], lg[:, 32:33], -1.0, None, ALU.mult, None, [("S", 0)], [("S", 0)])
            ts("vector", lg[:, 64:96], lg[:, 0:NE], lg[:, 35:36], None, ALU.is_ge, None, [("S", 0)], [("S", 0)])
            act(lg[:, 96:128], lg[:, 0:NE], AF.Exp, [("S", 0)], [("S", 0)], bias=lg[:, 40:41], scale=1.0)
            tt("vector", lg[:, 96:128], lg[:, 96:128], lg[:, 64:96], ALU.mult, [("S", 0)], [("S", 0)])
            S.op("vector", lambda e, lg=lg: e.tensor_reduce(lg[:, 41:42], lg[:, 96:128], AX.X, ALU.add), [("S", 0)], [("S", 0)])
            recip(lg[:, 41:42], lg[:, 41:42], [("S", 0)], [("S", 0)])
            ts("vector", Gt[:, t, :], lg[:, 96:128], lg[:, 41:42], None, ALU.mult, None, [("S", 0)], ["G"])
            pt = ps_rot()
            mm(ps[pt][0:NE, 0:128], Gt[:, t, :], ident, True, True, ["G", "cst"], [("ps", pt)])
            cp("scalar", gtT[0:NE, t * 128:(t + 1) * 128], ps[pt][0:NE, 0:128], [("ps", pt)], [(("P", 4), t // 4)])
        if l == 0:
            dump("gates0", Gt[:, :, :], ["G"])
        dma(wo[0:NE, 0:D], bdn_d[l], [], ["wo"])
        for t in range(16):
            for hf in range(2):
                pi = ps_rot()
                mm(ps[pi][:, :], gtT[0:NE, t * 128:(t + 1) * 128], wo[0:NE, hf * 512:(hf + 1) * 512], True, True,
                   [(("P", 4), t // 4), "wo"], [("ps", pi)])
                cp("vector" if hf else "scalar", acc[:, t, hf * 512:(hf + 1) * 512], ps[pi][:, :], [("ps", pi)], ak(t))
        Bf = H[:, :, :].rearrange("p c t -> p (c t)")
        actall = Bf[:, 0:8192].bitcast(BF16)
        actv = lambda tb, j: actall[:, (tb * 8 + j) * 512:(tb * 8 + j + 1) * 512]
        wst = lambda s_: Bf[:, 8192 + s_ * 2048:8192 + (s_ + 1) * 2048].rearrange("p (k n) -> p k n", k=8)
        wgb = lambda s_: Bf[:, 12288 + s_ * 1024:12288 + (s_ + 1) * 1024].bitcast(BF16).rearrange("p (k n) -> p k n", k=8)
        dstg = lambda s_: Bf[:, 14336 + s_ * 1024:14336 + (s_ + 1) * 1024]
        wdb = lambda j: St[j][:, :].bitcast(BF16)
        wbf = [wbuf[i][:, :, :].rearrange("p k n -> p (k n)") for i in range(2)]
        T = [P[4][:, k * 512:(k + 1) * 512] for k in range(4)] + [wbf[0][:, 0:512], wbf[0][:, 512:1024], wbf[1][:, 0:512], wbf[1][:, 512:1024]]
        Tk = [(("P", 4), k) for k in range(4)] + [("wbh", 0, 0), ("wbh", 0, 1), ("wbh", 1, 0), ("wbh", 1, 1)]
        ovk = [("ov", "act", tb, j) for tb in range(4) for j in range(8)] + \
              [("ov", n_, s_) for n_ in ("wst", "wgb", "dst") for s_ in range(2)] + Tk[4:]
        oldk = tl(hsrc, range(8), 0, S_LEN) + [("wb", 0), ("wb", 1)]
        fence(oldk, ovk)

        pieces = [(e_, j) for e_ in range(NE) for j in range(8)]
        it_ = [0]

        def load_piece(p):
            e_, j = pieces[p]
            s_ = p % 2
            wg = wgu_d[l, e_].rearrange("(kc p) n -> p kc n", p=128)
            dma(wst(s_)[:, :, 0:128], wg[:, :, j * 128:(j + 1) * 128], [], [("ov", "wst", s_)])
            dma(wst(s_)[:, :, 128:256], wg[:, :, D + j * 128:D + (j + 1) * 128], [], [("ov", "wst", s_)])
            dma(dstg(s_), wdn_d[l, e_, j * 128:(j + 1) * 128, :], [], [("ov", "dst", s_)])
            act(wgb(s_), wst(s_), AF.Identity, [("ov", "wst", s_)], [("ov", "wgb", s_)])

        load_piece(0)
        for p, (e_, j) in enumerate(pieces):
            s_ = p % 2
            if p + 1 < len(pieces):
                load_piece(p + 1)
            cp("gpsimd", wdb(j), dstg(s_), [("ov", "dst", s_)], [("S", j)])
            bg = sp[:, SP_BGU + e_ * 16 + j:SP_BGU + e_ * 16 + j + 1]
            bu = sp[:, SP_BGU + e_ * 16 + 8 + j:SP_BGU + e_ * 16 + 8 + j + 1]
            for tb in range(4):
                pg, pu = ps_acc(), ps_acc()
                for (pi, o) in ((pg, 0), (pu, 128)):
                    for kc in range(8):
                        mm(ps[pi][:, :], wgb(s_)[:, kc, o:o + 128], hb(kc)[:, tb * 512:(tb + 1) * 512], kc == 0, kc == 7,
                           [("ov", "wgb", s_)] + hbk(kc), [("ps", pi)])
                k3 = (it_[0] % 3) * 2
                it_[0] += 1
                g1, u2 = T[k3], T[k3 + 1]
                k0, k2 = Tk[k3], Tk[k3 + 1]
                ts("vector", g1, ps[pg][:, :], bg, 7.0, ALU.add, ALU.min, [("ps", pg), "sp"], [k0])
                act(g1, g1, AF.Silu, [k0], [k0], scale=1.702)
                ts("vector", u2, ps[pu][:, :], bu, 7.0, ALU.add, ALU.min, [("ps", pu), "sp"], [k2])
                ts("vector", u2, u2, -7.0, 1.0, ALU.max, ALU.add, [k2], [k2])
                stt(actv(tb, j), g1, 1.0 / 1.702, u2, ALU.mult, ALU.mult, [k0, k2], [("ov", "act", tb, j)])
            if j == 7:
                for t in range(16):
                    tb, tq = t // 4, t % 4
                    for hf in range(2):
                        pi = ps_rot()
                        for jj in range(8):
                            mm(ps[pi][:, :], actv(tb, jj)[:, tq * 128:(tq + 1) * 128], wdb(jj)[:, hf * 512:(hf + 1) * 512],
                               jj == 0, jj == 7, [("ov", "act", tb, jj), ("S", jj)], [("ps", pi)])
                        act(T[6 + hf], ps[pi][:, :], AF.Identity, [("ps", pi), "G"], [Tk[6 + hf]], scale=Gt[:, t, e_:e_ + 1])
                        tt("gpsimd", acc[:, t, hf * 512:(hf + 1) * 512], acc[:, t, hf * 512:(hf + 1) * 512], T[6 + hf], ALU.add,
                           [Tk[6 + hf]] + ak(t), ak(t))
        fence(ovk, oldk)

    def moe_finish(l, accsrc, dst):
        acc = bufs[accsrc][:, :, :].rearrange("p c t -> p (c t)").rearrange("p (t d) -> p t d", t=16)
        ak = lambda t: [(accsrc, t // 2, 8 * (t % 2) + k_) for k_ in range(8)]
        Xn = bufs[dst]
        for t in range(16):
            xo_i = t % 2
            xo = P[xo_i][:, 0:1024].rearrange("p (c q) -> p c q", c=8)
            dma(xo, xsp_d[:, :, t * 128:(t + 1) * 128], ["xsp"], bk(("P", xo_i)))
            for half in range(2):
                pi = ps_rot()
                for q in range(4):
                    c = half * 4 + q
                    tp(ps[pi][:, q * 128:(q + 1) * 128], acc[:, t, c * 128:(c + 1) * 128], ident, ak(t) + ["cst"], [("ps", pi)])
                for q in range(4):
                    c = half * 4 + q
                    stt(Xn[:, c, t * 128:(t + 1) * 128], ps[pi][:, q * 128:(q + 1) * 128], mv[:, 40 + c:41 + c], xo[:, c, :],
                        ALU.mult, ALU.add, [("ps", pi), "mv"] + bk(("P", xo_i)), tl(dst, [c], t * 128, t * 128 + 128))

    def final_out(src, other):
        X, Y = bufs[src], bufs[other]
        rstd_compute(src, ("P", 0), P[0])
        for c in range(8):
            tmp_i = 1 + c % 2
            tt("vector", P[tmp_i][:, 0:S_LEN], X[:, c, :], P[0][:, 0:S_LEN], ALU.mult, tl(src, [c], 0, S_LEN) + bk(("P", 0)), bk(("P", tmp_i)))
            act(Y[:, c, :], P[tmp_i][:, 0:S_LEN], AF.Identity, bk(("P", tmp_i)) + ["gsp"], tl(other, [c], 0, S_LEN), scale=gsp[:, 8 + c:9 + c])
        for t in range(16):
            stg_i = 3 + t % 2
            stg = P[stg_i]
            for half in range(2):
                pi = ps_rot()
                for q in range(4):
                    c = half * 4 + q
                    tp(ps[pi][:, q * 128:(q + 1) * 128], Y[:, c, t * 128:(t + 1) * 128], ident,
                       tl(other, [c], t * 128, t * 128 + 128) + ["cst"], [("ps", pi)])
                cp("vector" if half == 0 else "scalar", stg[:, half * 512:(half + 1) * 512], ps[pi][:, :], [("ps", pi)], bk(("P", stg_i)))
            dma(out_d[t * 128:(t + 1) * 128, :], stg[:, 0:D], bk(("P", stg_i)), [("out", t)])

    cur, oth = "A", "B"
    load_x(cur)
    for l in range(L):
        layer_prologue(l)
        if stop == "mod":
            break
        rstd_compute(cur, ("P", 0), P[0])
        mix_fence(oth)
        norm_mod(cur, oth, ("P", 0), P[0], 0, 8, bf=True)
        if l == 0:
            dump("h0", bufs[oth][:, :, :], tl(oth, range(8), 0, S_LEN))
        if stop == "h":
            break
        for h in range(4):
            retention_head(l, h, cur, oth)
            if stop == "ret0":
                break
        if stop in ("ret0", "ret"):
            break
        for h in range(6):
            moba_head(l, h, cur, oth)
            if stop == "moba0":
                break
        if stop in ("moba0", "moba"):
            break
        for i in range(3):
            lru_chunk(l, i, cur, oth)
            if stop in ("lru0", "lruA", "lruB", "lruC", "lruD", "lruE"):
                break
        if l == 0:
            dump("xmix0", bufs[cur][:, :, :], tl(cur, range(8), 0, S_LEN))
        if stop in ("lru0", "mix", "lruA", "lruB", "lruC", "lruD", "lruE"):
            break
        rstd_compute(cur, ("P", 0), P[0])
        mix_fence(oth)
        norm_mod(cur, oth, ("P", 0), P[0], 24, 32)
        for c_ in range(8):
            dma(xsp_d[:, c_, :], bufs[cur][:, c_, :], tl(cur, [c_], 0, S_LEN), ["xsp"])
        moe(l, cur, oth)
        moe_finish(l, cur, oth)
        cur, oth = oth, cur
    final_out(cur, oth)
    S.wait_keys("sync", [("out", t) for t in range(16)] + [("dbg", n) for n in dbg_d])

    def replay(name, e):
        for waits, fn, tok in S.streams[name]:
            for s_, v in waits:
                e.wait_ge(sems[s_], v)
            if fn is None:
                continue
            inst = fn(e)
            if tok[0] == name:
                inst.then_inc(sems[name], 1)
            else:
                inst.then_inc(sems[tok[0]], 16)

    with nc.Block() as block:
        @block.sync
        def _(e):
            replay("sync", e)

        @block.scalar
        def _(e):
            replay("scalar", e)

        @block.vector
        def _(e):
            replay("vector", e)

        @block.gpsimd
        def _(e):
            replay("gpsimd", e)

        @block.tensor
        def _(e):
            replay("tensor", e)
    es.close()
    return nc


def _consts():
    cst = np.zeros((128, 6 * 128), np.float32)
    cst[:, 0:128] = np.eye(128, dtype=np.float32)
    cst[:, 128:256] = 1.0
    a = np.zeros((128, 128), np.float32)
    a[0:64, 0:64] = 1.0 / 64
    a[64:128, 64:128] = 1.0 / 64
    cst[:, 256:384] = a
    cst[:, 384:448] = 1.0
    cst[:, 576:640] = 1.0
    p = np.arange(128)[:, None]
    c = np.arange(128)[None, :]
    cst[:, 640:768] = (c >= p).astype(np.float32)
    kind = np.zeros((8, S_LEN), np.float32)
    for n in range(8):
        kind[n, n * 256:(n + 1) * 256] = 1.0
    pos = np.arange(S_LEN, dtype=np.float64)
    half = 32
    inv = 1.0 / (10000.0 ** (np.arange(half, dtype=np.float64) / half))
    ang = pos[None, :] * inv[:, None]
    cos = np.concatenate([np.cos(ang), np.cos(ang)], 0)
    sin = np.concatenate([-np.sin(ang), np.sin(ang)], 0)
    rot = np.zeros((4, 4, 64, S_LEN), np.float64)
    for h in range(4):
        lg = np.log1p(-2.0 ** (-5.0 - h))
        dq = np.exp(lg * pos)[None, :]
        dk = np.exp(-lg * pos)[None, :] * (64 ** -0.5)
        rot[0, h] = cos * dq
        rot[1, h] = sin * dq
        rot[2, h] = cos * dk
        rot[3, h] = sin * dk
    return cst, kind, rot.astype(np.float32)


def _fm(v):
    v = np.asarray(v, np.float32)
    return np.ascontiguousarray(v.reshape(-1, 128).T)


def prepare_inputs(inp):
    cst, kind, rot = _consts()
    sp = np.zeros((NL, 128, SP_N), np.float32)
    wabd = np.zeros((NL, 3, 128, 128), np.float32)
    wxbd = np.zeros((NL, 3, 128, 128), np.float32)
    rbt = np.zeros((NL, 128, NE), np.float32)
    for l in range(NL):
        sp[l, :, SP_ADAB:SP_ADAB + 48] = _fm(inp["ada_b"][l])
        sp[l, :, SP_NW1:SP_NW1 + 8] = _fm(inp["norm_mix_w"][l])
        sp[l, :, SP_NW2:SP_NW2 + 8] = _fm(inp["norm_ffn_w"][l])
        sp[l, 0:64, SP_RNW:SP_RNW + 4] = np.asarray(inp["ret_norm_w"][l], np.float32).reshape(4, 64).T
        for j in range(4):
            sp[l, :, SP_CW + j * 3:SP_CW + j * 3 + 3] = _fm(inp["lru_conv_w"][l, j])
        sp[l, :, SP_CB:SP_CB + 3] = _fm(inp["lru_conv_b"][l])
        sp[l, :, SP_BA:SP_BA + 3] = _fm(inp["lru_gate_a_b"][l])
        sp[l, :, SP_BX:SP_BX + 3] = _fm(inp["lru_gate_x_b"][l])
        sp[l, :, SP_LAM:SP_LAM + 3] = _fm(inp["lru_lambda"][l])
        for e in range(NE):
            sp[l, :, SP_BGU + e * 16:SP_BGU + e * 16 + 16] = _fm(inp["moe_b_gu"][l, e])
        for i in range(3):
            for g in range(2):
                wabd[l, i, g * 64:(g + 1) * 64, g * 64:(g + 1) * 64] = inp["lru_gate_a_w"][l, 2 * i + g]
                wxbd[l, i, g * 64:(g + 1) * 64, g * 64:(g + 1) * 64] = inp["lru_gate_x_w"][l, 2 * i + g]
        rbt[l] = np.broadcast_to(np.asarray(inp["router_b"][l], np.float32)[None, :], (128, NE))
    shared = {
        "sp": sp, "rbt": rbt, "bdn": np.ascontiguousarray(inp["moe_b_down"], dtype=np.float32),
        "ada_w": np.ascontiguousarray(inp["ada_w"], dtype=np.float32),
        "w_in": np.ascontiguousarray(inp["w_in"], dtype=np.float32),
        "w_out": np.ascontiguousarray(inp["w_out"], dtype=np.float32),
        "router_w": np.ascontiguousarray(inp["router_w"], dtype=np.float32),
        "moe_w_gu": np.ascontiguousarray(inp["moe_w_gu"], dtype=np.float32),
        "moe_w_down": np.ascontiguousarray(inp["moe_w_down"], dtype=np.float32),
        "wabd": wabd, "wxbd": wxbd, "cst": cst, "kind": kind, "rot": rot,
    }
    maps = []
    for b in range(inp["x"].shape[0]):
        gsp = np.zeros((128, 16), np.float32)
        gsp[:, 0:8] = _fm(inp["c"][b])
        gsp[:, 8:16] = _fm(inp["final_norm_w"])
        m = dict(shared)
        m["x"] = np.ascontiguousarray(inp["x"][b], dtype=np.float32)
        m["gsp"] = gsp
        maps.append(m)
    return maps


def kernel(**inputs):
    inp = {k: np.asarray(v) for k, v in inputs.items()}
    maps = prepare_inputs(inp)
    nc = build_program()
    res = run_bass_kernel_spmd(nc, maps, core_ids=list(range(len(maps))))
    out = np.stack([np.asarray(r["out"], dtype=np.float32) for r in res.results], axis=0)
    return out
```

```python
import numpy as np
from contextlib import ExitStack
import concourse.bass as bass
import concourse.mybir as mybir
from concourse.bass_utils import run_bass_kernel_spmd

F32 = mybir.dt.float32
F32R = mybir.dt.float32r
BF16 = mybir.dt.bfloat16
AF = mybir.ActivationFunctionType
ALU = mybir.AluOpType
AX = mybir.AxisListType

D = 1024
S_LEN = 2048
NL = 2
NE = 32
IN_W = 2944
EPS = 1e-6
NEG = -32768.0
NDMA = 24
SAME_ENGINE_SYNC = True
ENGS = ["sync", "scalar", "vector", "gpsimd", "tensor"]

SP_ADAB = 0
SP_NW1 = 48
SP_NW2 = 56
SP_RNW = 64
SP_CW = 68
SP_CB = 80
SP_BA = 83
SP_BX = 86
SP_LAM = 89
SP_BGU = 92
SP_N = 92 + 512


class Sched:
    def __init__(self):
        self.streams = {e: [] for e in ENGS}
        self.cnt = {e: 0 for e in ENGS}
        self.known = {e: {} for e in ENGS}
        self.lastw = {}
        self.rd = {}
        self.ndma = 0
        self.dma_val = [0] * NDMA

    def _deps(self, reads, writes):
        toks = []
        for k in reads:
            t = self.lastw.get(k)
            if t is not None:
                toks.append(t)
        for k in writes:
            t = self.lastw.get(k)
            if t is not None:
                toks.append(t)
            for s, v in self.rd.get(k, {}).items():
                toks.append((s, v))
        return toks

    def _commit(self, tok, reads, writes):
        s, v = tok
        for k in reads:
            d = self.rd.setdefault(k, {})
            if d.get(s, 0) < v:
                d[s] = v
        for k in writes:
            self.lastw[k] = tok
            self.rd[k] = {}

    def _filter(self, eng, toks):
        best = {}
        for s, v in toks:
            if s == eng and (eng == "tensor" or not SAME_ENGINE_SYNC):
                continue
            if v > best.get(s, 0):
                best[s] = v
        out = []
        kn = self.known[eng]
        for s, v in best.items():
            if kn.get(s, 0) >= v:
                continue
            kn[s] = v
            out.append((s, v))
        return out

    def op(self, eng, fn, reads=(), writes=()):
        toks = self._deps(reads, writes)
        waits = self._filter(eng, toks)
        self.cnt[eng] += 1
        tok = (eng, self.cnt[eng])
        self.streams[eng].append((waits, fn, tok))
        self._commit(tok, reads, writes)

    def dma(self, eng, fn, reads=(), writes=()):
        toks = self._deps(reads, writes)
        i = self.ndma % NDMA
        self.ndma += 1
        if self.dma_val[i] > 0:
            toks.append((("dma", i), self.dma_val[i]))
        self.dma_val[i] += 16
        tok = (("dma", i), self.dma_val[i])
        waits = self._filter(eng, toks)
        self.streams[eng].append((waits, fn, tok))
        self._commit(tok, reads, writes)

    def wait_keys(self, eng, keys):
        toks = self._deps(keys, keys)
        waits = self._filter(eng, toks)
        self.streams[eng].append((waits, None, None))


def tl(name, cs, t0, t1):
    return [(name, c, t) for c in cs for t in range(t0 // 128, (t1 + 127) // 128)]


def bk(name, t0=0, t1=S_LEN):
    return [(name, b) for b in range(t0 // 512, (t1 + 511) // 512)]


def build_program(L=NL, dbg=(), stop=None):
    nc = bass.Bass("TRN2", target_bir_lowering=False)
    S = Sched()
    dt = lambda name, shape, kind="ExternalInput": nc.dram_tensor(name, shape, F32, kind=kind).ap()
    x_d = dt("x", [S_LEN, D])
    sp_d = dt("sp", [NL, 128, SP_N])
    gsp_d = dt("gsp", [128, 16])
    rbt_d = dt("rbt", [NL, 128, NE])
    bdn_d = dt("bdn", [NL, NE, D])
    adaw_d = dt("ada_w", [NL, D, 6 * D])
    win_d = dt("w_in", [NL, D, IN_W])
    wout_d = dt("w_out", [NL, D, D])
    rw_d = dt("router_w", [NL, D, NE])
    if stop is None:
        wgu_d = dt("moe_w_gu", [NL, NE, D, 2 * D])
        wdn_d = dt("moe_w_down", [NL, NE, D, D])
    wabd_d = dt("wabd", [NL, 3, 128, 128])
    wxbd_d = dt("wxbd", [NL, 3, 128, 128])
    cst_d = dt("cst", [128, 6 * 128])
    kind_d = dt("kind", [8, S_LEN])
    rot_d = dt("rot", [4, 4, 64, S_LEN])
    out_d = dt("out", [S_LEN, D], kind="ExternalOutput")
    xsp_d = dt("xsp", [128, 8, S_LEN], kind="Internal")
    dbg_d = {}
    for name, shape in dbg:
        dbg_d[name] = dt("dbg_" + name, list(shape), kind="ExternalOutput")

    es = ExitStack()
    sb = lambda name, shape: es.enter_context(nc.sbuf_tensor("sb_" + name, shape, F32))
    bufA = sb("bufA", [128, 8, S_LEN])
    bufB = sb("bufB", [128, 8, S_LEN])
    cst = sb("cst", [128, 6 * 128])
    sp = sb("sp", [128, SP_N])
    gsp = sb("gsp", [128, 16])
    rbt = sb("rbt", [128, NE])
    P = [sb(f"P{i}", [128, 2052]) for i in range(5)]
    St = [sb(f"S{i}", [128, 512]) for i in range(8)]
    wbuf = [sb(f"wb{i}", [128, 8, 128]) for i in range(2)]
    wo = sb("wo", [128, D])
    Gt = sb("G", [128, 16, NE])
    mv = sb("mv", [128, 64])
    fz = sb("fz", [128, 8])
    ps = [es.enter_context(nc.psum_tensor(f"ps{i}", [128, 512], F32)) for i in range(8)]
    sems = {}
    for e in ENGS[1:]:
        sems[e] = es.enter_context(nc.semaphore("sem_" + e))
    for i in range(NDMA):
        sems[("dma", i)] = es.enter_context(nc.semaphore(f"sem_dma{i}"))

    ident = cst[:, 0:128]
    ones = cst[:, 128:256]
    avg64 = cst[:, 256:384]
    tri = cst[:, 640:768]

    rot_i = [0]
    acc_i = [0]

    def ps_rot():
        i = rot_i[0] % 4
        rot_i[0] += 1
        return i

    def ps_acc():
        i = 4 + acc_i[0] % 4
        acc_i[0] += 1
        return i

    def dma(out, in_, R, W, eng="sync"):
        S.dma(eng, lambda e: e.dma_start(out=out, in_=in_), R, W)

    def mm(out, lhsT, rhs, start, stop, R, W, r32=False):
        if r32:
            lhsT, rhs = lhsT.bitcast(F32R), rhs.bitcast(F32R)
        S.op("tensor", lambda e: e.matmul(out, lhsT, rhs, start=start, stop=stop), R, W)

    def tp(out, in_, idn, R, W):
        S.op("tensor", lambda e: e.transpose(out, in_, idn), R, W)

    def act(out, in_, func, R, W, bias=None, scale=None):
        kw = {}
        if bias is not None:
            kw["bias"] = bias
        if scale is not None:
            kw["scale"] = scale
        S.op("scalar", lambda e: e.activation(out, in_, func, **kw), R, W)

    def tt(eng, out, in0, in1, op, R, W):
        S.op(eng, lambda e: e.tensor_tensor(out, in0, in1, op), R, W)

    def ts(eng, out, in0, s1, s2, op0, op1, R, W):
        if op1 is None:
            S.op(eng, lambda e: e.tensor_scalar(out, in0, s1, None, op0), R, W)
        else:
            S.op(eng, lambda e: e.tensor_scalar(out, in0, s1, s2, op0, op1), R, W)

    def stt(out, in0, sc, in1, op0, op1, R, W):
        S.op("vector", lambda e: e.scalar_tensor_tensor(out, in0, sc, in1, op0, op1), R, W)

    def cp(eng, out, in_, R, W):
        if eng == "scalar":
            S.op(eng, lambda e: e.activation(out, in_, AF.Identity), R, W)
        else:
            S.op(eng, lambda e: e.tensor_copy(out, in_), R, W)

    def recip(out, in_, R, W):
        S.op("vector", lambda e: e.reciprocal(out, in_), R, W)

    def memset(eng, ap, val, W):
        S.op(eng, lambda e: e.memset(ap, val), (), W)

    def dump(name, src, R):
        if name in dbg_d:
            dma(dbg_d[name], src, R, [("dbg", name)])

    dma(cst[:], cst_d, [], ["cst"])
    dma(gsp[:], gsp_d, [], ["gsp"])

    bufs = {"A": bufA, "B": bufB}

    def bfl(name):
        return bufs[name][:, :, :].rearrange("p c t -> p (c t)")

    def hbv(name, c):
        return bfl(name)[:, 0:8192].bitcast(BF16)[:, c * 2048:(c + 1) * 2048]

    def wbb(name, i):
        return bfl(name)[:, 8192 + i * 512:8192 + (i + 1) * 512].bitcast(BF16).rearrange("p (k n) -> p k n", k=8)

    def wob(name):
        return bfl(name)[:, 9216:9728].bitcast(BF16)

    def mix_fence(name):
        S.op("gpsimd", lambda e: e.memset(fz[:, 1:2], 0.0), (),
             tl(name, range(8), 0, S_LEN) + [("wbb", 0), ("wbb", 1), "wob", "fz"])

    def load_x(dst):
        X = bufs[dst]
        for tt_ in range(16):
            stg = P[tt_ % 2]
            dma(stg[:, 0:D], x_d[tt_ * 128:(tt_ + 1) * 128, :], [], [("P", tt_ % 2)])
            for half in range(2):
                pi = ps_rot()
                for q in range(4):
                    c = half * 4 + q
                    tp(ps[pi][:, q * 128:(q + 1) * 128], stg[:, c * 128:(c + 1) * 128], ident,
                       [("P", tt_ % 2), "cst"], [("ps", pi)])
                cp("vector" if half == 0 else "scalar",
                   X[:, half * 4:half * 4 + 4, tt_ * 128:(tt_ + 1) * 128],
                   ps[pi][:, :].rearrange("p (q t) -> p q t", q=4),
                   [("ps", pi)], tl(dst, range(half * 4, half * 4 + 4), tt_ * 128, tt_ * 128 + 128))

    def rstd_compute(src, rbuf_key, rbuf):
        X = bufs[src]
        for b in range(4):
            pi = ps_acc()
            for c in range(8):
                sq = St[c % 4]
                act(sq[:, :], X[:, c, b * 512:(b + 1) * 512], AF.Square,
                    tl(src, [c], b * 512, b * 512 + 512), [("S", c % 4)])
                mm(ps[pi][:, :], ones, sq[:, :], c == 0, c == 7, [("S", c % 4), "cst"], [("ps", pi)])
            act(rbuf[:, b * 512:(b + 1) * 512], ps[pi][:, :], AF.Sqrt, [("ps", pi), "mv"], [(rbuf_key, b)],
                bias=mv[:, 63:64], scale=1.0 / D)
            recip(rbuf[:, b * 512:(b + 1) * 512], rbuf[:, b * 512:(b + 1) * 512], [(rbuf_key, b)], [(rbuf_key, b)])

    def norm_mod(src, dst, rbuf_key, rbuf, acol, bcol, bf=False):
        X, H = bufs[src], bufs[dst]
        for c in range(8):
            tmp_i = 1 + c % 2
            tmp = P[tmp_i]
            tt("vector", tmp[:, 0:S_LEN], X[:, c, :], rbuf[:, 0:S_LEN], ALU.mult,
               tl(src, [c], 0, S_LEN) + bk(rbuf_key), bk(("P", tmp_i)))
            act(hbv(dst, c) if bf else H[:, c, :], tmp[:, 0:S_LEN], AF.Identity, bk(("P", tmp_i)) + ["mv"], tl(dst, [c], 0, S_LEN),
                bias=mv[:, bcol + c:bcol + c + 1], scale=mv[:, acol + c:acol + c + 1])

    memset("vector", mv[:, :], 0.0, ["mv"])
    memset("vector", mv[:, 63:64], EPS, ["mv"])
    for i_ in range(5):
        memset("gpsimd" if i_ % 2 else "vector", P[i_][:, :], 0.0, bk(("P", i_)))
    for i_ in range(8):
        memset("gpsimd" if i_ % 2 else "vector", St[i_][:, :], 0.0, [("S", i_)])

    def layer_prologue(l):
        dma(sp[:], sp_d[l], [], ["sp"])
        dma(rbt[:], rbt_d[l], [], ["rbt"])
        cact = St[7]
        act(cact[:, 0:8], gsp[:, 0:8], AF.Silu, ["gsp"], [("S", 7)])
        pm = ps_acc()
        aw = adaw_d[l].rearrange("(kc p) n -> p kc n", p=128)
        for blk in range(24):
            wtile = P[3 + blk % 2]
            wv = wtile[:, 0:2048].rearrange("p (k n) -> p k n", k=8)
            dma(wv, aw[:, :, blk * 256:(blk + 1) * 256], [], bk(("P", 3 + blk % 2)))
            for jj in range(2):
                j = blk * 2 + jj
                for kc in range(8):
                    mm(ps[pm][:, j:j + 1], wv[:, kc, jj * 128:(jj + 1) * 128], cact[:, kc:kc + 1], kc == 0, kc == 7,
                       bk(("P", 3 + blk % 2)) + [("S", 7)], [("ps", pm)], r32=False)
        modt = St[6]
        tt("vector", modt[:, 0:48], ps[pm][:, 0:48], sp[:, SP_ADAB:SP_ADAB + 48], ALU.add, [("ps", pm), "sp"], [("S", 6)])
        ts("vector", mv[:, 0:8], modt[:, 8:16], 1.0, None, ALU.add, None, [("S", 6)], ["mv"])
        tt("vector", mv[:, 0:8], mv[:, 0:8], sp[:, SP_NW1:SP_NW1 + 8], ALU.mult, ["mv", "sp"], ["mv"])
        cp("vector", mv[:, 8:16], modt[:, 0:8], [("S", 6)], ["mv"])
        cp("vector", mv[:, 16:24], modt[:, 16:24], [("S", 6)], ["mv"])
        ts("vector", mv[:, 24:32], modt[:, 32:40], 1.0, None, ALU.add, None, [("S", 6)], ["mv"])
        tt("vector", mv[:, 24:32], mv[:, 24:32], sp[:, SP_NW2:SP_NW2 + 8], ALU.mult, ["mv", "sp"], ["mv"])
        cp("vector", mv[:, 32:40], modt[:, 24:32], [("S", 6)], ["mv"])
        cp("vector", mv[:, 40:48], modt[:, 40:48], [("S", 6)], ["mv"])
        act(mv[:, 54:57], sp[:, SP_LAM:SP_LAM + 3], AF.Exp, ["sp"], ["mv"], scale=-1.0)
        act(mv[:, 54:57], mv[:, 54:57], AF.Ln, ["mv"], ["mv"], bias=1.0)
        ts("vector", mv[:, 48:51], mv[:, 54:57], -8.0, None, ALU.mult, None, ["mv"], ["mv"])
        ts("vector", mv[:, 51:54], mv[:, 54:57], -16.0, None, ALU.mult, None, ["mv"], ["mv"])
        if l == 0:
            dump("mod0", modt[:, 0:48], [("S", 6)])

    wb_i = [0]

    def load_wcols(l, col_pieces, hsrc):
        i = wb_i[0] % 2
        wb_i[0] += 1
        wv = win_d[l].rearrange("(kc p) n -> p kc n", p=128)
        o = 0
        for c0, n in col_pieces:
            dma(wbuf[i][:, :, o:o + n], wv[:, :, c0:c0 + n], [], [("wb", i)])
            o += n
        cp("gpsimd", wbb(hsrc, i)[:, :, 0:o], wbuf[i][:, :, 0:o], [("wb", i)], [("wbb", i)])
        return i, o

    def proj_fm(l, hsrc, col_pieces, evac):
        i, M = load_wcols(l, col_pieces, hsrc)
        for b in range(4):
            pi = ps_rot()
            for kc in range(8):
                mm(ps[pi][0:M, :], wbb(hsrc, i)[:, kc, 0:M], hbv(hsrc, kc)[:, b * 512:(b + 1) * 512], kc == 0, kc == 7,
                   [("wbb", i)] + tl(hsrc, [kc], b * 512, b * 512 + 512), [("ps", pi)])
            evac(b, pi, M)

    def proj_tm(l, hsrc, col0, n, evac):
        i, M = load_wcols(l, [(col0, n)], hsrc)
        for t in range(16):
            pi = ps_rot()
            for kc in range(8):
                mm(ps[pi][:, 0:n], hbv(hsrc, kc)[:, t * 128:(t + 1) * 128], wbb(hsrc, i)[:, kc, 0:n], kc == 0, kc == 7,
                   [("wbb", i)] + tl(hsrc, [kc], t * 128, t * 128 + 128), [("ps", pi)])
            evac(t, pi)

    def wout_partial(l, xdst, row0, nrows, ysrc_ap, ykeys_fn, hsrc):
        X = bufs[xdst]
        dma(wo[0:nrows, :], wout_d[l, row0:row0 + nrows, :], [], ["wo"])
        cp("gpsimd", wob(hsrc)[0:nrows, :], wo[0:nrows, :], ["wo"], ["wob"])
        for b in range(4):
            for dc in range(8):
                pi = ps_rot()
                mm(ps[pi][:, :], wob(hsrc)[0:nrows, dc * 128:(dc + 1) * 128], ysrc_ap(b), True, True,
                   ["wob"] + ykeys_fn(b), [("ps", pi)])
                k = tl(xdst, [dc], b * 512, b * 512 + 512)
                if dc % 2 == 0:
                    stt(X[:, dc, b * 512:(b + 1) * 512], ps[pi][:, :], mv[:, 16 + dc:17 + dc], X[:, dc, b * 512:(b + 1) * 512],
                        ALU.mult, ALU.add, [("ps", pi), "mv"] + k, k)
                else:
                    ti = 6 + (dc // 2) % 2
                    act(St[ti][:, :], ps[pi][:, :], AF.Identity, [("ps", pi), "mv"], [("S", ti)], scale=mv[:, 16 + dc:17 + dc])
                    tt("gpsimd", X[:, dc, b * 512:(b + 1) * 512], X[:, dc, b * 512:(b + 1) * 512], St[ti][:, :], ALU.add,
                       [("S", ti)] + k, k)

    def retention_head(l, h, xsrc, hsrc):
        qb_, kb_, vb_, gb_ = P[0], P[1], P[2], P[3]
        c0 = h * 64

        def rot_evac(dst, dkey, tab):
            store = {}

            def ev_a(b, pi, M):
                store[b] = pi
            return store, ev_a

        for (dst, dkey, base, tab) in ((qb_, ("P", 0), 0, 0), (kb_, ("P", 1), 256, 2)):
            ia, _ = load_wcols(l, [(base + c0, 64)], hsrc)
            ib_, _ = load_wcols(l, [(base + c0 + 32, 32), (base + c0, 32)], hsrc)
            for b in range(4):
                dma(St[0][0:64, :], rot_d[tab, h, :, b * 512:(b + 1) * 512], [], [("S", 0)])
                dma(St[1][0:64, :], rot_d[tab + 1, h, :, b * 512:(b + 1) * 512], [], [("S", 1)])
                pa, pb = ps_rot(), ps_rot()
                for (pi, wi) in ((pa, ia), (pb, ib_)):
                    for kc in range(8):
                        mm(ps[pi][0:64, :], wbb(hsrc, wi)[:, kc, 0:64], hbv(hsrc, kc)[:, b * 512:(b + 1) * 512], kc == 0, kc == 7,
                           [("wbb", wi)] + tl(hsrc, [kc], b * 512, b * 512 + 512), [("ps", pi)])
                tt("vector", St[2][0:64, :], ps[pa][0:64, :], St[0][0:64, :], ALU.mult, [("ps", pa), ("S", 0)], [("S", 2)])
                tt("vector", St[3][0:64, :], ps[pb][0:64, :], St[1][0:64, :], ALU.mult, [("ps", pb), ("S", 1)], [("S", 3)])
                tt("gpsimd", dst[0:64, 0:1024].bitcast(BF16)[:, b * 512:(b + 1) * 512], St[2][0:64, :], St[3][0:64, :], ALU.add,
                   [("S", 2), ("S", 3)], [(dkey, 0), (dkey, 1)])
        qbv = qb_[0:64, 0:1024].bitcast(BF16)
        kbv = kb_[0:64, 0:1024].bitcast(BF16)
        yb = qb_[0:64, 1024:2048].bitcast(BF16)
        QK = [(("P", 0), 0), (("P", 0), 1)]
        KK = [(("P", 1), 0), (("P", 1), 1)]
        YK = [(("P", 0), 2), (("P", 0), 3)]
        vv = vb_[:, 0:512].bitcast(BF16).rearrange("p (t e) -> p t e", t=16)

        def ev_v(t, pi):
            cp("scalar", vv[:, t, :], ps[pi][:, 0:64], [("ps", pi)], [(("P", 2), 0)])
        proj_tm(l, hsrc, 512 + c0, 64, ev_v)

        def ev_g(b, pi, M):
            act(gb_[0:64, b * 512:(b + 1) * 512], ps[pi][0:64, :], AF.Silu, [("ps", pi)], [(("P", 3), b)])
        proj_fm(l, hsrc, [(768 + c0, 64)], ev_g)
        if l == 0 and h == 0:
            dump("qrot0", qb_[0:64, 0:S_LEN], bk(("P", 0)))
            dump("krot0", kb_[0:64, 0:S_LEN], bk(("P", 1)))
        for ib in range(4):
            po = ps_acc()
            njt = ib * 4 + 4
            def score(jt, ib=ib):
                d = jt - 4 * ib
                c_lo = max(d, 0) * 128
                pi = ps_rot()
                mm(ps[pi][:, c_lo:512], kbv[:, jt * 128:(jt + 1) * 128], qbv[:, ib * 512 + c_lo:(ib + 1) * 512], True, True,
                   KK + QK, [("ps", pi)])
                si = 2 + jt % 4
                sT = St[si][:, 0:256].bitcast(BF16)
                if d >= 0:
                    tt("vector", sT[:, c_lo:c_lo + 128], ps[pi][:, c_lo:c_lo + 128], tri, ALU.mult, [("ps", pi), "cst"], [("S", si)])
                    if c_lo + 128 < 512:
                        cp("scalar", sT[:, c_lo + 128:512], ps[pi][:, c_lo + 128:512], [("ps", pi)], [("S", si)])
                else:
                    cp("scalar" if jt % 2 else "vector", sT[:, :], ps[pi][:, :], [("ps", pi)], [("S", si)])
                return si, c_lo
            pend = score(0)
            for jt in range(njt):
                nxt = score(jt + 1) if jt + 1 < njt else None
                si, c_lo = pend
                mm(ps[po][0:64, c_lo:512], vv[:, jt, :], St[si][:, 0:256].bitcast(BF16)[:, c_lo:512], jt == 0, jt == njt - 1,
                   [(("P", 2), 0), ("S", si)], [("ps", po)])
                pend = nxt
            y = St[6]
            cp("vector", y[0:64, :], ps[po][0:64, :], [("ps", po)], [("S", 6)])
            pm = ps_rot()
            mm(ps[pm][0:64, :], avg64[0:64, 0:64], y[0:64, :], True, True, [("S", 6), "cst"], [("ps", pm)])
            yc = St[7]
            tt("vector", yc[0:64, :], y[0:64, :], ps[pm][0:64, :], ALU.subtract, [("S", 6), ("ps", pm)], [("S", 7)])
            act(y[0:64, :], yc[0:64, :], AF.Square, [("S", 7)], [("S", 6)])
            pv = ps_rot()
            mm(ps[pv][0:64, :], avg64[0:64, 0:64], y[0:64, :], True, True, [("S", 6), "cst"], [("ps", pv)])
            act(y[0:64, :], ps[pv][0:64, :], AF.Sqrt, [("ps", pv), "mv"], [("S", 6)], bias=mv[0:64, 63:64], scale=1.0)
            recip(y[0:64, :], y[0:64, :], [("S", 6)], [("S", 6)])
            tt("vector", yc[0:64, :], yc[0:64, :], y[0:64, :], ALU.mult, [("S", 6), ("S", 7)], [("S", 7)])
            stt(yb[:, ib * 512:(ib + 1) * 512], yc[0:64, :], sp[0:64, SP_RNW + h:SP_RNW + h + 1],
                gb_[0:64, ib * 512:(ib + 1) * 512], ALU.mult, ALU.mult,
                [("S", 7), "sp", (("P", 3), ib)], YK)
        if l == 0 and h == 0:
            dump("yret0", qb_[0:64, 0:S_LEN], bk(("P", 0)))
        wout_partial(l, xsrc, c0, 64, lambda b: yb[:, b * 512:(b + 1) * 512], lambda b: YK, hsrc)

    def moba_head(l, h, xsrc, hsrc):
        qa_, ka_, vb_, bp_ = P[0], P[1], P[2], P[3]
        c0 = h * 64
        dma(ka_[64:72, 0:S_LEN], kind_d, [], bk(("P", 1)))

        def ev_q(b, pi, M):
            act(qa_[0:64, b * 512:(b + 1) * 512], ps[pi][0:64, :], AF.Identity, [("ps", pi)], [(("P", 0), b)], scale=0.125)
        proj_fm(l, hsrc, [(1024 + c0, 64)], ev_q)

        def ev_k(b, pi, M):
            cp("vector", ka_[0:64, b * 512:(b + 1) * 512], ps[pi][0:64, :], [("ps", pi)], [(("P", 1), b)])
        proj_fm(l, hsrc, [(1408 + c0, 64)], ev_k)
        vv = vb_[:, 0:2048].rearrange("p (t e) -> p t e", t=16)
        memset("gpsimd", vv[:, :, 64:128], 1.0, bk(("P", 2)))

        def ev_v(t, pi):
            cp("scalar", vv[:, t, 0:64], ps[pi][:, 0:64], [("ps", pi)], [(("P", 2), t // 4)])
        proj_tm(l, hsrc, 1792 + c0, 64, ev_v)
        km = St[6]
        S.op("vector", lambda e: e.tensor_reduce(km[0:64, 0:8], ka_[0:64, 0:S_LEN].rearrange("p (n k) -> p n k", n=8), AX.X, ALU.add),
             bk(("P", 1)), [("S", 6)])
        bpv = bp_[:, 0:16 * 72].rearrange("p (t c) -> p t c", t=16)
        memset("gpsimd", bpv[:, :, 0:64], 0.0, bk(("P", 3)))
        memset("gpsimd", bpv[:, :, 64:72], NEG, bk(("P", 3)))
        gsb = St[7]
        for t in range(16):
            own = t // 2
            if own <= 3:
                if own > 0:
                    memset("gpsimd", bpv[:, t, 64:64 + own], 0.0, bk(("P", 3)))
            else:
                pi = ps_rot()
                mm(ps[pi][:, 0:8], qa_[0:64, t * 128:(t + 1) * 128], km[0:64, 0:8], True, True,
                   [(("P", 0), t // 4), ("S", 6)], [("ps", pi)], r32=False)
                g8 = gsb[:, t * 16:t * 16 + 8]
                m8 = gsb[:, t * 16 + 8:t * 16 + 16]
                memset("vector", g8, -1e30, [("S", 7)])
                cp("vector", gsb[:, t * 16:t * 16 + own], ps[pi][:, 0:own], [("ps", pi)], [("S", 7)])
                S.op("vector", lambda e, m8=m8, g8=g8: e.max(m8, g8), [("S", 7)], [("S", 7)])
                ts("vector", g8[:, 0:own], g8[:, 0:own], m8[:, 2:3], None, ALU.is_ge, None, [("S", 7)], [("S", 7)])
                ts("vector", bpv[:, t, 64:64 + own], g8[:, 0:own], -NEG, NEG, ALU.mult, ALU.add, [("S", 7)], bk(("P", 3)))
            memset("gpsimd", bpv[:, t, 64 + own:65 + own], 0.0, bk(("P", 3)))
        for t in range(16):
            pi = ps_rot()
            mm(ps[pi][0:72, 0:128], bpv[:, t, :], ident, True, True, bk(("P", 3)) + ["cst"], [("ps", pi)])
            cp("vector", qa_[64:72, t * 128:(t + 1) * 128], ps[pi][64:72, 0:128], [("ps", pi)], [(("P", 0), t // 4)])
        if l == 0 and h == 0:
            dump("mqaug0", qa_[0:72, 0:S_LEN], bk(("P", 0)))
        for ib in range(4):
            po = ps_acc()
            njt = ib * 4 + 4
            def score(jt, ib=ib):
                d = jt - 4 * ib
                c_lo = max(d, 0) * 128
                pi = ps_rot()
                mm(ps[pi][:, c_lo:512], ka_[0:72, jt * 128:(jt + 1) * 128], qa_[0:72, ib * 512 + c_lo:(ib + 1) * 512], True, True,
                   [(("P", 1), jt // 4), (("P", 0), ib)], [("ps", pi)])
                si = 2 + jt % 4
                eT = St[si]
                act(eT[:, c_lo:512], ps[pi][:, c_lo:512], AF.Exp, [("ps", pi)], [("S", si)])
                if d >= 0:
                    tt("gpsimd", eT[:, c_lo:c_lo + 128], eT[:, c_lo:c_lo + 128], tri, ALU.mult, [("S", si), "cst"], [("S", si)])
                return si, c_lo
            pend = score(0)
            for jt in range(njt):
                nxt = score(jt + 1) if jt + 1 < njt else None
                si, c_lo = pend
                mm(ps[po][:, c_lo:512], vv[:, jt, :], St[si][:, c_lo:512], jt == 0, jt == njt - 1,
                   [(("P", 2), jt // 4), ("S", si)], [("ps", po)])
                pend = nxt
            rd = St[6]
            memset("gpsimd", rd[0:64, :], 0.0, [("S", 6)])
            recip(rd[64:128, :], ps[po][64:128, :], [("ps", po)], [("S", 6)])
            pm = ps_rot()
            mm(ps[pm][0:64, :], ident[:, 64:128], rd[:, :], True, True, [("S", 6), "cst"], [("ps", pm)], r32=False)
            yn = St[7]
            cp("scalar", yn[0:64, :], ps[pm][0:64, :], [("ps", pm)], [("S", 7)])
            tt("vector", qa_[0:64, ib * 512:ib * 512 + 256].bitcast(BF16), ps[po][0:64, :], yn[0:64, :], ALU.mult,
               [("ps", po), ("S", 7)], [(("P", 0), ib)])
        if l == 0 and h == 0:
            dump("ymoba0", qa_[0:64, 0:S_LEN], bk(("P", 0)))
        wout_partial(l, xsrc, 256 + c0, 64, lambda b: qa_[0:64, b * 512:b * 512 + 256].bitcast(BF16), lambda b: [(("P", 0), b)], hsrc)

    def lru_chunk(l, i, xsrc, hsrc):
        lx, ub, rb_, ib_, gb_ = P[0], P[1], P[2], P[3], P[4]
        c0 = i * 128
        memset("gpsimd", lx[:, 0:3], 0.0, [(("P", 0), 0)])

        def ev_x(b, pi, M):
            cp("vector", lx[:, 3 + b * 512:3 + (b + 1) * 512], ps[pi][:, :], [("ps", pi)], [(("P", 0), b), (("P", 0), min(b + 1, 3))])
        proj_fm(l, hsrc, [(2176 + c0, 128)], ev_x)
        allx = bk(("P", 0))
        cw = lambda j: sp[:, SP_CW + j * 3 + i:SP_CW + j * 3 + i + 1]
        ts("vector", ub[:, 0:S_LEN], lx[:, 0:S_LEN], cw(0), sp[:, SP_CB + i:SP_CB + i + 1], ALU.mult, ALU.add,
           allx + ["sp"], bk(("P", 1)))
        for j in range(1, 4):
            stt(ub[:, 0:S_LEN], lx[:, j:j + S_LEN], cw(j), ub[:, 0:S_LEN], ALU.mult, ALU.add, allx + ["sp"] + bk(("P", 1)), bk(("P", 1)))
        if stop == "lruA":
            dump("ylru0", ub[:, 0:S_LEN], bk(("P", 1)))
            return
        dma(wo[:, 0:128], wabd_d[l, i], [], ["wo"])
        dma(wo[:, 128:256], wxbd_d[l, i], [], ["wo"])
        for b in range(4):
            pa, px = ps_rot(), ps_rot()
            mm(ps[pa][:, :], wo[:, 0:128], ub[:, b * 512:(b + 1) * 512], True, True, ["wo", (("P", 1), b)], [("ps", pa)])
            mm(ps[px][:, :], wo[:, 128:256], ub[:, b * 512:(b + 1) * 512], True, True, ["wo", (("P", 1), b)], [("ps", px)])
            act(rb_[:, b * 512:(b + 1) * 512], ps[pa][:, :], AF.Sigmoid, [("ps", pa), "sp"], [(("P", 2), b)],
                bias=sp[:, SP_BA + i:SP_BA + i + 1], scale=1.0)
            act(ib_[:, b * 512:(b + 1) * 512], ps[px][:, :], AF.Sigmoid, [("ps", px), "sp"], [(("P", 3), b)],
                bias=sp[:, SP_BX + i:SP_BX + i + 1], scale=1.0)
        if stop == "lruB":
            dump("ylru0", rb_[:, 0:S_LEN], bk(("P", 2)))
            return
        tt("gpsimd", ib_[:, 0:S_LEN], ib_[:, 0:S_LEN], ub[:, 0:S_LEN], ALU.mult, bk(("P", 3)) + bk(("P", 1)), bk(("P", 3)))
        act(ub[:, 0:S_LEN], rb_[:, 0:S_LEN], AF.Exp, bk(("P", 2)) + ["mv"], bk(("P", 1)), scale=mv[:, 51 + i:52 + i])
        act(ub[:, 0:S_LEN], ub[:, 0:S_LEN], AF.Sqrt, bk(("P", 1)), bk(("P", 1)), bias=1.0, scale=-1.0)
        tt("gpsimd", ib_[:, 0:S_LEN], ib_[:, 0:S_LEN], ub[:, 0:S_LEN], ALU.mult, bk(("P", 3)) + bk(("P", 1)), bk(("P", 3)))
        act(rb_[:, 0:S_LEN], rb_[:, 0:S_LEN], AF.Exp, bk(("P", 2)) + ["mv"], bk(("P", 2)), scale=mv[:, 48 + i:49 + i])
        S.op("vector", lambda e: e.tensor_tensor_scan(ub[:, 0:S_LEN], rb_[:, 0:S_LEN], ib_[:, 0:S_LEN], 0.0, ALU.mult, ALU.add),
             bk(("P", 2)) + bk(("P", 3)), bk(("P", 1)))
        if stop == "lruC":
            dump("ylru0", ub[:, 0:S_LEN], bk(("P", 1)))
            return

        def ev_g(b, pi, M):
            sl = slice(b * 512, (b + 1) * 512)
            cp("vector", gb_[:, sl], ps[pi][:, :], [("ps", pi)], [(("P", 4), b)])
            act(rb_[:, sl], gb_[:, sl], AF.Square, [(("P", 4), b)], [(("P", 2), b)])
        proj_fm(l, hsrc, [(2560 + c0, 128)], ev_g)
        if stop == "lruD":
            dump("ylru0", gb_[:, 0:S_LEN], bk(("P", 4)))
            return
        ts("vector", rb_[:, 0:S_LEN], rb_[:, 0:S_LEN], 0.044715, 1.0, ALU.mult, ALU.add, bk(("P", 2)), bk(("P", 2)))
        tt("gpsimd", rb_[:, 0:S_LEN], rb_[:, 0:S_LEN], gb_[:, 0:S_LEN], ALU.mult, bk(("P", 2)) + bk(("P", 4)), bk(("P", 2)))
        act(rb_[:, 0:S_LEN], rb_[:, 0:S_LEN], AF.Sigmoid, bk(("P", 2)), bk(("P", 2)), scale=1.5957691216057308)
        tt("vector", gb_[:, 0:S_LEN], gb_[:, 0:S_LEN], rb_[:, 0:S_LEN], ALU.mult, bk(("P", 2)) + bk(("P", 4)), bk(("P", 4)))
        ylb = rb_[:, 0:1024].bitcast(BF16)
        tt("vector", ylb, gb_[:, 0:S_LEN], ub[:, 0:S_LEN], ALU.mult, bk(("P", 1)) + bk(("P", 4)) + bk(("P", 2)), bk(("P", 2)))
        if l == 0 and i == 0:
            dump("ylru0", gb_[:, 0:S_LEN], bk(("P", 4)))
        if stop == "lruE":
            return
        wout_partial(l, xsrc, 640 + c0, 128, lambda b: ylb[:, b * 512:(b + 1) * 512], lambda b: bk(("P", 2)), hsrc)

    def fence(keys_wait, keys_new):
        S.op("gpsimd", lambda e: e.memset(fz[:, 0:1], 0.0), (), list(keys_wait) + list(keys_new) + ["fz"])

    def moe(l, xsrc, hsrc):
        H = bufs[hsrc]
        acc = bufs[xsrc][:, :, :].rearrange("p c t -> p (c t)").rearrange("p (t d) -> p t d", t=16)
        ak = lambda t: [(xsrc, t // 2, 8 * (t % 2) + k_) for k_ in range(8)]
        hb = lambda c: P[c // 2][:, 0:2048].bitcast(BF16)[:, (c % 2) * 2048:(c % 2 + 1) * 2048]
        hbk = lambda c: bk(("P", c // 2))
        for c in range(8):
            cp("vector" if c % 2 == 0 else "gpsimd", hb(c), H[:, c, :], tl(hsrc, [c], 0, S_LEN), hbk(c))
        rwv = wo[:, 0:256].rearrange("p (k n) -> p k n", k=8)
        dma(rwv, rw_d[l].rearrange("(kc p) n -> p kc n", p=128), [], ["wo"])
        gtT = P[4]
        for t in range(16):
            pi = ps_rot()
            for kc in range(8):
                mm(ps[pi][:, 0:NE], H[:, kc, t * 128:(t + 1) * 128], rwv[:, kc, :], kc == 0, kc == 7,
                   ["wo"] + tl(hsrc, [kc], t * 128, t * 128 + 128), [("ps", pi)])
            lg = St[0]
            tt("vector", lg[:, 0:NE], ps[pi][:, 0:NE], rbt[:, :], ALU.add, [("ps", pi), "rbt"], [("S", 0)])
            S.op("vector", lambda e, lg=lg: e.max(lg[:, 32:40], lg[:, 0:NE]), [("S", 0)], [("S", 0)])
            ts("vector", lg[:, 40:41], lg[:, 32:33], -1.0, None, ALU.mult, None, [("S", 0)], [("S", 0)])
            ts("vector", lg[:, 64:96], lg[:, 0:NE], lg[:, 35:36], None, ALU.is_ge, None, [("S", 0)], [("S", 0)])
            act(lg[:, 96:128], lg[:, 0:NE], AF.Exp, [("S", 0)], [("S", 0)], bias=lg[:, 40:41], scale=1.0)
            tt("vector", lg[:, 96:128], lg[:, 96:128], lg[:, 64:96], ALU.mult, [("S", 0)], [("S", 0)])
            S.op("vector", lambda e, lg=lg: e.tensor_reduce(lg[:, 41:42], lg[:, 96:128], AX.X, ALU.add), [("S", 0)], [("S", 0)])
            recip(lg[:, 41:42], lg[:, 41:42], [("S", 0)], [("S", 0)])
            ts("vector", Gt[:, t, :], lg[:, 96:128], lg[:, 41:42], None, ALU.mult, None, [("S", 0)], ["G"])
            pt = ps_rot()
            mm(ps[pt][0:NE, 0:128], Gt[:, t, :], ident, True, True, ["G", "cst"], [("ps", pt)])
            cp("scalar", gtT[0:NE, t * 128:(t + 1) * 128], ps[pt][0:NE, 0:128], [("ps", pt)], [(("P", 4), t // 4)])
        if l == 0:
            dump("gates0", Gt[:, :, :], ["G"])
        dma(wo[0:NE, 0:D], bdn_d[l], [], ["wo"])
        for t in range(16):
            for hf in range(2):
                pi = ps_rot()
                mm(ps[pi][:, :], gtT[0:NE, t * 128:(t + 1) * 128], wo[0:NE, hf * 512:(hf + 1) * 512], True, True,
                   [(("P", 4), t // 4), "wo"], [("ps", pi)])
                cp("vector" if hf else "scalar", acc[:, t, hf * 512:(hf + 1) * 512], ps[pi][:, :], [("ps", pi)], ak(t))
        Bf = H[:, :, :].rearrange("p c t -> p (c t)")
        actall = Bf[:, 0:8192].bitcast(BF16)
        actv = lambda tb, j: actall[:, (tb * 8 + j) * 512:(tb * 8 + j + 1) * 512]
        wst = lambda s_: Bf[:, 8192 + s_ * 2048:8192 + (s_ + 1) * 2048].rearrange("p (k n) -> p k n", k=8)
        wgb = lambda s_: Bf[:, 12288 + s_ * 1024:12288 + (s_ + 1) * 1024].bitcast(BF16).rearrange("p (k n) -> p k n", k=8)
        dstg = lambda s_: Bf[:, 14336 + s_ * 1024:14336 + (s_ + 1) * 1024]
        wdb = lambda j: St[j][:, :].bitcast(BF16)
        wbf = [wbuf[i][:, :, :].rearrange("p k n -> p (k n)") for i in range(2)]
        T = [P[4][:, k * 512:(k + 1) * 512] for k in range(4)] + [wbf[0][:, 0:512], wbf[0][:, 512:1024], wbf[1][:, 0:512], wbf[1][:, 512:1024]]
        Tk = [(("P", 4), k) for k in range(4)] + [("wbh", 0, 0), ("wbh", 0, 1), ("wbh", 1, 0), ("wbh", 1, 1)]
        ovk = [("ov", "act", tb, j) for tb in range(4) for j in range(8)] + \
              [("ov", n_, s_) for n_ in ("wst", "wgb", "dst") for s_ in range(2)] + Tk[4:]
        oldk = tl(hsrc, range(8), 0, S_LEN) + [("wb", 0), ("wb", 1)]
        fence(oldk, ovk)

        pieces = [(e_, j) for e_ in range(NE) for j in range(8)]
        it_ = [0]

        def load_piece(p):
            e_, j = pieces[p]
            s_ = p % 2
            wg = wgu_d[l, e_].rearrange("(kc p) n -> p kc n", p=128)
            dma(wst(s_)[:, :, 0:128], wg[:, :, j * 128:(j + 1) * 128], [], [("ov", "wst", s_)])
            dma(wst(s_)[:, :, 128:256], wg[:, :, D + j * 128:D + (j + 1) * 128], [], [("ov", "wst", s_)])
            dma(dstg(s_), wdn_d[l, e_, j * 128:(j + 1) * 128, :], [], [("ov", "dst", s_)])
            act(wgb(s_), wst(s_), AF.Identity, [("ov", "wst", s_)], [("ov", "wgb", s_)])

        load_piece(0)
        for p, (e_, j) in enumerate(pieces):
            s_ = p % 2
            if p + 1 < len(pieces):
                load_piece(p + 1)
            cp("gpsimd", wdb(j), dstg(s_), [("ov", "dst", s_)], [("S", j)])
            bg = sp[:, SP_BGU + e_ * 16 + j:SP_BGU + e_ * 16 + j + 1]
            bu = sp[:, SP_BGU + e_ * 16 + 8 + j:SP_BGU + e_ * 16 + 8 + j + 1]
            for tb in range(4):
                pg, pu = ps_acc(), ps_acc()
                for (pi, o) in ((pg, 0), (pu, 128)):
                    for kc in range(8):
                        mm(ps[pi][:, :], wgb(s_)[:, kc, o:o + 128], hb(kc)[:, tb * 512:(tb + 1) * 512], kc == 0, kc == 7,
                           [("ov", "wgb", s_)] + hbk(kc), [("ps", pi)])
                k3 = (it_[0] % 3) * 2
                it_[0] += 1
                g1, u2 = T[k3], T[k3 + 1]
                k0, k2 = Tk[k3], Tk[k3 + 1]
                ts("vector", g1, ps[pg][:, :], bg, 7.0, ALU.add, ALU.min, [("ps", pg), "sp"], [k0])
                act(g1, g1, AF.Silu, [k0], [k0], scale=1.702)
                ts("vector", u2, ps[pu][:, :], bu, 7.0, ALU.add, ALU.min, [("ps", pu), "sp"], [k2])
                ts("vector", u2, u2, -7.0, 1.0, ALU.max, ALU.add, [k2], [k2])
                stt(actv(tb, j), g1, 1.0 / 1.702, u2, ALU.mult, ALU.mult, [k0, k2], [("ov", "act", tb, j)])
            if j == 7:
                for t in range(16):
                    tb, tq = t // 4, t % 4
                    for hf in range(2):
                        pi = ps_rot()
                        for jj in range(8):
                            mm(ps[pi][:, :], actv(tb, jj)[:, tq * 128:(tq + 1) * 128], wdb(jj)[:, hf * 512:(hf + 1) * 512],
                               jj == 0, jj == 7, [("ov", "act", tb, jj), ("S", jj)], [("ps", pi)])
                        act(T[6 + hf], ps[pi][:, :], AF.Identity, [("ps", pi), "G"], [Tk[6 + hf]], scale=Gt[:, t, e_:e_ + 1])
                        tt("gpsimd", acc[:, t, hf * 512:(hf + 1) * 512], acc[:, t, hf * 512:(hf + 1) * 512], T[6 + hf], ALU.add,
                           [Tk[6 + hf]] + ak(t), ak(t))
        fence(ovk, oldk)

    def moe_finish(l, accsrc, dst):
        acc = bufs[accsrc][:, :, :].rearrange("p c t -> p (c t)").rearrange("p (t d) -> p t d", t=16)
        ak = lambda t: [(accsrc, t // 2, 8 * (t % 2) + k_) for k_ in range(8)]
        Xn = bufs[dst]
        for t in range(16):
            xo_i = t % 2
            xo = P[xo_i][:, 0:1024].rearrange("p (c q) -> p c q", c=8)
            dma(xo, xsp_d[:, :, t * 128:(t + 1) * 128], ["xsp"], bk(("P", xo_i)))
            for half in range(2):
                pi = ps_rot()
                for q in range(4):
                    c = half * 4 + q
                    tp(ps[pi][:, q * 128:(q + 1) * 128], acc[:, t, c * 128:(c + 1) * 128], ident, ak(t) + ["cst"], [("ps", pi)])
                for q in range(4):
                    c = half * 4 + q
                    stt(Xn[:, c, t * 128:(t + 1) * 128], ps[pi][:, q * 128:(q + 1) * 128], mv[:, 40 + c:41 + c], xo[:, c, :],
                        ALU.mult, ALU.add, [("ps", pi), "mv"] + bk(("P", xo_i)), tl(dst, [c], t * 128, t * 128 + 128))

    def final_out(src, other):
        X, Y = bufs[src], bufs[other]
        rstd_compute(src, ("P", 0), P[0])
        for c in range(8):
            tmp_i = 1 + c % 2
            tt("vector", P[tmp_i][:, 0:S_LEN], X[:, c, :], P[0][:, 0:S_LEN], ALU.mult, tl(src, [c], 0, S_LEN) + bk(("P", 0)), bk(("P", tmp_i)))
            act(Y[:, c, :], P[tmp_i][:, 0:S_LEN], AF.Identity, bk(("P", tmp_i)) + ["gsp"], tl(other, [c], 0, S_LEN), scale=gsp[:, 8 + c:9 + c])
        for t in range(16):
            stg_i = 3 + t % 2
            stg = P[stg_i]
            for half in range(2):
                pi = ps_rot()
                for q in range(4):
                    c = half * 4 + q
                    tp(ps[pi][:, q * 128:(q + 1) * 128], Y[:, c, t * 128:(t + 1) * 128], ident,
                       tl(other, [c], t * 128, t * 128 + 128) + ["cst"], [("ps", pi)])
                cp("vector" if half == 0 else "scalar", stg[:, half * 512:(half + 1) * 512], ps[pi][:, :], [("ps", pi)], bk(("P", stg_i)))
            dma(out_d[t * 128:(t + 1) * 128, :], stg[:, 0:D], bk(("P", stg_i)), [("out", t)])

    cur, oth = "A", "B"
    load_x(cur)
    for l in range(L):
        layer_prologue(l)
        if stop == "mod":
            break
        rstd_compute(cur, ("P", 0), P[0])
        mix_fence(oth)
        norm_mod(cur, oth, ("P", 0), P[0], 0, 8, bf=True)
        if l == 0:
            dump("h0", bufs[oth][:, :, :], tl(oth, range(8), 0, S_LEN))
        if stop == "h":
            break
        for h in range(4):
            retention_head(l, h, cur, oth)
            if stop == "ret0":
                break
        if stop in ("ret0", "ret"):
            break
        for h in range(6):
            moba_head(l, h, cur, oth)
            if stop == "moba0":
                break
        if stop in ("moba0", "moba"):
            break
        for i in range(3):
            lru_chunk(l, i, cur, oth)
            if stop in ("lru0", "lruA", "lruB", "lruC", "lruD", "lruE"):
                break
        if l == 0:
            dump("xmix0", bufs[cur][:, :, :], tl(cur, range(8), 0, S_LEN))
        if stop in ("lru0", "mix", "lruA", "lruB", "lruC", "lruD", "lruE"):
            break
        rstd_compute(cur, ("P", 0), P[0])
        mix_fence(oth)
        norm_mod(cur, oth, ("P", 0), P[0], 24, 32)
        for c_ in range(8):
            dma(xsp_d[:, c_, :], bufs[cur][:, c_, :], tl(cur, [c_], 0, S_LEN), ["xsp"])
        moe(l, cur, oth)
        moe_finish(l, cur, oth)
        cur, oth = oth, cur
    final_out(cur, oth)
    S.wait_keys("sync", [("out", t) for t in range(16)] + [("dbg", n) for n in dbg_d])

    def replay(name, e):
        for waits, fn, tok in S.streams[name]:
            for s_, v in waits:
                e.wait_ge(sems[s_], v)
            if fn is None:
                continue
            inst = fn(e)
            if tok[0] == name:
                inst.then_inc(sems[name], 1)
            else:
                inst.then_inc(sems[tok[0]], 16)

    with nc.Block() as block:
        @block.sync
        def _(e):
            replay("sync", e)

        @block.scalar
        def _(e):
            replay("scalar", e)

        @block.vector
        def _(e):
            replay("vector", e)

        @block.gpsimd
        def _(e):
            replay("gpsimd", e)

        @block.tensor
        def _(e):
            replay("tensor", e)
    es.close()
    return nc


def _consts():
    cst = np.zeros((128, 6 * 128), np.float32)
    cst[:, 0:128] = np.eye(128, dtype=np.float32)
    cst[:, 128:256] = 1.0
    a = np.zeros((128, 128), np.float32)
    a[0:64, 0:64] = 1.0 / 64
    a[64:128, 64:128] = 1.0 / 64
    cst[:, 256:384] = a
    cst[:, 384:448] = 1.0
    cst[:, 576:640] = 1.0
    p = np.arange(128)[:, None]
    c = np.arange(128)[None, :]
    cst[:, 640:768] = (c >= p).astype(np.float32)
    kind = np.zeros((8, S_LEN), np.float32)
    for n in range(8):
        kind[n, n * 256:(n + 1) * 256] = 1.0
    pos = np.arange(S_LEN, dtype=np.float64)
    half = 32
    inv = 1.0 / (10000.0 ** (np.arange(half, dtype=np.float64) / half))
    ang = pos[None, :] * inv[:, None]
    cos = np.concatenate([np.cos(ang), np.cos(ang)], 0)
    sin = np.concatenate([-np.sin(ang), np.sin(ang)], 0)
    rot = np.zeros((4, 4, 64, S_LEN), np.float64)
    for h in range(4):
        lg = np.log1p(-2.0 ** (-5.0 - h))
        dq = np.exp(lg * pos)[None, :]
        dk = np.exp(-lg * pos)[None, :] * (64 ** -0.5)
        rot[0, h] = cos * dq
        rot[1, h] = sin * dq
        rot[2, h] = cos * dk
        rot[3, h] = sin * dk
    return cst, kind, rot.astype(np.float32)


def _fm(v):
    v = np.asarray(v, np.float32)
    return np.ascontiguousarray(v.reshape(-1, 128).T)


def prepare_inputs(inp):
    cst, kind, rot = _consts()
    sp = np.zeros((NL, 128, SP_N), np.float32)
    wabd = np.zeros((NL, 3, 128, 128), np.float32)
    wxbd = np.zeros((NL, 3, 128, 128), np.float32)
    rbt = np.zeros((NL, 128, NE), np.float32)
    for l in range(NL):
        sp[l, :, SP_ADAB:SP_ADAB + 48] = _fm(inp["ada_b"][l])
        sp[l, :, SP_NW1:SP_NW1 + 8] = _fm(inp["norm_mix_w"][l])
        sp[l, :, SP_NW2:SP_NW2 + 8] = _fm(inp["norm_ffn_w"][l])
        sp[l, 0:64, SP_RNW:SP_RNW + 4] = np.asarray(inp["ret_norm_w"][l], np.float32).reshape(4, 64).T
        for j in range(4):
            sp[l, :, SP_CW + j * 3:SP_CW + j * 3 + 3] = _fm(inp["lru_conv_w"][l, j])
        sp[l, :, SP_CB:SP_CB + 3] = _fm(inp["lru_conv_b"][l])
        sp[l, :, SP_BA:SP_BA + 3] = _fm(inp["lru_gate_a_b"][l])
        sp[l, :, SP_BX:SP_BX + 3] = _fm(inp["lru_gate_x_b"][l])
        sp[l, :, SP_LAM:SP_LAM + 3] = _fm(inp["lru_lambda"][l])
        for e in range(NE):
            sp[l, :, SP_BGU + e * 16:SP_BGU + e * 16 + 16] = _fm(inp["moe_b_gu"][l, e])
        for i in range(3):
            for g in range(2):
                wabd[l, i, g * 64:(g + 1) * 64, g * 64:(g + 1) * 64] = inp["lru_gate_a_w"][l, 2 * i + g]
                wxbd[l, i, g * 64:(g + 1) * 64, g * 64:(g + 1) * 64] = inp["lru_gate_x_w"][l, 2 * i + g]
        rbt[l] = np.broadcast_to(np.asarray(inp["router_b"][l], np.float32)[None, :], (128, NE))
    shared = {
        "sp": sp, "rbt": rbt, "bdn": np.ascontiguousarray(inp["moe_b_down"], dtype=np.float32),
        "ada_w": np.ascontiguousarray(inp["ada_w"], dtype=np.float32),
        "w_in": np.ascontiguousarray(inp["w_in"], dtype=np.float32),
        "w_out": np.ascontiguousarray(inp["w_out"], dtype=np.float32),
        "router_w": np.ascontiguousarray(inp["router_w"], dtype=np.float32),
        "moe_w_gu": np.ascontiguousarray(inp["moe_w_gu"], dtype=np.float32),
        "moe_w_down": np.ascontiguousarray(inp["moe_w_down"], dtype=np.float32),
        "wabd": wabd, "wxbd": wxbd, "cst": cst, "kind": kind, "rot": rot,
    }
    maps = []
    for b in range(inp["x"].shape[0]):
        gsp = np.zeros((128, 16), np.float32)
        gsp[:, 0:8] = _fm(inp["c"][b])
        gsp[:, 8:16] = _fm(inp["final_norm_w"])
        m = dict(shared)
        m["x"] = np.ascontiguousarray(inp["x"][b], dtype=np.float32)
        m["gsp"] = gsp
        maps.append(m)
    return maps


def kernel(**inputs):
    inp = {k: np.asarray(v) for k, v in inputs.items()}
    maps = prepare_inputs(inp)
    nc = build_program()
    res = run_bass_kernel_spmd(nc, maps, core_ids=list(range(len(maps))))
    out = np.stack([np.asarray(r["out"], dtype=np.float32) for r in res.results], axis=0)
    return out
```
